# Optimizing a Trainium2 kernel written in Bass

```python
import math
import jax, jax.numpy as jnp
from jax import lax
import numpy as np

D_MODEL = 1024
BATCH = 8
SEQ = 4096
DEPTH = 2

GRID_W = 64
CTX_LEN = 256

SSD_EXPAND = 2
SSD_INNER = SSD_EXPAND * D_MODEL
SSD_HEAD_DIM = 64
SSD_HEADS = SSD_INNER // SSD_HEAD_DIM
SSD_GROUPS = 4
SSD_HPG = SSD_HEADS // SSD_GROUPS
SSD_STATE = 128
SSD_CHUNK = 64
SSD_CONV = 3
XBC_WIDTH = SSD_INNER + 2 * SSD_GROUPS * SSD_STATE

SC_WIDTH = D_MODEL
SC_CONV = 3

N_BRANCH = 2
N_MOD = 6
IN_WIDTH = SSD_INNER + XBC_WIDTH + 2 * SSD_HEADS + 3 * SC_WIDTH + N_BRANCH * D_MODEL

FFN_DENSE = 2816
N_EXPERTS = 8
TOP_K = 2
FFN_EXPERT = 3584
N_DENSE = (DEPTH + 1) // 2
N_MOE = DEPTH // 2

EPS = 1e-6

kernel_name = "hybrid_ssd_shortconv_moe_diffusion_block"


def rmsnorm(x, g):
    xf = x.astype(jnp.float32)
    y = xf * lax.rsqrt(jnp.mean(xf * xf, axis=-1, keepdims=True) + EPS)
    return (y * g.astype(jnp.float32)).astype(x.dtype)


def modulate(h, shift, scale):
    return h * (1.0 + scale) + shift


def dwconv_centred(u, w, axis):
    k = w.shape[0]
    p = k // 2
    n = u.shape[axis]
    pad = [(0, 0)] * u.ndim
    pad[axis] = (p, p)
    up = jnp.pad(u, pad)
    return sum(lax.slice_in_dim(up, j, j + n, axis=axis) * w[j] for j in range(k))


def split_proj(proj):
    offs = np.cumsum([SSD_INNER, XBC_WIDTH, 2 * SSD_HEADS, 3 * SC_WIDTH]).tolist()
    return jnp.split(proj, offs, axis=-1)


def ssd_inputs(xbc, conv_w, conv_b):
    xbc = jax.nn.silu(dwconv_centred(xbc, conv_w, axis=1) + conv_b)
    xs, bm, cm = jnp.split(xbc, [SSD_INNER, SSD_INNER + SSD_GROUPS * SSD_STATE], axis=-1)
    b, n = xs.shape[:2]
    return (xs.reshape(b, n, SSD_HEADS, SSD_HEAD_DIM),
            bm.reshape(b, n, SSD_GROUPS, SSD_STATE),
            cm.reshape(b, n, SSD_GROUPS, SSD_STATE))


def ssd_dt(dt_raw, dt_bias, a_log):
    dt = jax.nn.softplus(dt_raw.astype(jnp.float32) + dt_bias.reshape(-1).astype(jnp.float32))
    a = -jnp.exp(a_log.astype(jnp.float32))
    return dt[..., :SSD_HEADS], dt[..., SSD_HEADS:], a[0], a[1]


def ssd_scan(x, dt, a, bm, cm, s0):
    b, n = x.shape[:2]
    nc = n // SSD_CHUNK
    xc = (x * dt[..., None]).reshape(b, nc, SSD_CHUNK, SSD_GROUPS, SSD_HPG, SSD_HEAD_DIM)
    a_cs = jnp.cumsum((dt * a).reshape(b, nc, SSD_CHUNK, SSD_GROUPS, SSD_HPG), axis=2)
    bc = bm.reshape(b, nc, SSD_CHUNK, SSD_GROUPS, SSD_STATE)
    cc = cm.reshape(b, nc, SSD_CHUNK, SSD_GROUPS, SSD_STATE)
    seg = a_cs[:, :, :, None] - a_cs[:, :, None, :]
    lower = jnp.tril(jnp.ones((SSD_CHUNK, SSD_CHUNK), dtype=bool))[:, :, None, None]
    decay_ls = jnp.exp(jnp.where(lower, seg, -jnp.inf))
    scores = jnp.einsum("bclgn,bcsgn->bclsg", cc, bc)
    y_diag = jnp.einsum("bclsgh,bcsghp->bclghp", scores[..., None] * decay_ls, xc)
    decay_to_end = jnp.exp(a_cs[:, :, -1:] - a_cs)
    chunk_states = jnp.einsum("bclgn,bclghp->bcghpn", bc, xc * decay_to_end[..., None])
    chunk_decay = jnp.exp(a_cs[:, :, -1])

    def step(s, inp):
        st, dec = inp
        return s * dec[..., None, None] + st, s

    s_final, s_enter = lax.scan(step, s0, (jnp.moveaxis(chunk_states, 1, 0),
                                           jnp.moveaxis(chunk_decay, 1, 0)))
    s_enter = jnp.moveaxis(s_enter, 0, 1)
    y_off = jnp.einsum("bclgn,bcghpn->bclghp", cc, s_enter) * jnp.exp(a_cs)[..., None]
    y = (y_diag + y_off).reshape(b, n, SSD_HEADS, SSD_HEAD_DIM)
    return y, s_final


def ssd_final_state(x, dt, a, bm):
    b, n = x.shape[:2]
    a_cs = jnp.cumsum(dt * a, axis=1)
    w = jnp.exp(a_cs[:, -1:] - a_cs) * dt
    xw = (x * w[..., None]).reshape(b, n, SSD_GROUPS, SSD_HPG, SSD_HEAD_DIM)
    return jnp.einsum("blgn,blghp->bghpn", bm, xw)


def flip(t):
    return jnp.flip(t, axis=1)


def short_conv(sc, conv_w, grid_rows):
    gb, gc, hv = jnp.split(sc, 3, axis=-1)
    u = gc * hv
    if grid_rows is None:
        v = dwconv_centred(u, conv_w, axis=1)
    else:
        b, n, ch = u.shape
        v = dwconv_centred(u.reshape(b, grid_rows, GRID_W, ch), conv_w, axis=2).reshape(b, n, ch)
    return gb * v


def token_mixer(h, s0_f, s0_b, grid_rows, w_in, b_gate, conv_w, conv_b, dt_bias, a_log,
                d_skip, ssd_norm_g, w_ssd_out, sc_conv_w, w_sc_out, w_o):
    b, n, _ = h.shape
    z, xbc, dt_raw, sc, gl = split_proj(h @ w_in)
    xs, bm, cm = ssd_inputs(xbc, conv_w, conv_b)
    dt_f, dt_b, a_f, a_b = ssd_dt(dt_raw, dt_bias, a_log)
    y_f, s_f = ssd_scan(xs, dt_f, a_f, bm, cm, s0_f)
    y_b, s_b = ssd_scan(flip(xs), flip(dt_b), a_b, flip(bm), flip(cm), s0_b)
    y = y_f + flip(y_b) + d_skip.astype(jnp.float32)[:, None] * xs.astype(jnp.float32)
    y = y.reshape(b, n, SSD_INNER) * jax.nn.silu(z.astype(jnp.float32))
    y_ssd = rmsnorm(y, ssd_norm_g).astype(h.dtype) @ w_ssd_out
    y_sc = short_conv(sc, sc_conv_w, grid_rows) @ w_sc_out
    g = jax.nn.sigmoid((gl + b_gate).astype(jnp.float32)).reshape(b, n, N_BRANCH, D_MODEL).astype(h.dtype)
    out = (g[:, :, 0] * y_ssd + g[:, :, 1] * y_sc) @ w_o
    return out, s_f, s_b


def context_states(hc, w_in, conv_w, conv_b, dt_bias, a_log):
    cols = hc @ w_in[:, SSD_INNER:SSD_INNER + XBC_WIDTH + 2 * SSD_HEADS]
    xbc, dt_raw = jnp.split(cols, [XBC_WIDTH], axis=-1)
    xs, bm, _ = ssd_inputs(xbc, conv_w, conv_b)
    dt_f, dt_b, a_f, a_b = ssd_dt(dt_raw, dt_bias, a_log)
    s_f = ssd_final_state(xs, dt_f, a_f, bm)
    s_b = ssd_final_state(flip(xs), flip(dt_b), a_b, flip(bm))
    return s_f, s_b


def swiglu(h, w1, w3, w2):
    return (jax.nn.silu(h @ w1) * (h @ w3)) @ w2


def moe_swiglu(h, router_w, w1, w3, w2):
    shp = h.shape
    t = h.reshape(-1, shp[-1])
    logits = (t @ router_w).astype(jnp.float32)
    top_v, top_i = lax.top_k(logits, TOP_K)
    top_w = jax.nn.softmax(top_v, axis=-1)
    gates = jnp.sum(jax.nn.one_hot(top_i, N_EXPERTS, dtype=jnp.float32) * top_w[..., None], axis=1)
    out = jnp.zeros_like(t)
    for e in range(N_EXPERTS):
        out = out + gates[:, e:e + 1].astype(t.dtype) * swiglu(t, w1[e], w3[e], w2[e])
    return out.reshape(shp)


def channel_mixer(h, i, ffn_w1, ffn_w3, ffn_w2, router_w, moe_w1, moe_w3, moe_w2):
    j = i // 2
    if i % 2 == 0:
        return swiglu(h, ffn_w1[j], ffn_w3[j], ffn_w2[j])
    return moe_swiglu(h, router_w[j], moe_w1[j], moe_w3[j], moe_w2[j])


def setup_inputs(seed: int = 0) -> dict:
    key = jax.random.key(seed)
    ks = iter(jax.random.split(key, 40))
    D = D_MODEL

    def nrm(shape, scale):
        return jax.random.normal(next(ks), shape, jnp.float32) * scale

    dt0 = jnp.exp(jax.random.uniform(next(ks), (DEPTH, 2, SSD_HEADS), jnp.float32,
                                     minval=math.log(1e-3), maxval=math.log(1e-1)))
    dt_bias = dt0 + jnp.log(-jnp.expm1(-dt0))
    a_log = jnp.log(jax.random.uniform(next(ks), (DEPTH, 2, SSD_HEADS), jnp.float32, minval=1.0, maxval=16.0))
    return {
        "x": nrm((BATCH, SEQ, D), 1.0),
        "c": nrm((BATCH, D), 1.0),
        "ctx": nrm((BATCH, CTX_LEN, D), 1.0),
        "c_ctx": nrm((D,), 1.0),
        "w_mod": nrm((DEPTH, D, N_MOD * D), 0.5 * D ** -0.5),
        "b_mod": nrm((DEPTH, N_MOD * D), 0.02),
        "norm1_g": 1.0 + nrm((DEPTH, D), 0.1),
        "norm2_g": 1.0 + nrm((DEPTH, D), 0.1),
        "w_in": nrm((DEPTH, D, IN_WIDTH), D ** -0.5),
        "b_gate": nrm((DEPTH, N_BRANCH * D), 0.1),
        "ssd_conv_w": nrm((DEPTH, SSD_CONV, XBC_WIDTH), SSD_CONV ** -0.5),
        "ssd_conv_b": nrm((DEPTH, XBC_WIDTH), 0.02),
        "ssd_dt_bias": dt_bias,
        "ssd_a_log": a_log,
        "ssd_d": 1.0 + nrm((DEPTH, SSD_HEADS), 0.1),
        "ssd_norm_g": 1.0 + nrm((DEPTH, SSD_INNER), 0.1),
        "w_ssd_out": nrm((DEPTH, SSD_INNER, D), SSD_INNER ** -0.5),
        "sc_conv_w": nrm((DEPTH, SC_CONV, SC_WIDTH), SC_CONV ** -0.5),
        "w_sc_out": nrm((DEPTH, SC_WIDTH, D), SC_WIDTH ** -0.5),
        "w_o": nrm((DEPTH, D, D), D ** -0.5),
        "ffn_w1": nrm((N_DENSE, D, FFN_DENSE), D ** -0.5),
        "ffn_w3": nrm((N_DENSE, D, FFN_DENSE), D ** -0.5),
        "ffn_w2": nrm((N_DENSE, FFN_DENSE, D), FFN_DENSE ** -0.5),
        "router_w": nrm((N_MOE, D, N_EXPERTS), D ** -0.5),
        "moe_w1": nrm((N_MOE, N_EXPERTS, D, FFN_EXPERT), D ** -0.5),
        "moe_w3": nrm((N_MOE, N_EXPERTS, D, FFN_EXPERT), D ** -0.5),
        "moe_w2": nrm((N_MOE, N_EXPERTS, FFN_EXPERT, D), FFN_EXPERT ** -0.5),
        "final_g": 1.0 + nrm((D,), 0.1),
    }


def reference(x, c, ctx, c_ctx, w_mod, b_mod, norm1_g, norm2_g, w_in, b_gate, ssd_conv_w,
              ssd_conv_b, ssd_dt_bias, ssd_a_log, ssd_d, ssd_norm_g, w_ssd_out, sc_conv_w,
              w_sc_out, w_o, ffn_w1, ffn_w3, ffn_w2, router_w, moe_w1, moe_w3, moe_w2, final_g):
    b = x.shape[0]
    rows = x.shape[1] // GRID_W
    for i in range(DEPTH):
        last = i == DEPTH - 1
        mx = (jax.nn.silu(c) @ w_mod[i] + b_mod[i]).reshape(b, N_MOD, 1, D_MODEL)
        mc = (jax.nn.silu(c_ctx) @ w_mod[i] + b_mod[i]).reshape(N_MOD, D_MODEL)
        mix_p = (w_in[i], b_gate[i], ssd_conv_w[i], ssd_conv_b[i], ssd_dt_bias[i], ssd_a_log[i],
                 ssd_d[i], ssd_norm_g[i], w_ssd_out[i], sc_conv_w[i], w_sc_out[i], w_o[i])

        hc = modulate(rmsnorm(ctx, norm1_g[i]), mc[0], mc[1])
        hx = modulate(rmsnorm(x, norm1_g[i]), mx[:, 0], mx[:, 1])
        if last:
            s_f, s_b = context_states(hc, w_in[i], ssd_conv_w[i], ssd_conv_b[i], ssd_dt_bias[i], ssd_a_log[i])
        else:
            zeros = jnp.zeros((b, SSD_GROUPS, SSD_HPG, SSD_HEAD_DIM, SSD_STATE), jnp.float32)
            yc, s_f, s_b = token_mixer(hc, zeros, zeros, None, *mix_p)
            ctx = ctx + mc[2] * yc
        yx, _, _ = token_mixer(hx, s_f, s_b, rows, *mix_p)
        x = x + mx[:, 2] * yx

        hx = modulate(rmsnorm(x, norm2_g[i]), mx[:, 3], mx[:, 4])
        x = x + mx[:, 5] * channel_mixer(hx, i, ffn_w1, ffn_w3, ffn_w2, router_w, moe_w1, moe_w3, moe_w2)
        if not last:
            hc = modulate(rmsnorm(ctx, norm2_g[i]), mc[3], mc[4])
            ctx = ctx + mc[5] * channel_mixer(hc, i, ffn_w1, ffn_w3, ffn_w2, router_w, moe_w1, moe_w3, moe_w2)
    return rmsnorm(x, final_g)
```

```python
import contextlib
import numpy as np
import concourse.bass as bass
import concourse.mybir as mybir
from concourse.bass_utils import run_bass_kernel_spmd

F32 = mybir.dt.float32
BF16 = mybir.dt.bfloat16
AF = mybir.ActivationFunctionType
ALU = mybir.AluOpType

D = 1024
SEQ = 4096
CTX = 256
DEPTH = 2
INNER = 2048
NH = 32
NG = 4
XBC = 3072
INW = 10304
C_Z, C_XBC, C_DT, C_SC, C_GL = 0, 2048, 5120, 5184, 8256
FFN_DENSE = 2816
FFN_EXP = 3584
NEXP = 8
EPS = 1e-6
TILE = 256
WIN = TILE + 2

NDS = 12
SAME_ENGINE_SYNC = True


class KB:
    def __init__(self):
        self.nc = bass.Bass("TRN2", target_bir_lowering=False)
        nc = self.nc
        self.es = contextlib.ExitStack()
        self.eng = {"pe": nc.tensor, "act": nc.scalar, "dve": nc.vector, "pool": nc.gpsimd, "sp": nc.sync}
        self.sem, self.cnt = {}, {}
        for e in self.eng:
            self.sem[e] = self.es.enter_context(nc.semaphore("s_" + e))
            self.cnt[e] = 0
        self.dsems, self.dcount, self.drr = {}, {}, {}
        self.semobj = {}
        for e, s in self.sem.items():
            self.semobj[("c", e)] = s
        for q in ("sp", "pool", "act"):
            self.dsems[q] = []
            for i in range(NDS):
                s = self.es.enter_context(nc.semaphore(f"d_{q}{i}"))
                self.dsems[q].append(("d", q, i))
                self.semobj[("d", q, i)] = s
                self.dcount[("d", q, i)] = 0
            self.drr[q] = 0
        self.waited, self.res_w, self.res_r = {}, {}, {}
        self.ninst = 0

    def sb(self, name, shape, dt=F32, stack=None):
        self.nuid = getattr(self, "nuid", 0) + 1
        return (stack or self.es).enter_context(self.nc.sbuf_tensor(f"{name}_u{self.nuid}", list(shape), dt))

    def ps(self, name, shape, dt=F32, stack=None):
        return (stack or self.es).enter_context(self.nc.psum_tensor(name, list(shape), dt))

    def dram(self, name, shape, dt=F32, kind="Internal"):
        return self.nc.dram_tensor(name, list(shape), dt, kind=kind).ap()

    def _wait(self, e, key, val):
        if key == ("c", e) and (e == "pe" or not SAME_ENGINE_SYNC):
            return
        k = (e, key)
        if self.waited.get(k, 0) >= val:
            return
        self.eng[e].wait_ge(self.semobj[key], val)
        self.waited[k] = val

    def _deps(self, reads, writes):
        deps = {}
        for r in reads:
            t = self.res_w.get(r)
            if t is not None:
                deps[t[0]] = max(deps.get(t[0], 0), t[1])
        for w in writes:
            t = self.res_w.get(w)
            if t is not None:
                deps[t[0]] = max(deps.get(t[0], 0), t[1])
            for k, v in self.res_r.get(w, {}).items():
                deps[k] = max(deps.get(k, 0), v)
        return deps

    def _record(self, token, reads, writes):
        for r in reads:
            d = self.res_r.setdefault(r, {})
            d[token[0]] = max(d.get(token[0], 0), token[1])
        for w in writes:
            self.res_w[w] = token
            self.res_r[w] = {}

    def op(self, e, fn, reads=(), writes=()):
        for key, val in self._deps(reads, writes).items():
            self._wait(e, key, val)
        inst = fn(self.eng[e])
        self.cnt[e] += 1
        inst.then_inc(self.sem[e], 1)
        self._record((("c", e), self.cnt[e]), reads, writes)
        self.ninst += 1
        return inst

    def dma(self, q, out, in_, reads=(), writes=(), **kw):
        i = self.drr[q] % NDS
        self.drr[q] += 1
        key = self.dsems[q][i]
        if self.dcount[key] > 0:
            self._wait(q, key, 16 * self.dcount[key])
        for k, val in self._deps(reads, writes).items():
            self._wait(q, k, val)
        inst = self.eng[q].dma_start(out=out, in_=in_, **kw)
        inst.then_inc(self.semobj[key], 16)
        self.dcount[key] += 1
        self._record((key, 16 * self.dcount[key]), reads, writes)
        self.ninst += 1
        return inst

    def barrier(self):
        for e in self.eng:
            for f in self.eng:
                if f != e and self.cnt[f] > 0:
                    self._wait(e, ("c", f), self.cnt[f])
            for key, c in self.dcount.items():
                if c > 0:
                    self._wait(e, key, 16 * c)

    def finish(self):
        self.barrier()
        self.es.close()
        return self.nc


def bc(ap, shape):
    return ap.to_broadcast(list(shape))


class Prog:
    def __init__(self, debug=False, stop_after=None):
        self.debug = debug
        self.stop_after = stop_after
        self.kb = KB()
        kb = self.kb
        nc = kb.nc
        I = lambda n, s: nc.dram_tensor(n, list(s), F32, kind="ExternalInput").ap()
        self.x = I("x", [SEQ, D])
        self.c = I("c", [D])
        self.ctx = I("ctx", [CTX, D])
        self.c_ctx = I("c_ctx", [D])
        self.w_mod = I("w_mod", [DEPTH, D, 6 * D])
        self.b_mod = I("b_mod", [DEPTH, 6 * D])
        self.norm1_g = I("norm1_g", [DEPTH, D])
        self.norm2_g = I("norm2_g", [DEPTH, D])
        self.w_in = I("w_in", [DEPTH, D, INW])
        self.b_gate = I("b_gate", [DEPTH, 2 * D])
        self.ssd_conv_w = I("ssd_conv_w", [DEPTH, 3, XBC])
        self.ssd_conv_b = I("ssd_conv_b", [DEPTH, XBC])
        self.ssd_dt_bias = I("ssd_dt_bias", [DEPTH, 2, NH])
        self.ssd_a_log = I("ssd_a_log", [DEPTH, 2, NH])
        self.ssd_d = I("ssd_d", [DEPTH, NH])
        self.ssd_norm_g = I("ssd_norm_g", [DEPTH, INNER])
        self.w_ssd_out = I("w_ssd_out", [DEPTH, INNER, D])
        self.sc_conv_w = I("sc_conv_w", [DEPTH, 3, D])
        self.w_sc_out = I("w_sc_out", [DEPTH, D, D])
        self.w_o = I("w_o", [DEPTH, D, D])
        self.ffn_w1 = I("ffn_w1", [1, D, FFN_DENSE])
        self.ffn_w3 = I("ffn_w3", [1, D, FFN_DENSE])
        self.ffn_w2 = I("ffn_w2", [1, FFN_DENSE, D])
        self.router_w = I("router_w", [1, D, NEXP])
        self.moe_w1 = I("moe_w1", [1, NEXP, D, FFN_EXP])
        self.moe_w3 = I("moe_w3", [1, NEXP, D, FFN_EXP])
        self.moe_w2 = I("moe_w2", [1, NEXP, FFN_EXP, D])
        self.final_g = I("final_g", [D])
        self.consts_d = I("consts", [128, 512])
        self.out = nc.dram_tensor("out", [SEQ, D], F32, kind="ExternalOutput").ap()
        self.xT = [kb.dram(f"xT{i}", [D, SEQ]) for i in range(2)]
        self.cT = [kb.dram(f"cT{i}", [D, CTX]) for i in range(2)]
        self.YB = kb.dram("YB", [SEQ, INNER])
        self.XB = kb.dram("XBst", [SEQ // TILE, 128, 24 * TILE], BF16)
        self.XS = kb.dram("XSst", [SEQ // 128, 128, INNER], BF16)
        self.BS = kb.dram("BSst", [SEQ // 128, 128, 512], BF16)
        self.NCS = 4
        self.csrow = kb.dram("csrow", [self.NCS, NH * 128])
        self.csrr = 0
        self.dbg = {}
        self.consts = kb.sb("consts", [128, 512])
        kb.dma("sp", self.consts[:], self.consts_d, writes=["consts"])
        self.ident = self.consts[:, 0:128]
        self.triF = self.consts[:, 128:256]
        self.triB = self.consts[:, 256:384]
        self.ones = self.consts[:, 384:512]
        self.identb = kb.sb("identb", [128, 128], BF16)
        kb.op("dve", lambda e: e.tensor_copy(self.identb[:], self.ident), reads=["consts"], writes=["identb"])
        self.cst = kb.sb("cst", [128, 4])
        kb.op("dve", lambda e: e.memset(self.cst[:, 0:1], 1.0), writes=["cst"])
        kb.op("dve", lambda e: e.memset(self.cst[:, 1:2], EPS), writes=["cst"])
        kb.op("dve", lambda e: e.memset(self.cst[:, 2:3], 0.0), writes=["cst"])
        self.one_c = self.cst[:, 0:1]
        self.eps_c = self.cst[:, 1:2]
        self.pA = kb.ps("pA", [128, 512])
        self.pB = kb.ps("pB", [128, 512])
        self.pT = kb.ps("pT", [128, 512])
        self.pTb = self.pT[:].bitcast(BF16)
        self.pS = kb.ps("pS", [128, 512])
        self.pYd = kb.ps("pYd", [128, 512])
        self.pYo = kb.ps("pYo", [128, 512])
        self.pSt = kb.ps("pSt", [128, 512])
        self.pM = kb.ps("pM", [128, 512])
        self.pab = 0
        self.modres = [kb.sb(f"modres{i}", [128, 2, 6, 8], F32) for i in range(DEPTH)]
        self.rot2 = [(self.pA, "pA"), (self.pB, "pB")]
        self.rot6 = [(self.pA, "pA"), (self.pB, "pB"), (self.pS, "pS"), (self.pYd, "pYd"), (self.pYo, "pYo"), (self.pSt, "pSt")]
        self.rot = self.rot6
        self.wdram = {}

    def load_w(self, w2d, c0, ncols, r0=0, nk=8, key=None):
        kb = self.kb
        slot = self.wrr % self.NWB
        self.wrr += 1
        buf = self.wbuf[slot]
        n = nk * ncols
        assert n <= 4096
        v = buf[:, 0:n].rearrange("p (k n) -> p k n", n=ncols)
        wn = f"wbuf{slot}"
        if key is not None and key in self.wdram:
            kb.dma("sp", buf[:, 0:n], self.wdram[key], reads=[("wd", key)], writes=[wn])
            return v, wn
        src = w2d.rearrange("(kc p) n -> p kc n", p=128)
        for k0 in range(0, nk, 8):
            k1 = min(nk, k0 + 8)
            kb.dma("pool", v[:, k0:k1, :], src[:, r0 + k0:r0 + k1, c0:c0 + ncols], writes=[wn])
        if key is not None:
            scr = kb.dram(f"wd{len(self.wdram)}", [128, n], BF16)
            kb.dma("sp", scr, buf[:, 0:n], reads=[wn], writes=[("wd", key)])
            self.wdram[key] = scr
        return v, wn

    def next_pab(self):
        self.pab = (self.pab + 1) % len(self.rot)
        return self.rot[self.pab]

    def set_wbufs(self, n, stack):
        self.NWB = n
        self.wbuf = [self.kb.sb(f"wbuf{i}", [128, 4096], BF16, stack) for i in range(n)]
        self.wrr = 0

    def colvec(self, name, src1d, n, stack=None):
        kb = self.kb
        t = kb.sb(name, [128, n], F32, stack)
        with kb.nc.allow_non_contiguous_dma(reason="small param vector"):
            kb.dma("sp", t[:], src1d.rearrange("(c p) -> p c", p=128), writes=[name])
        return t

    def rowbc(self, name, src1d, n, stack=None):
        kb = self.kb
        t = kb.sb(name, [128, n], F32, stack)
        kb.dma("sp", t[:], src1d.partition_broadcast(128), writes=[name])
        return t

    def to_fm(self, src_tm, dstT, T):
        kb = self.kb
        with contextlib.ExitStack() as st:
            xin = [kb.sb(f"tfm_in{i}", [128, D], F32, st) for i in range(2)]
            xo = [kb.sb(f"tfm_o{i}", [128, 8, 128], F32, st) for i in range(2)]
            for t in range(T // 128):
                a, o = xin[t % 2], xo[t % 2]
                an, on = f"tfm_in{t % 2}", f"tfm_o{t % 2}"
                kb.dma("sp", a[:], src_tm[t * 128:(t + 1) * 128, :], writes=[an])
                for h in range(2):
                    ps, pn = self.next_pab()
                    for j in range(4):
                        kc = h * 4 + j
                        kb.op("pe", lambda e, ps=ps, j=j, kc=kc, a=a: e.transpose(ps[:, j * 128:(j + 1) * 128], a[:, kc * 128:(kc + 1) * 128], self.ident),
                              reads=[an, "consts"], writes=[pn])
                    kb.op("act", lambda e, ps=ps, o=o, h=h: e.copy(o[:, h * 4:(h + 1) * 4, :], ps[:].rearrange("p (j t) -> p j t", j=4)),
                          reads=[pn], writes=[on])
                kb.dma("sp", dstT.rearrange("(kc p) t -> p kc t", p=128)[:, :, t * 128:(t + 1) * 128], o[:], reads=[on], writes=[("dram", id(dstT))])
            kb.barrier()

    def mod_vectors(self, i, st):
        kb = self.kb
        cc = kb.sb("mod_cc", [128, 8, 2], F32, st)
        with kb.nc.allow_non_contiguous_dma(reason="small"):
            kb.dma("sp", cc[:, :, 0], self.c.rearrange("(c p) -> p c", p=128), writes=["mod_cc"])
            kb.dma("sp", cc[:, :, 1], self.c_ctx.rearrange("(c p) -> p c", p=128), writes=["mod_cc"])
        sc = kb.sb("mod_sc", [128, 8, 2], F32, st)
        kb.op("act", lambda e: e.activation(out=sc[:], in_=cc[:], func=AF.Silu), reads=["mod_cc"], writes=["mod_sc"])
        bm = self.colvec("mod_b", self.b_mod[i], 48, st)
        n1 = self.colvec("mod_n1", self.norm1_g[i], 8, st)
        n2 = self.colvec("mod_n2", self.norm2_g[i], 8, st)
        mv = kb.sb("mod_mv", [128, 48, 2], F32, st)
        wst = kb.sb("mod_w", [128, 8, 512], F32, st)
        wsrc = self.w_mod[i].rearrange("(kc p) n -> p kc n", p=128)
        for blk in range(12):
            kb.dma("sp", wst[:], wsrc[:, :, blk * 512:(blk + 1) * 512], writes=["mod_w"])
            for j in range(4):
                col = blk * 4 + j
                for kc in range(8):
                    kb.op("pe", lambda e, j=j, kc=kc: e.matmul(self.pM[:, 0:2], wst[:, kc, j * 128:(j + 1) * 128], sc[:, kc, :], start=(kc == 0), stop=(kc == 7)),
                          reads=["mod_w", "mod_sc"], writes=["pM"])
                kb.op("dve", lambda e, col=col: e.tensor_tensor(mv[:, col, :], self.pM[:, 0:2], bc(bm[:, col:col + 1], [128, 2]), ALU.add),
                      reads=["pM", "mod_b"], writes=["mod_mv"])
        res = self.modres[i]
        for who in range(2):
            for half, nrm in ((0, n1), (1, n2)):
                b0 = half * 3
                kb.op("dve", lambda e, who=who, b0=b0: e.tensor_copy(res[:, who, b0 + 1, :], mv[:, b0 * 8:(b0 + 1) * 8, who]), reads=["mod_mv"], writes=[f"modres{i}"])
                kb.op("dve", lambda e, who=who, b0=b0, nrm=nrm: e.scalar_tensor_tensor(res[:, who, b0, :], mv[:, (b0 + 1) * 8:(b0 + 2) * 8, who], 1.0, nrm[:], ALU.add, ALU.mult),
                      reads=["mod_mv", "mod_n1", "mod_n2"], writes=[f"modres{i}"])
                kb.op("dve", lambda e, who=who, b0=b0: e.tensor_copy(res[:, who, b0 + 2, :], mv[:, (b0 + 2) * 8:(b0 + 3) * 8, who]), reads=["mod_mv"], writes=[f"modres{i}"])
        return res, f"modres{i}"

    def make_hT(self, srcT, T, t0, ncols, lead, res, resn, who, vec0, hT, hTn, tmp):
        kb = self.kb
        xw, xwn, sq, sqn, rs, rsn = tmp
        lo = t0 - lead
        hi = lo + ncols
        clo, chi = max(lo, 0), min(hi, T)
        j0, j1 = clo - lo, chi - lo
        src = srcT.rearrange("(kc p) t -> p kc t", p=128)
        kb.dma("sp", xw[:, :, j0:j1], src[:, :, clo:chi], reads=[("dram", id(srcT))], writes=[xwn])
        sqk = [f"{sqn}{kc}" for kc in range(8)]
        kb.op("act", lambda e: e.activation(out=sq[:, :, j0:j1], in_=xw[:, :, j0:j1], func=AF.Square), reads=[xwn], writes=sqk)
        for kc in range(8):
            kb.op("pe", lambda e, kc=kc: e.matmul(self.pM[:, j0:j1], self.ones, sq[:, kc, j0:j1], start=(kc == 0), stop=(kc == 7)),
                  reads=[sqk[kc], "consts"], writes=["pM"])
        kb.op("act", lambda e: e.activation(out=rs[:, j0:j1], in_=self.pM[:, j0:j1], func=AF.Sqrt, bias=self.eps_c, scale=1.0 / D),
              reads=["pM", "cst"], writes=[rsn])
        kb.op("dve", lambda e: e.reciprocal(rs[:, j0:j1], rs[:, j0:j1]), reads=[rsn], writes=[rsn])
        for kc in range(8):
            kb.op("dve", lambda e, kc=kc: e.tensor_tensor(sq[:, kc, j0:j1], xw[:, kc, j0:j1], rs[:, j0:j1], ALU.mult), reads=[xwn, rsn], writes=[sqk[kc]])
        for kc in range(8):
            kb.op("act", lambda e, kc=kc: e.activation(out=hT[:, kc, j0:j1], in_=sq[:, kc, j0:j1], func=AF.Identity,
                                                        bias=res[:, who, vec0 + 1, kc:kc + 1], scale=res[:, who, vec0, kc:kc + 1]),
                  reads=[sqk[kc], resn], writes=[hTn])
        if j0 > 0:
            kb.op("dve", lambda e: e.memset(hT[:, :, 0:j0], 0.0), writes=[hTn])
        if j1 < ncols:
            kb.op("dve", lambda e: e.memset(hT[:, :, j1:ncols], 0.0), writes=[hTn])

    def mixer_pass(self, i, d, srcT, dstT, T, who, res, resn, S, Sn, grid, lp, write_out):
        kb = self.kb
        st = contextlib.ExitStack()
        w_in = self.w_in[i]
        ntile = T // TILE
        self.uid = getattr(self, "uid", 0)

        def sbt(n, s, dt=F32, stack=None):
            self.uid += 1
            return kb.sb(f"{n}_{self.uid}", s, dt, stack or st)
        nset = 2
        hsets = []
        for k in range(nset):
            hsets.append((sbt(f"mx_xw{k}", [128, 8, WIN]), sbt(f"mx_sq{k}", [128, 8, WIN]), sbt(f"mx_rs{k}", [128, WIN]), sbt(f"mx_hT{k}", [128, 8, WIN], BF16)))

        def hset(k):
            xw_, sq_, rs_, hT_ = hsets[k]
            return xw_, f"mx_xw{k}", sq_, rs_, hT_, f"mx_hT{k}", (xw_, f"mx_xw{k}", sq_, f"mx_sq{k}", rs_, f"mx_rs{k}")
        Sbf = sbt("mx_Sbf", [128, INNER], BF16)
        kb.op("act", lambda e: e.copy(Sbf[:], S[:]), reads=[f"{Sn}{g}" for g in range(NG)], writes=[f"mx_Sbf{g}" for g in range(NG)])
        self.rot = self.rot6
        self.set_wbufs(4 if d == 1 else 3, st)
        dtb = sbt("mx_dtb", [64, 1]); alg = sbt("mx_alg", [64, 1]); aneg = sbt("mx_aneg", [64, 1])
        with kb.nc.allow_non_contiguous_dma(reason="small"):
            kb.dma("sp", dtb[:], self.ssd_dt_bias[i].rearrange("a (h o) -> (a h) o", o=1), writes=["mx_dtb"])
            kb.dma("sp", alg[:], self.ssd_a_log[i].rearrange("a (h o) -> (a h) o", o=1), writes=["mx_alg"])
        kb.op("act", lambda e: e.activation(out=aneg[:], in_=alg[:], func=AF.Exp), reads=["mx_alg"], writes=["mx_aneg"])
        kb.op("dve", lambda e: e.tensor_scalar(aneg[:], aneg[:], -1.0, None, ALU.mult), reads=["mx_aneg"], writes=["mx_aneg"])
        cw = sbt("mx_cw", [128, 3, 24]); cb = self.colvec("mx_cb", self.ssd_conv_b[i], 24, st)
        with kb.nc.allow_non_contiguous_dma(reason="small"):
            for j in range(3):
                kb.dma("sp", cw[:, j, :], self.ssd_conv_w[i, j].rearrange("(c p) -> p c", p=128), writes=["mx_cw"])
        tri = self.triB if d == 1 else self.triF
        if d == 0:
            Dbc = sbt("mx_Dbc", [128, NH])
            kb.dma("sp", Dbc[:], self.ssd_d[i].partition_broadcast(128), writes=["mx_Dbc"])
            gbc = self.rowbc("mx_gbc", self.ssd_norm_g[i], INNER, st)
            Did = sbt("mx_Did", [128, NH, 128], BF16)
            kb.op("dve", lambda e: e.tensor_tensor(Did[:], bc(self.ident.unsqueeze(1), [128, NH, 128]), bc(Dbc[:].unsqueeze(2), [128, NH, 128]), ALU.mult),
                  reads=["consts", "mx_Dbc"], writes=["mx_Did"])
            ynT = sbt("mx_ynT", [128, 16, TILE], BF16)
            bgate = self.colvec("mx_bg", self.b_gate[i], 16, st)
            scw = sbt("mx_scw", [128, 3, 8])
            with kb.nc.allow_non_contiguous_dma(reason="small"):
                for j in range(3):
                    kb.dma("sp", scw[:, j, :], self.sc_conv_w[i, j].rearrange("(c p) -> p c", p=128), writes=["mx_scw"])

        tiles = list(range(ntile))
        if d == 1:
            tiles = tiles[::-1]
        for tix, ti in enumerate(tiles):
            t0 = ti * TILE
            if d == 0 or tix == 0:
                sA = contextlib.ExitStack()
                xbcT = sbt("mx_xbcT", [128, 24, TILE], BF16, sA)
                cvt = [sbt(f"mx_cvt{k}", [128, TILE], F32, sA) for k in range(4)] if d == 1 else None
                dtT = sbt("mx_dtT", [64, TILE], F32, sA)
                dAT = sbt("mx_dAT", [64, TILE], F32, sA)
                dtA_tm = [sbt(f"mx_dtA_tm{k}", [128, 128], F32, sA) for k in range(2)]
                dt_tm = [t[:, 0:64] for t in dtA_tm]
                cstot = [sbt(f"mx_cstot{k}", [128, 2 * NH], F32, sA) for k in range(2)]
                ecd = [sbt(f"mx_ecd{k}", [128, 2 * NH], F32, sA) for k in range(2)]
                cs_sb = [t[:, 0:NH] for t in cstot]
                ecs_sb = [t[:, 0:NH] for t in ecd]
                cd_sb = [t[:, NH:2 * NH] for t in ecd]
                w_sb = [sbt(f"mx_w{k}", [128, NH], F32, sA) for k in range(2)]
                csT_sb = [sbt(f"mx_csT{k}", [NH, 128], F32, sA) for k in range(2)]
                xs_tm = [sbt(f"mx_xs{k}", [128, INNER], BF16, sA) for k in range(2)]
                B_tm = [sbt(f"mx_B{k}", [128, 512], BF16, sA) for k in range(2)]
                xdt = sbt("mx_xdt", [128, INNER], BF16, sA)
                xwt = sbt("mx_xwt", [128, INNER], BF16, sA)
                scm = sbt("mx_scm", [128, 4, 128], F32, sA)
                csb = [sbt(f"mx_csb{k}", [128, 8, 128], F32, sA) for k in range(4)]
                MT = [sbt(f"mx_MT{k}", [128, 8, 128], BF16, sA) for k in range(4)]
                ytmp = [sbt(f"mx_ytmp{k}", [128, 512], F32, sA) for k in range(4)]
                Yc = [sbt(f"mx_Yc{k}", [128, INNER], F32, sA) for k in range(2)]
                if d == 0:
                    zs = [sbt(f"mx_zs{k}", [128, 512], F32, sA) for k in range(2)]
                    ss = sbt("mx_ss", [128, 2], F32, sA)
                    yn_tm = sbt("mx_yn", [128, INNER], BF16, sA)
            xw, xwn, sq, rs, hT, hTn, tmp = hset(tix % nset)
            if tix == 0:
                self.make_hT(srcT, T, t0, WIN, 1, res, resn, who, 0, hT, hTn, tmp)
            if d == 0:
                kb.dma("sp", xbcT[:].rearrange("p a b -> p (a b)"), self.XB[ti], reads=[("XB", ti)], writes=["mx_xbcT"])
                for c in (0, 1):
                    cg = ti * 2 + c
                    kb.dma("sp", xs_tm[c][:], self.XS[cg], reads=[("XS", cg)], writes=[f"mx_xs{c}"])
                    kb.dma("sp", B_tm[c][:], self.BS[cg], reads=[("BS", cg)], writes=[f"mx_B{c}"])
            wb, wn = self.load_w(w_in, C_DT, 64, key=("in", i, C_DT))
            for kc in range(8):
                kb.op("pe", lambda e, wb=wb, kc=kc: e.matmul(self.pM[0:64, 0:WIN], wb[:, kc, 0:64], hT[:, kc, :], start=(kc == 0), stop=(kc == 7)),
                      reads=[wn, hTn], writes=["pM"])
            kb.op("act", lambda e: e.activation(out=dtT[:], in_=self.pM[0:64, 1:1 + TILE], func=AF.Exp, bias=dtb[:, 0:1]), reads=["pM", "mx_dtb"], writes=["mx_dtT"])
            kb.op("act", lambda e: e.activation(out=dtT[:], in_=dtT[:], func=AF.Ln, bias=self.one_c[0:64, :]), reads=["mx_dtT", "cst"], writes=["mx_dtT"])
            kb.op("dve", lambda e: e.tensor_scalar(dAT[:], dtT[:], aneg[:, 0:1], None, ALU.mult), reads=["mx_dtT", "mx_aneg"], writes=["mx_dAT"])
            chunks = [0, 1] if d == 0 else [1, 0]
            cslot = {}
            for c in chunks:
                cl = slice(c * 128, (c + 1) * 128)
                dsl = slice(d * NH, (d + 1) * NH)
                b1, b1n = self.next_pab()
                kb.op("pe", lambda e: e.transpose(b1[:, 0:64], dtT[:, cl], self.ident[0:64, 0:64]), reads=["mx_dtT", "consts"], writes=[b1n])
                kb.op("pe", lambda e: e.transpose(b1[:, 64:128], dAT[:, cl], self.ident[0:64, 0:64]), reads=["mx_dAT", "consts"], writes=[b1n])
                kb.op("act", lambda e: e.copy(dtA_tm[c][:], b1[:, 0:128]), reads=[b1n], writes=[f"mx_dt_tm{c}"])
                dt_c = dtA_tm[c][:, 0:64]
                dA_c = dtA_tm[c][:, 64:128]
                b2, b2n = self.next_pab()
                kb.op("pe", lambda e: e.matmul(b2[:, 0:NH], tri, dA_c[:, dsl], start=True, stop=True), reads=[f"mx_dt_tm{c}", "consts"], writes=[b2n])
                kb.op("pe", lambda e: e.matmul(b2[:, NH:2 * NH], self.ones, dA_c[:, dsl], start=True, stop=True), reads=[f"mx_dt_tm{c}", "consts"], writes=[b2n])
                kb.op("pe", lambda e: e.matmul(b2[0:NH, 128:256], dA_c[:, dsl], tri, start=True, stop=True), reads=[f"mx_dt_tm{c}", "consts"], writes=[b2n])
                kb.op("act", lambda e: e.copy(cstot[c][:], b2[:, 0:2 * NH]), reads=[b2n], writes=[f"mx_cs{c}"])
                kb.op("act", lambda e: e.copy(csT_sb[c][:], b2[0:NH, 128:256]), reads=[b2n], writes=[f"mx_csT{c}"])
                cslot[c] = self.csrr % self.NCS
                self.csrr += 1
                kb.dma("sp", self.csrow[cslot[c]].rearrange("(h l) -> h l", l=128), csT_sb[c][:], reads=[f"mx_csT{c}"], writes=[f"csrow{cslot[c]}"])
                kb.op("act", lambda e: e.activation(out=ecd[c][:], in_=cstot[c][:], func=AF.Exp), reads=[f"mx_cs{c}"], writes=[f"mx_ecs{c}"])
                kb.op("dve", lambda e: e.tensor_tensor(w_sb[c][:], cstot[c][:, NH:2 * NH], cstot[c][:, 0:NH], ALU.subtract), reads=[f"mx_cs{c}"], writes=[f"mx_w{c}"])
                kb.op("act", lambda e: e.activation(out=w_sb[c][:], in_=w_sb[c][:], func=AF.Exp), reads=[f"mx_w{c}"], writes=[f"mx_w{c}"])
                kb.op("dve", lambda e: e.tensor_tensor(w_sb[c][:], w_sb[c][:], dt_c[:, dsl], ALU.mult), reads=[f"mx_w{c}", f"mx_dt_tm{c}"], writes=[f"mx_w{c}"])
            if d == 1:
                pend = None
                for blk in range(6):
                    wb, wn = self.load_w(w_in, C_XBC + blk * 512, 512, key=("in", i, C_XBC + blk * 512))
                    for j in range(4):
                        cc = blk * 4 + j
                        ps, pn = self.next_pab()
                        for kc in range(8):
                            kb.op("pe", lambda e, ps=ps, wb=wb, j=j, kc=kc: e.matmul(ps[:, 0:WIN], wb[:, kc, j * 128:(j + 1) * 128], hT[:, kc, :], start=(kc == 0), stop=(kc == 7)),
                                  reads=[wn, hTn], writes=[pn])
                        cv = cvt[cc % 4]; cvn = f"mx_cvt{cc % 4}"
                        kb.op("act", lambda e, ps=ps, cv=cv, cc=cc: e.activation(out=cv[:], in_=ps[:, 1:1 + TILE], func=AF.Identity, bias=cb[:, cc:cc + 1], scale=cw[:, 1, cc:cc + 1]),
                              reads=[pn, "mx_cb", "mx_cw"], writes=[cvn])
                        kb.op("dve", lambda e, ps=ps, cv=cv, cc=cc: e.scalar_tensor_tensor(cv[:], ps[:, 0:TILE], cw[:, 0, cc:cc + 1], cv[:], ALU.mult, ALU.add),
                              reads=[pn, "mx_cw", cvn], writes=[cvn])
                        kb.op("dve", lambda e, ps=ps, cv=cv, cc=cc: e.scalar_tensor_tensor(cv[:], ps[:, 2:2 + TILE], cw[:, 2, cc:cc + 1], cv[:], ALU.mult, ALU.add),
                              reads=[pn, "mx_cw", cvn], writes=[cvn])
                        if pend is not None:
                            pend()

                        def pend(cv=cv, cc=cc, cvn=cvn):
                            kb.op("act", lambda e: e.activation(out=xbcT[:, cc, :], in_=cv[:], func=AF.Silu), reads=[cvn], writes=["mx_xbcT"])
                pend()
                kb.dma("sp", self.XB[ti], xbcT[:].rearrange("p a b -> p (a b)"), reads=["mx_xbcT"], writes=[("XB", ti)])
                if tix + 1 < len(tiles):
                    nx = hset((tix + 1) % nset)
                    self.make_hT(srcT, T, tiles[tix + 1] * TILE, WIN, 1, res, resn, who, 0, nx[4], nx[5], nx[6])
                for c in chunks:
                    cl = slice(c * 128, (c + 1) * 128)
                    cg = ti * 2 + c
                    for half in range(2):
                        bt, btn = self.next_pab()
                        btb = bt[:].bitcast(BF16)
                        for j in range(8):
                            cc = half * 8 + j
                            kb.op("pe", lambda e, cc=cc, j=j: e.transpose(btb[:, j * 128:(j + 1) * 128], xbcT[:, cc, cl], self.identb[:]),
                                  reads=["mx_xbcT", "identb"], writes=[btn])
                        kb.op("act", lambda e: e.copy(xs_tm[c][:, half * 1024:(half + 1) * 1024], btb), reads=[btn], writes=[f"mx_xs{c}"])
                    bt, btn = self.next_pab()
                    btb = bt[:].bitcast(BF16)
                    for j in range(4):
                        kb.op("pe", lambda e, j=j: e.transpose(btb[:, j * 128:(j + 1) * 128], xbcT[:, 16 + j, cl], self.identb[:]),
                              reads=["mx_xbcT", "identb"], writes=[btn])
                    kb.op("act", lambda e: e.copy(B_tm[c][:], btb[:, 0:512]), reads=[btn], writes=[f"mx_B{c}"])
                    kb.dma("sp", self.XS[cg], xs_tm[c][:], reads=[f"mx_xs{c}"], writes=[("XS", cg)])
                    kb.dma("sp", self.BS[cg], B_tm[c][:], reads=[f"mx_B{c}"], writes=[("BS", cg)])
            if d == 0 and tix + 1 < len(tiles):
                nx = hset((tix + 1) % nset)
                self.make_hT(srcT, T, tiles[tix + 1] * TILE, WIN, 1, res, resn, who, 0, nx[4], nx[5], nx[6])
            for c in chunks:
                cl = slice(c * 128, (c + 1) * 128)
                tok0 = t0 + c * 128
                dsl = slice(d * NH, (d + 1) * NH)
                slot = cslot[c]
                kb.op("dve", lambda e, c=c, dsl=dsl: e.tensor_tensor(xdt[:].rearrange("p (h q) -> p h q", q=64), xs_tm[c][:].rearrange("p (h q) -> p h q", q=64),
                                                                     bc(dt_tm[c][:, dsl].unsqueeze(2), [128, NH, 64]), ALU.mult),
                      reads=[f"mx_xs{c}", f"mx_dt_tm{c}"], writes=["mx_xdt"])
                kb.op("pool", lambda e, c=c: e.tensor_tensor(xwt[:].rearrange("p (h q) -> p h q", q=64), xs_tm[c][:].rearrange("p (h q) -> p h q", q=64),
                                                             bc(w_sb[c][:].unsqueeze(2), [128, NH, 64]), ALU.mult),
                      reads=[f"mx_xs{c}", f"mx_w{c}"], writes=["mx_xwt"])
                for g in range(NG):
                    kb.op("pe", lambda e, g=g, cl=cl: e.matmul(self.pS[:, g * 128:(g + 1) * 128], xbcT[:, 16 + g, cl], xbcT[:, 20 + g, cl], start=True, stop=True),
                          reads=["mx_xbcT"], writes=["pS"])
                kb.op("dve", lambda e: e.tensor_tensor(scm[:], self.pS[:].rearrange("p (g l) -> p g l", g=4), bc(tri.unsqueeze(1), [128, 4, 128]), ALU.mult),
                      reads=["pS", "consts"], writes=["mx_scm"])
                Y = Yc[c]; Yn = f"mx_Yc{c}"
                Yg = [f"{Yn}g{g}" for g in range(NG)]
                if d == 0:
                    kb.dma("sp", Y[:], self.YB[tok0:tok0 + 128, :], reads=[("YB", tok0 // 128)], writes=Yg)
                for g in range(NG):
                    kb.dma("sp", csb[g][:].rearrange("p h l -> p (h l)"), self.csrow[slot, g * 1024:(g + 1) * 1024].partition_broadcast(128),
                           reads=[f"csrow{slot}"], writes=[f"mx_csb{g}"])
                for g in range(NG):
                    kb.op("dve" if g < 2 else "pool", lambda e, c=c, g=g: e.tensor_tensor(csb[g][:], csb[g][:], bc(cs_sb[c][:, g * 8:(g + 1) * 8].unsqueeze(2), [128, 8, 128]), ALU.subtract),
                          reads=[f"mx_csb{g}", f"mx_cs{c}"], writes=[f"mx_csb{g}"])
                for g in range(NG):
                    kb.op("act", lambda e, g=g: e.activation(out=csb[g][:], in_=csb[g][:], func=AF.Exp), reads=[f"mx_csb{g}"], writes=[f"mx_csb{g}"])
                for g in range(NG):
                    gs_ = slice(g * 512, (g + 1) * 512)
                    kb.op("pool", lambda e, g=g, gs_=gs_: e.tensor_tensor(S[:, gs_].rearrange("p (h q) -> p h q", q=64), S[:, gs_].rearrange("p (h q) -> p h q", q=64),
                                                                     bc(cd_sb[c][:, g * 8:(g + 1) * 8].unsqueeze(2), [128, 8, 64]), ALU.mult),
                          reads=[f"{Sn}{g}", f"mx_ecs{c}"], writes=[f"{Sn}{g}"])
                for g in range(NG):
                    kb.op("dve", lambda e, g=g: e.scalar_tensor_tensor(MT[g][:], csb[g][:], 1.0, bc(scm[:, g, :].unsqueeze(1), [128, 8, 128]), ALU.min, ALU.mult),
                          reads=[f"mx_csb{g}", "mx_scm"], writes=[f"mx_MT{g}"])

                def s2(g):
                    yd, ydn = self.next_pab(); yo, yon = self.next_pab(); stb, stn = self.next_pab()
                    gs = slice(g * 512, (g + 1) * 512)
                    for h in range(8):
                        hh = g * 8 + h
                        kb.op("pe", lambda e, g=g, h=h, hh=hh, yd=yd: e.matmul(yd[:, h * 64:(h + 1) * 64], MT[g][:, h, :], xdt[:, hh * 64:(hh + 1) * 64], start=True, stop=(d == 1)),
                              reads=[f"mx_MT{g}", "mx_xdt"], writes=[ydn])
                        if d == 0:
                            kb.op("pe", lambda e, h=h, hh=hh, yd=yd: e.matmul(yd[:, h * 64:(h + 1) * 64], Did[:, hh, :], xs_tm[c][:, hh * 64:(hh + 1) * 64], start=False, stop=True),
                                  reads=["mx_Did", f"mx_xs{c}"], writes=[ydn])
                    kb.op("pe", lambda e, g=g, yo=yo, gs=gs: e.matmul(yo[:], xbcT[:, 20 + g, cl], Sbf[:, gs], start=True, stop=True),
                          reads=["mx_xbcT", f"mx_Sbf{g}"], writes=[yon])
                    kb.op("pe", lambda e, g=g, stb=stb, gs=gs: e.matmul(stb[:], B_tm[c][:, g * 128:(g + 1) * 128], xwt[:, gs], start=True, stop=True),
                          reads=[f"mx_B{c}", "mx_xwt"], writes=[stn])
                    return (yd, ydn, yo, yon, stb, stn)

                def s3(g, bk):
                    yd, ydn, yo, yon, stb, stn = bk
                    gs = slice(g * 512, (g + 1) * 512)
                    yt = ytmp[g]; ytn = f"mx_ytmp{g}"
                    kb.op("dve", lambda e: e.tensor_tensor(yt[:].rearrange("p (h q) -> p h q", q=64), yo[:].rearrange("p (h q) -> p h q", q=64),
                                                           bc(ecs_sb[c][:, g * 8:(g + 1) * 8].unsqueeze(2), [128, 8, 64]), ALU.mult),
                          reads=[yon, f"mx_ecs{c}"], writes=[ytn])
                    if d == 1:
                        kb.op("dve", lambda e: e.tensor_tensor(Y[:, gs], yd[:], yt[:], ALU.add), reads=[ydn, ytn], writes=[Yg[g]])
                    else:
                        kb.op("dve", lambda e: e.tensor_tensor(yt[:], yd[:], yt[:], ALU.add), reads=[ydn, ytn], writes=[ytn])
                        kb.op("pool", lambda e: e.tensor_tensor(Y[:, gs], Y[:, gs], yt[:], ALU.add), reads=[Yg[g], ytn], writes=[Yg[g]])
                    kb.op("dve", lambda e: e.tensor_tensor(S[:, gs], S[:, gs], stb[:], ALU.add), reads=[f"{Sn}{g}", stn], writes=[f"{Sn}{g}"])
                    kb.op("act", lambda e: e.copy(Sbf[:, gs], S[:, gs]), reads=[f"{Sn}{g}"], writes=[f"mx_Sbf{g}"])

                bks = {}
                bks[0] = s2(0)
                bks[1] = s2(1)
                s3(0, bks[0])
                bks[2] = s2(2)
                s3(1, bks[1])
                bks[3] = s2(3)
                s3(2, bks[2])
                s3(3, bks[3])
                if d == 1:
                    kb.dma("sp", self.YB[tok0:tok0 + 128, :], Y[:], reads=Yg, writes=[("YB", tok0 // 128)])
            if d == 1:
                if tix == len(tiles) - 1:
                    kb.barrier()
                    sA.close()
                continue
            for blk in range(4):
                wb, wn = self.load_w(w_in, C_Z + blk * 512, 512, key=("in", i, C_Z + blk * 512))
                for c in chunks:
                    ps, pn = self.next_pab()
                    for kc in range(8):
                        kb.op("pe", lambda e, ps=ps, wb=wb, kc=kc, c=c: e.matmul(ps[:], hT[:, kc, 1 + c * 128:1 + (c + 1) * 128], wb[:, kc, :], start=(kc == 0), stop=(kc == 7)),
                              reads=[wn, hTn], writes=[pn])
                    zk = (blk * 2 + c) % 2
                    kb.op("act", lambda e, ps=ps, zk=zk: e.activation(out=zs[zk][:], in_=ps[:], func=AF.Silu), reads=[pn], writes=[f"mx_zs{zk}"])
                    bs = slice(blk * 512, (blk + 1) * 512)
                    kb.op("dve", lambda e, c=c, bs=bs, zk=zk: e.tensor_tensor(Yc[c][:, bs], Yc[c][:, bs], zs[zk][:], ALU.mult), reads=[f"mx_Yc{c}g{blk}", f"mx_zs{zk}"], writes=[f"mx_Yc{c}g{blk}"])
            for c in chunks:
                Y = Yc[c]; Yn = f"mx_Yc{c}"
                Yg = [f"{Yn}g{g}" for g in range(NG)]
                kb.op("act", lambda e, Y=Y: e.activation(out=yn_tm[:], in_=Y[:], func=AF.Square, accum_out=ss[:, 0:1]), reads=Yg, writes=["mx_yn", "mx_ss"])
                kb.op("act", lambda e: e.activation(out=ss[:, 1:2], in_=ss[:, 0:1], func=AF.Sqrt, bias=self.eps_c, scale=1.0 / INNER), reads=["mx_ss", "cst"], writes=["mx_ss"])
                kb.op("dve", lambda e: e.reciprocal(ss[:, 1:2], ss[:, 1:2]), reads=["mx_ss"], writes=["mx_ss"])
                kb.op("dve", lambda e, Y=Y: e.scalar_tensor_tensor(yn_tm[:], Y[:], ss[:, 1:2], gbc[:], ALU.mult, ALU.mult), reads=Yg + ["mx_ss", "mx_gbc"], writes=["mx_yn"])
                for half in range(2):
                    for j in range(8):
                        kc = half * 8 + j
                        kb.op("pe", lambda e, j=j, kc=kc: e.transpose(self.pTb[:, j * 128:(j + 1) * 128], yn_tm[:, kc * 128:(kc + 1) * 128], self.identb[:]),
                              reads=["mx_yn", "identb"], writes=["pT"])
                    kb.op("act", lambda e, c=c, half=half: e.copy(ynT[:, half * 8:(half + 1) * 8, c * 128:(c + 1) * 128], self.pTb[:].rearrange("p (j t) -> p j t", j=8)),
                          reads=["pT"], writes=["mx_ynT"])
            kb.barrier()
            sA.close()
            sC = contextlib.ExitStack()
            gbs = sbt("mx_gbs", [128, 8, TILE], F32, sC); gcs = sbt("mx_gcs", [128, 8, TILE], F32, sC); uu = sbt("mx_u", [128, 8, TILE], F32, sC)
            vv = gcs
            svT = sbt("mx_svT", [128, 8, TILE], BF16, sC)
            gT = sbt("mx_gT", [128, 16, TILE], F32, sC)
            t1s = [sbt(f"mx_t1{k}", [128, TILE], F32, sC) for k in range(2)]; t2s = [sbt(f"mx_t2{k}", [128, TILE], F32, sC) for k in range(2)]
            mT = sbt("mx_mT", [128, 8, TILE], BF16, sC)
            xot = sbt("mx_xo", [128, 8, TILE], F32, sC); xon = "mx_xo"
            for blk in range(6):
                wb, wn = self.load_w(w_in, C_SC + blk * 512, 512, key=("in", i, C_SC + blk * 512))
                for j in range(4):
                    cc = blk * 4 + j
                    kind, f = cc // 8, cc % 8
                    ps, pn = self.next_pab()
                    for kc in range(8):
                        kb.op("pe", lambda e, ps=ps, wb=wb, j=j, kc=kc: e.matmul(ps[:, 0:TILE], wb[:, kc, j * 128:(j + 1) * 128], hT[:, kc, 1:1 + TILE], start=(kc == 0), stop=(kc == 7)),
                              reads=[wn, hTn], writes=[pn])
                    if kind == 0:
                        kb.op("act", lambda e, ps=ps, f=f: e.copy(gbs[:, f, :], ps[:, 0:TILE]), reads=[pn], writes=["mx_gbs"])
                    elif kind == 1:
                        kb.op("act", lambda e, ps=ps, f=f: e.copy(gcs[:, f, :], ps[:, 0:TILE]), reads=[pn], writes=["mx_gcs"])
                    else:
                        kb.op("dve", lambda e, ps=ps, f=f: e.tensor_tensor(uu[:, f, :], gcs[:, f, :], ps[:, 0:TILE], ALU.mult), reads=[pn, "mx_gcs"], writes=["mx_u"])
            rows = TILE // grid
            for f in range(8):
                u3 = uu[:, f, :].rearrange("p (r w) -> p r w", w=grid)
                v3 = vv[:, f, :].rearrange("p (r w) -> p r w", w=grid)
                kb.op("act", lambda e, f=f: e.activation(out=vv[:, f, :], in_=uu[:, f, :], func=AF.Identity, scale=scw[:, 1, f:f + 1]), reads=["mx_u", "mx_scw"], writes=["mx_gcs"])
                kb.op("dve", lambda e, f=f, u3=u3, v3=v3: e.scalar_tensor_tensor(v3[:, :, 1:grid], u3[:, :, 0:grid - 1], scw[:, 0, f:f + 1], v3[:, :, 1:grid], ALU.mult, ALU.add),
                      reads=["mx_u", "mx_scw", "mx_gcs"], writes=["mx_gcs"])
                kb.op("dve", lambda e, f=f, u3=u3, v3=v3: e.scalar_tensor_tensor(v3[:, :, 0:grid - 1], u3[:, :, 1:grid], scw[:, 2, f:f + 1], v3[:, :, 0:grid - 1], ALU.mult, ALU.add),
                      reads=["mx_u", "mx_scw", "mx_gcs"], writes=["mx_gcs"])
                kb.op("pool", lambda e, f=f: e.tensor_tensor(svT[:, f, :], gbs[:, f, :], vv[:, f, :], ALU.mult), reads=["mx_gbs", "mx_gcs"], writes=["mx_svT"])
            for blk in range(4):
                wb, wn = self.load_w(w_in, C_GL + blk * 512, 512, key=("in", i, C_GL + blk * 512))
                for j in range(4):
                    cc = blk * 4 + j
                    ps, pn = self.next_pab()
                    for kc in range(8):
                        kb.op("pe", lambda e, ps=ps, wb=wb, j=j, kc=kc: e.matmul(ps[:, 0:TILE], wb[:, kc, j * 128:(j + 1) * 128], hT[:, kc, 1:1 + TILE], start=(kc == 0), stop=(kc == 7)),
                              reads=[wn, hTn], writes=[pn])
                    kb.op("act", lambda e, ps=ps, cc=cc: e.activation(out=gT[:, cc, :], in_=ps[:, 0:TILE], func=AF.Sigmoid, bias=bgate[:, cc:cc + 1]), reads=[pn, "mx_bg"], writes=["mx_gT"])
            for ob in range(4):
                wso, wson = self.load_w(self.w_ssd_out[i], ob * 256, 256, 0, 16, key=("so", i, ob))
                wsc, wscn = self.load_w(self.w_sc_out[i], ob * 256, 256, 0, 8, key=("sc", i, ob))
                for j in range(2):
                    fo = ob * 2 + j
                    for kc in range(16):
                        kb.op("pe", lambda e, wso=wso, j=j, kc=kc: e.matmul(self.pA[:, 0:TILE], wso[:, kc, j * 128:(j + 1) * 128], ynT[:, kc, :], start=(kc == 0), stop=(kc == 15)),
                              reads=[wson, "mx_ynT"], writes=["pA"])
                    for kc in range(8):
                        kb.op("pe", lambda e, wsc=wsc, j=j, kc=kc: e.matmul(self.pB[:, 0:TILE], wsc[:, kc, j * 128:(j + 1) * 128], svT[:, kc, :], start=(kc == 0), stop=(kc == 7)),
                              reads=[wscn, "mx_svT"], writes=["pB"])
                    t1 = t1s[fo % 2]; t2 = t2s[fo % 2]
                    kb.op("dve", lambda e, fo=fo: e.tensor_tensor(t1[:], gT[:, fo, :], self.pA[:, 0:TILE], ALU.mult), reads=["mx_gT", "pA"], writes=[f"mx_t1{fo % 2}"])
                    kb.op("dve", lambda e, fo=fo: e.tensor_tensor(t2[:], gT[:, 8 + fo, :], self.pB[:, 0:TILE], ALU.mult), reads=["mx_gT", "pB"], writes=[f"mx_t2{fo % 2}"])
                    kb.op("pool", lambda e, fo=fo: e.tensor_tensor(mT[:, fo, :], t1[:], t2[:], ALU.add), reads=[f"mx_t1{fo % 2}", f"mx_t2{fo % 2}"], writes=["mx_mT"])
            for ob in range(2):
                wo, won = self.load_w(self.w_o[i], ob * 512, 512, 0, 8, key=("wo", i, ob))
                for j in range(4):
                    fo = ob * 4 + j
                    ps, pn = self.next_pab()
                    for kc in range(8):
                        kb.op("pe", lambda e, ps=ps, wo=wo, j=j, kc=kc: e.matmul(ps[:, 0:TILE], wo[:, kc, j * 128:(j + 1) * 128], mT[:, kc, :], start=(kc == 0), stop=(kc == 7)),
                              reads=[won, "mx_mT"], writes=[pn])
                    kb.op("dve", lambda e, ps=ps, fo=fo, xot=xot: e.scalar_tensor_tensor(xot[:, fo, :], ps[:, 0:TILE], res[:, who, 2, fo:fo + 1], xw[:, fo, 1:1 + TILE], ALU.mult, ALU.add),
                          reads=[pn, resn, xwn], writes=[xon])
            if write_out:
                kb.dma("sp", dstT.rearrange("(kc p) t -> p kc t", p=128)[:, :, t0:t0 + TILE], xot[:], reads=[xon], writes=[("dram", id(dstT))])
            kb.barrier()
            sC.close()
        kb.barrier()
        st.close()

    def ffn(self, i, srcT, dstT, T, who, res, resn, moe):
        kb = self.kb
        st = contextlib.ExitStack()
        sbt = lambda n, s, dt=F32: kb.sb(n, s, dt, st)
        TS = min(T, 1024)
        self.rot = self.rot2
        self.set_wbufs(6, st)
        NE = NEXP if moe else 1
        HID = FFN_EXP if moe else FFN_DENSE
        blocks = [(b0, min(512, HID - b0)) for b0 in range(0, HID, 512)]
        xs = sbt("ff_x", [128, 8, TS])
        h2 = sbt("ff_h2", [128, 8, TS], BF16)
        acc = sbt("ff_acc", [128, 8, TS])
        sq, rs = sbt("ff_sq", [128, 8, 128]), sbt("ff_rs", [128, 128])
        hf = sbt("ff_hf", [128, 8, 128])
        tmp = None
        sa = [sbt(f"ff_sa{k}", [128, TILE]) for k in range(3)]; tt = [sbt(f"ff_tt{k}", [128, TILE]) for k in range(3)]
        hid = [sbt(f"ff_hid{k}", [128, 4, TILE], BF16) for k in range(2)]
        pacc = self.pS
        pbanks = [(self.pS, "pS"), (self.pYd, "pYd"), (self.pYo, "pYo"), (self.pSt, "pSt")]
        if moe:
            rw = sbt("ff_rw", [128, 8, NEXP])
            kb.dma("sp", rw[:], self.router_w[0].rearrange("(kc p) n -> p kc n", p=128), writes=["ff_rw"])
            lg = sbt("ff_lg", [128, NEXP]); l2 = sbt("ff_l2", [128, NEXP]); m1 = sbt("ff_m1", [128, 4])
            mk1 = sbt("ff_mk1", [128, NEXP]); mk2 = sbt("ff_mk2", [128, NEXP]); gt = sbt("ff_gt", [128, NEXP])
            dg = sbt("ff_dg", [128, NEXP, 128])
            gbc = sbt("ff_gbc", [128, NEXP, TS])
        for s0 in range(0, T, TS):
            src = srcT.rearrange("(kc p) t -> p kc t", p=128)
            kb.dma("sp", xs[:], src[:, :, s0:s0 + TS], reads=[("dram", id(srcT))], writes=["ff_x"])
            for q in range(TS // 128):
                ql = slice(q * 128, (q + 1) * 128)
                kb.op("act", lambda e, ql=ql: e.activation(out=sq[:], in_=xs[:, :, ql], func=AF.Square), reads=["ff_x"], writes=["ff_sq"])
                for kc in range(8):
                    kb.op("pe", lambda e, kc=kc: e.matmul(self.pM[:, 0:128], self.ones, sq[:, kc, :], start=(kc == 0), stop=(kc == 7)), reads=["ff_sq", "consts"], writes=["pM"])
                kb.op("act", lambda e: e.activation(out=rs[:], in_=self.pM[:, 0:128], func=AF.Sqrt, bias=self.eps_c, scale=1.0 / D), reads=["pM", "cst"], writes=["ff_rs"])
                kb.op("dve", lambda e: e.reciprocal(rs[:], rs[:]), reads=["ff_rs"], writes=["ff_rs"])
                for kc in range(8):
                    kb.op("dve", lambda e, kc=kc, ql=ql: e.tensor_tensor(sq[:, kc, :], xs[:, kc, ql], rs[:], ALU.mult), reads=["ff_x", "ff_rs"], writes=["ff_sq"])
                    kb.op("act", lambda e, kc=kc: e.activation(out=hf[:, kc, :], in_=sq[:, kc, :], func=AF.Identity, bias=res[:, who, 4, kc:kc + 1], scale=res[:, who, 3, kc:kc + 1]),
                          reads=["ff_sq", resn], writes=["ff_hf"])
                kb.op("pool", lambda e, ql=ql: e.tensor_copy(h2[:, :, ql], hf[:]), reads=["ff_hf"], writes=["ff_h2"])
                if moe:
                    rwf = sbt("ff_rwf", [128, 8, NEXP]) if False else None
                    for kc in range(8):
                        kb.op("pe", lambda e, kc=kc: e.matmul(self.pM[:, 256:256 + NEXP], hf[:, kc, :], rw[:, kc, :], start=(kc == 0), stop=(kc == 7)), reads=["ff_hf", "ff_rw"], writes=["pM"])
                    kb.op("act", lambda e: e.copy(lg[:], self.pM[:, 256:256 + NEXP]), reads=["pM"], writes=["ff_lg"])
                    kb.op("dve", lambda e: e.tensor_reduce(m1[:, 0:1], lg[:], mybir.AxisListType.X, ALU.max), reads=["ff_lg"], writes=["ff_m1"])
                    kb.op("dve", lambda e: e.tensor_tensor(mk1[:], lg[:], bc(m1[:, 0:1], [128, NEXP]), ALU.is_equal), reads=["ff_lg", "ff_m1"], writes=["ff_mk1"])
                    kb.op("dve", lambda e: e.scalar_tensor_tensor(l2[:], mk1[:], -1e30, lg[:], ALU.mult, ALU.add), reads=["ff_mk1", "ff_lg"], writes=["ff_l2"])
                    kb.op("dve", lambda e: e.tensor_reduce(m1[:, 1:2], l2[:], mybir.AxisListType.X, ALU.max), reads=["ff_l2"], writes=["ff_m1"])
                    kb.op("dve", lambda e: e.tensor_tensor(mk2[:], l2[:], bc(m1[:, 1:2], [128, NEXP]), ALU.is_equal), reads=["ff_l2", "ff_m1"], writes=["ff_mk2"])
                    kb.op("dve", lambda e: e.tensor_tensor(m1[:, 2:3], m1[:, 1:2], m1[:, 0:1], ALU.subtract), reads=["ff_m1"], writes=["ff_m1"])
                    kb.op("act", lambda e: e.activation(out=m1[:, 2:3], in_=m1[:, 2:3], func=AF.Exp), reads=["ff_m1"], writes=["ff_m1"])
                    kb.op("dve", lambda e: e.tensor_scalar(m1[:, 2:3], m1[:, 2:3], 1.0, None, ALU.add), reads=["ff_m1"], writes=["ff_m1"])
                    kb.op("dve", lambda e: e.reciprocal(m1[:, 2:3], m1[:, 2:3]), reads=["ff_m1"], writes=["ff_m1"])
                    kb.op("dve", lambda e: e.tensor_scalar(m1[:, 3:4], m1[:, 2:3], -1.0, 1.0, ALU.mult, ALU.add), reads=["ff_m1"], writes=["ff_m1"])
                    kb.op("dve", lambda e: e.tensor_scalar(gt[:], mk1[:], m1[:, 2:3], None, ALU.mult), reads=["ff_mk1", "ff_m1"], writes=["ff_gt"])
                    kb.op("dve", lambda e: e.scalar_tensor_tensor(gt[:], mk2[:], m1[:, 3:4], gt[:], ALU.mult, ALU.add), reads=["ff_mk2", "ff_m1", "ff_gt"], writes=["ff_gt"])
                    kb.op("dve", lambda e: e.tensor_tensor(dg[:], bc(self.ident.unsqueeze(1), [128, NEXP, 128]), bc(gt[:].unsqueeze(2), [128, NEXP, 128]), ALU.mult),
                          reads=["consts", "ff_gt"], writes=["ff_dg"])
                    for hh in range(2):
                        ps, pn = self.next_pab()
                        kb.op("pe", lambda e, ps=ps, hh=hh: e.matmul(ps[:], self.ones, dg[:, hh * 4:(hh + 1) * 4, :].rearrange("p a b -> p (a b)"), start=True, stop=True),
                              reads=["ff_dg", "consts"], writes=[pn])
                        kb.op("act", lambda e, ps=ps, hh=hh, ql=ql: e.copy(gbc[:, hh * 4:(hh + 1) * 4, ql], ps[:].rearrange("p (a b) -> p a b", a=4)), reads=[pn], writes=["ff_gbc"])
            kb.barrier()
            items = []
            for ex in range(NE):
                for bi, (b0, bn) in enumerate(blocks):
                    for tq in range(TS // TILE):
                        items.append((ex, bi, b0, bn, tq))
            wcache = {}
            slots = [(self.pA, "pA"), (self.pB, "pB"), (self.pM, "pM"), (self.pT, "pT")]
            self.ffs = getattr(self, "ffs", 0)

            def slot():
                self.ffs = (self.ffs + 1) % len(slots)
                t, n = slots[self.ffs]
                return t[:, 0:TILE], n

            def Wsrc(ex):
                if moe:
                    return self.moe_w1[0, ex], self.moe_w3[0, ex], self.moe_w2[0, ex]
                return self.ffn_w1[0], self.ffn_w3[0], self.ffn_w2[0]

            def AB(n):
                ex, bi, b0, bn, tq = items[n]
                nh = bn // 128
                W1, W3, W2 = Wsrc(ex)
                if (ex, bi, 1) not in wcache:
                    wcache[(ex, bi, 1)] = self.load_w(W1, b0, bn)
                    wcache[(ex, bi, 3)] = self.load_w(W3, b0, bn)
                ntq = TS // TILE
                if tq == min(1, ntq - 1) and n + ntq - tq < len(items):
                    ex2, bi2, b02, bn2, _ = items[n + ntq - tq]
                    if (ex2, bi2, 1) not in wcache:
                        W1b, W3b, _w = Wsrc(ex2)
                        wcache[(ex2, bi2, 1)] = self.load_w(W1b, b02, bn2)
                        wcache[(ex2, bi2, 3)] = self.load_w(W3b, b02, bn2)
                w1, w1n = wcache[(ex, bi, 1)]
                w3, w3n = wcache[(ex, bi, 3)]
                tl = slice(tq * TILE, (tq + 1) * TILE)
                hd = hid[n % 2]; hdn = f"ff_hid{n % 2}"
                for hc in range(nh):
                    pa, pan = slot()
                    pb_, pbn_ = slot()
                    for kc in range(8):
                        kb.op("pe", lambda e, kc=kc: e.matmul(pa, w1[:, kc, hc * 128:(hc + 1) * 128], h2[:, kc, tl], start=(kc == 0), stop=(kc == 7)),
                              reads=[w1n, "ff_h2"], writes=[pan])
                    for kc in range(8):
                        kb.op("pe", lambda e, kc=kc: e.matmul(pb_, w3[:, kc, hc * 128:(hc + 1) * 128], h2[:, kc, tl], start=(kc == 0), stop=(kc == 7)),
                              reads=[w3n, "ff_h2"], writes=[pbn_])
                    k3 = (n * 4 + hc) % 3
                    kb.op("act", lambda e: e.activation(out=sa[k3][:], in_=pa, func=AF.Silu), reads=[pan], writes=[f"ff_sa{k3}"])
                    if moe:
                        kb.op("dve", lambda e: e.tensor_tensor(tt[k3][:], sa[k3][:], pb_, ALU.mult), reads=[f"ff_sa{k3}", pbn_], writes=[f"ff_tt{k3}"])
                        kb.op("dve", lambda e: e.tensor_tensor(hd[:, hc, :], tt[k3][:], gbc[:, ex, tl], ALU.mult), reads=[f"ff_tt{k3}", "ff_gbc"], writes=[hdn])
                    else:
                        kb.op("dve", lambda e: e.tensor_tensor(hd[:, hc, :], sa[k3][:], pb_, ALU.mult), reads=[f"ff_sa{k3}", pbn_], writes=[hdn])

            def W2s(n):
                ex, bi, b0, bn, tq = items[n]
                nh = bn // 128
                W1, W3, W2 = Wsrc(ex)
                if (ex, bi, 2) not in wcache:
                    wcache[(ex, bi, 2)] = self.load_w_rows(W2, b0 // 128, nh)
                w2, w2n = wcache[(ex, bi, 2)]
                tl = slice(tq * TILE, (tq + 1) * TILE)
                hd = hid[n % 2]; hdn = f"ff_hid{n % 2}"
                for fo in range(8):
                    pb, pbn = pbanks[fo // 2]
                    osl = slice((fo % 2) * TILE, (fo % 2 + 1) * TILE)
                    for hc in range(nh):
                        kb.op("pe", lambda e, hc=hc: e.matmul(pb[:, osl], w2[:, hc, fo * 128:(fo + 1) * 128], hd[:, hc, :], start=(hc == 0), stop=(hc == nh - 1)),
                              reads=[w2n, hdn], writes=[pbn])
                first = (ex == 0 and bi == 0)
                for k4 in range(4):
                    pb, pbn = pbanks[k4]
                    a_v = acc[:, 2 * k4:2 * k4 + 2, tl]
                    p_v = pb[:].rearrange("p (a t) -> p a t", a=2)
                    if first:
                        kb.op("act", lambda e: e.copy(a_v, p_v), reads=[pbn], writes=[f"ff_acc{tq}"])
                    else:
                        kb.op("dve", lambda e: e.tensor_tensor(a_v, a_v, p_v, ALU.add), reads=[pbn, f"ff_acc{tq}"], writes=[f"ff_acc{tq}"])

            AB(0)
            for n in range(1, len(items)):
                AB(n)
                W2s(n - 1)
            W2s(len(items) - 1)
            accn = [f"ff_acc{tq}" for tq in range(TS // TILE)]
            for fo in range(8):
                kb.op("dve", lambda e, fo=fo: e.scalar_tensor_tensor(acc[:, fo, :], acc[:, fo, :], res[:, who, 5, fo:fo + 1], xs[:, fo, :], ALU.mult, ALU.add),
                      reads=accn + [resn, "ff_x"], writes=accn)
            kb.dma("sp", dstT.rearrange("(kc p) t -> p kc t", p=128)[:, :, s0:s0 + TS], acc[:], reads=accn, writes=[("dram", id(dstT))])
            kb.barrier()
        kb.barrier()
        st.close()

    def load_w_rows(self, w2d, r0, nk):
        kb = self.kb
        slot = self.wrr % self.NWB
        self.wrr += 1
        buf = self.wbuf[slot]
        v = buf[:, 0:nk * 1024].rearrange("p (k n) -> p k n", n=1024)
        src = w2d.rearrange("(kc p) n -> p kc n", p=128)
        kb.dma("pool", v, src[:, r0:r0 + nk, :], writes=[f"wbuf{slot}"])
        return v, f"wbuf{slot}"

    def final(self, srcT):
        kb = self.kb
        st = contextlib.ExitStack()
        sbt = lambda n, s, dt=F32: kb.sb(n, s, dt, st)
        fg = self.colvec("fn_g", self.final_g, 8, st)
        xw = [sbt(f"fn_x{k}", [128, 8, 128]) for k in range(2)]
        sq, rs = sbt("fn_sq", [128, 8, 128]), sbt("fn_rs", [128, 128])
        ot = [sbt(f"fn_o{k}", [128, D]) for k in range(2)]
        src = srcT.rearrange("(kc p) t -> p kc t", p=128)
        for t in range(SEQ // 128):
            x_, xn = xw[t % 2], f"fn_x{t % 2}"
            o_, on = ot[t % 2], f"fn_o{t % 2}"
            kb.dma("sp", x_[:], src[:, :, t * 128:(t + 1) * 128], reads=[("dram", id(srcT))], writes=[xn])
            kb.op("act", lambda e, x_=x_: e.activation(out=sq[:], in_=x_[:], func=AF.Square), reads=[xn], writes=["fn_sq"])
            for kc in range(8):
                kb.op("pe", lambda e, kc=kc: e.matmul(self.pM[:, 0:128], self.ones, sq[:, kc, :], start=(kc == 0), stop=(kc == 7)), reads=["fn_sq", "consts"], writes=["pM"])
            kb.op("act", lambda e: e.activation(out=rs[:], in_=self.pM[:, 0:128], func=AF.Sqrt, bias=self.eps_c, scale=1.0 / D), reads=["pM", "cst"], writes=["fn_rs"])
            kb.op("dve", lambda e: e.reciprocal(rs[:], rs[:]), reads=["fn_rs"], writes=["fn_rs"])
            for kc in range(8):
                kb.op("dve", lambda e, kc=kc, x_=x_: e.scalar_tensor_tensor(sq[:, kc, :], x_[:, kc, :], fg[:, kc:kc + 1], rs[:], ALU.mult, ALU.mult), reads=[xn, "fn_g", "fn_rs"], writes=["fn_sq"])
            for h in range(2):
                ps, pn = self.next_pab()
                for j in range(4):
                    kc = h * 4 + j
                    kb.op("pe", lambda e, ps=ps, j=j, kc=kc: e.transpose(ps[:, j * 128:(j + 1) * 128], sq[:, kc, :], self.ident), reads=["fn_sq", "consts"], writes=[pn])
                kb.op("act", lambda e, ps=ps, h=h, o_=o_: e.copy(o_[:, h * 512:(h + 1) * 512], ps[:]), reads=[pn], writes=[on])
            kb.dma("sp", self.out[t * 128:(t + 1) * 128, :], o_[:], reads=[on], writes=["out"])
        kb.barrier()
        st.close()

    def build(self):
        kb = self.kb
        self.to_fm(self.x, self.xT[0], SEQ)
        self.to_fm(self.ctx, self.cT[0], CTX)
        xa, xb = self.xT
        ca, cb_ = self.cT
        Sf = kb.sb("S_f", [128, INNER]); Sb = kb.sb("S_b", [128, INNER])
        for i in range(DEPTH):
            last = i == DEPTH - 1
            st = contextlib.ExitStack()
            res, resn = self.mod_vectors(i, st)
            kb.barrier()
            st.close()
            kb.op("dve", lambda e: e.memset(Sf[:], 0.0), writes=[f"S_f{g}" for g in range(NG)])
            kb.op("dve", lambda e: e.memset(Sb[:], 0.0), writes=[f"S_b{g}" for g in range(NG)])
            self.mixer_pass(i, 1, ca, cb_, CTX, 1, res, resn, Sb, "S_b", CTX, last, False)
            self.mixer_pass(i, 0, ca, cb_, CTX, 1, res, resn, Sf, "S_f", CTX, last, not last)
            self.mixer_pass(i, 1, xa, xb, SEQ, 0, res, resn, Sb, "S_b", 64, last, False)
            self.mixer_pass(i, 0, xa, xb, SEQ, 0, res, resn, Sf, "S_f", 64, last, True)
            self.ffn(i, xb, xa, SEQ, 0, res, resn, moe=(i % 2 == 1))
            if not last:
                self.ffn(i, cb_, ca, CTX, 1, res, resn, moe=(i % 2 == 1))
        self.final(xa)
        return kb.finish()


def _consts():
    c = np.zeros((128, 512), np.float32)
    c[:, 0:128] = np.eye(128, dtype=np.float32)
    l = np.arange(128)
    c[:, 128:256] = (l[:, None] <= l[None, :]).astype(np.float32)
    c[:, 256:384] = (l[:, None] >= l[None, :]).astype(np.float32)
    c[:, 384:512] = 1.0
    return c


_NAMES = ["w_mod", "b_mod", "norm1_g", "norm2_g", "w_in", "b_gate", "ssd_conv_w", "ssd_conv_b", "ssd_dt_bias", "ssd_a_log",
          "ssd_d", "ssd_norm_g", "w_ssd_out", "sc_conv_w", "w_sc_out", "w_o", "ffn_w1", "ffn_w3", "ffn_w2", "router_w",
          "moe_w1", "moe_w3", "moe_w2", "final_g", "c_ctx"]


def kernel(**inputs):
    prog = Prog()
    nc = prog.build()
    shared = {n: np.ascontiguousarray(np.asarray(inputs[n], dtype=np.float32)) for n in _NAMES}
    shared["consts"] = _consts()
    x = np.asarray(inputs["x"], dtype=np.float32)
    c = np.asarray(inputs["c"], dtype=np.float32)
    ctx = np.asarray(inputs["ctx"], dtype=np.float32)
    in_maps = []
    for b in range(8):
        m = dict(shared)
        m["x"] = np.ascontiguousarray(x[b])
        m["c"] = np.ascontiguousarray(c[b])
        m["ctx"] = np.ascontiguousarray(ctx[b])
        in_maps.append(m)
    res = run_bass_kernel_spmd(nc, in_maps, core_ids=list(range(8)))
    return np.stack([np.asarray(r["out"]) for r in res.results], axis=0).astype(np.float32)
```

```python
import contextlib
import numpy as np
import concourse.bass as bass
import concourse.mybir as mybir
from concourse.bass_utils import run_bass_kernel_spmd

F32 = mybir.dt.float32
BF16 = mybir.dt.bfloat16
AF = mybir.ActivationFunctionType
ALU = mybir.AluOpType

D = 1024
SEQ = 4096
CTX = 256
DEPTH = 2
INNER = 2048
NH = 32
NG = 4
XBC = 3072
INW = 10304
C_Z, C_XBC, C_DT, C_SC, C_GL = 0, 2048, 5120, 5184, 8256
FFN_DENSE = 2816
FFN_EXP = 3584
NEXP = 8
EPS = 1e-6
TILE = 256
WIN = TILE + 2

NDS = 12
SAME_ENGINE_SYNC = True


class KB:
    def __init__(self):
        self.nc = bass.Bass("TRN2", target_bir_lowering=False)
        nc = self.nc
        self.es = contextlib.ExitStack()
        self.eng = {"pe": nc.tensor, "act": nc.scalar, "dve": nc.vector, "pool": nc.gpsimd, "sp": nc.sync}
        self.sem, self.cnt = {}, {}
        for e in self.eng:
            self.sem[e] = self.es.enter_context(nc.semaphore("s_" + e))
            self.cnt[e] = 0
        self.dsems, self.dcount, self.drr = {}, {}, {}
        self.semobj = {}
        for e, s in self.sem.items():
            self.semobj[("c", e)] = s
        for q in ("sp", "pool", "act"):
            self.dsems[q] = []
            for i in range(NDS):
                s = self.es.enter_context(nc.semaphore(f"d_{q}{i}"))
                self.dsems[q].append(("d", q, i))
                self.semobj[("d", q, i)] = s
                self.dcount[("d", q, i)] = 0
            self.drr[q] = 0
        self.waited, self.res_w, self.res_r = {}, {}, {}
        self.ninst = 0

    def sb(self, name, shape, dt=F32, stack=None):
        self.nuid = getattr(self, "nuid", 0) + 1
        return (stack or self.es).enter_context(self.nc.sbuf_tensor(f"{name}_u{self.nuid}", list(shape), dt))

    def ps(self, name, shape, dt=F32, stack=None):
        return (stack or self.es).enter_context(self.nc.psum_tensor(name, list(shape), dt))

    def dram(self, name, shape, dt=F32, kind="Internal"):
        return self.nc.dram_tensor(name, list(shape), dt, kind=kind).ap()

    def _wait(self, e, key, val):
        if key == ("c", e) and (e == "pe" or not SAME_ENGINE_SYNC):
            return
        k = (e, key)
        if self.waited.get(k, 0) >= val:
            return
        self.eng[e].wait_ge(self.semobj[key], val)
        self.waited[k] = val

    def _deps(self, reads, writes):
        deps = {}
        for r in reads:
            t = self.res_w.get(r)
            if t is not None:
                deps[t[0]] = max(deps.get(t[0], 0), t[1])
        for w in writes:
            t = self.res_w.get(w)
            if t is not None:
                deps[t[0]] = max(deps.get(t[0], 0), t[1])
            for k, v in self.res_r.get(w, {}).items():
                deps[k] = max(deps.get(k, 0), v)
        return deps

    def _record(self, token, reads, writes):
        for r in reads:
            d = self.res_r.setdefault(r, {})
            d[token[0]] = max(d.get(token[0], 0), token[1])
        for w in writes:
            self.res_w[w] = token
            self.res_r[w] = {}

    def op(self, e, fn, reads=(), writes=()):
        for key, val in self._deps(reads, writes).items():
            self._wait(e, key, val)
        inst = fn(self.eng[e])
        self.cnt[e] += 1
        inst.then_inc(self.sem[e], 1)
        self._record((("c", e), self.cnt[e]), reads, writes)
        self.ninst += 1
        return inst

    def dma(self, q, out, in_, reads=(), writes=(), **kw):
        i = self.drr[q] % NDS
        self.drr[q] += 1
        key = self.dsems[q][i]
        if self.dcount[key] > 0:
            self._wait(q, key, 16 * self.dcount[key])
        for k, val in self._deps(reads, writes).items():
            self._wait(q, k, val)
        inst = self.eng[q].dma_start(out=out, in_=in_, **kw)
        inst.then_inc(self.semobj[key], 16)
        self.dcount[key] += 1
        self._record((key, 16 * self.dcount[key]), reads, writes)
        self.ninst += 1
        return inst

    def barrier(self):
        for e in self.eng:
            for f in self.eng:
                if f != e and self.cnt[f] > 0:
                    self._wait(e, ("c", f), self.cnt[f])
            for key, c in self.dcount.items():
                if c > 0:
                    self._wait(e, key, 16 * c)

    def finish(self):
        self.barrier()
        self.es.close()
        return self.nc


def bc(ap, shape):
    return ap.to_broadcast(list(shape))


class Prog:
    def __init__(self, debug=False, stop_after=None):
        self.debug = debug
        self.stop_after = stop_after
        self.kb = KB()
        kb = self.kb
        nc = kb.nc
        I = lambda n, s: nc.dram_tensor(n, list(s), F32, kind="ExternalInput").ap()
        self.x = I("x", [SEQ, D])
        self.c = I("c", [D])
        self.ctx = I("ctx", [CTX, D])
        self.c_ctx = I("c_ctx", [D])
        self.w_mod = I("w_mod", [DEPTH, D, 6 * D])
        self.b_mod = I("b_mod", [DEPTH, 6 * D])
        self.norm1_g = I("norm1_g", [DEPTH, D])
        self.norm2_g = I("norm2_g", [DEPTH, D])
        self.w_in = I("w_in", [DEPTH, D, INW])
        self.b_gate = I("b_gate", [DEPTH, 2 * D])
        self.ssd_conv_w = I("ssd_conv_w", [DEPTH, 3, XBC])
        self.ssd_conv_b = I("ssd_conv_b", [DEPTH, XBC])
        self.ssd_dt_bias = I("ssd_dt_bias", [DEPTH, 2, NH])
        self.ssd_a_log = I("ssd_a_log", [DEPTH, 2, NH])
        self.ssd_d = I("ssd_d", [DEPTH, NH])
        self.ssd_norm_g = I("ssd_norm_g", [DEPTH, INNER])
        self.w_ssd_out = I("w_ssd_out", [DEPTH, INNER, D])
        self.sc_conv_w = I("sc_conv_w", [DEPTH, 3, D])
        self.w_sc_out = I("w_sc_out", [DEPTH, D, D])
        self.w_o = I("w_o", [DEPTH, D, D])
        self.ffn_w1 = I("ffn_w1", [1, D, FFN_DENSE])
        self.ffn_w3 = I("ffn_w3", [1, D, FFN_DENSE])
        self.ffn_w2 = I("ffn_w2", [1, FFN_DENSE, D])
        self.router_w = I("router_w", [1, D, NEXP])
        self.moe_w1 = I("moe_w1", [1, NEXP, D, FFN_EXP])
        self.moe_w3 = I("moe_w3", [1, NEXP, D, FFN_EXP])
        self.moe_w2 = I("moe_w2", [1, NEXP, FFN_EXP, D])
        self.final_g = I("final_g", [D])
        self.consts_d = I("consts", [128, 512])
        self.out = nc.dram_tensor("out", [SEQ, D], F32, kind="ExternalOutput").ap()
        self.xT = [kb.dram(f"xT{i}", [D, SEQ]) for i in range(2)]
        self.cT = [kb.dram(f"cT{i}", [D, CTX]) for i in range(2)]
        self.YB = kb.dram("YB", [SEQ, INNER])
        self.XB = kb.dram("XBst", [SEQ // TILE, 128, 24 * TILE], BF16)
        self.XS = kb.dram("XSst", [SEQ // 128, 128, INNER], BF16)
        self.BS = kb.dram("BSst", [SEQ // 128, 128, 512], BF16)
        self.NCS = 4
        self.csrow = kb.dram("csrow", [self.NCS, NH * 128])
        self.csrr = 0
        self.dbg = {}
        self.consts = kb.sb("consts", [128, 512])
        kb.dma("sp", self.consts[:], self.consts_d, writes=["consts"])
        self.ident = self.consts[:, 0:128]
        self.triF = self.consts[:, 128:256]
        self.triB = self.consts[:, 256:384]
        self.ones = self.consts[:, 384:512]
        self.identb = kb.sb("identb", [128, 128], BF16)
        kb.op("dve", lambda e: e.tensor_copy(self.identb[:], self.ident), reads=["consts"], writes=["identb"])
        self.cst = kb.sb("cst", [128, 4])
        kb.op("dve", lambda e: e.memset(self.cst[:, 0:1], 1.0), writes=["cst"])
        kb.op("dve", lambda e: e.memset(self.cst[:, 1:2], EPS), writes=["cst"])
        kb.op("dve", lambda e: e.memset(self.cst[:, 2:3], 0.0), writes=["cst"])
        self.one_c = self.cst[:, 0:1]
        self.eps_c = self.cst[:, 1:2]
        self.pA = kb.ps("pA", [128, 512])
        self.pB = kb.ps("pB", [128, 512])
        self.pT = kb.ps("pT", [128, 512])
        self.pTb = self.pT[:].bitcast(BF16)
        self.pS = kb.ps("pS", [128, 512])
        self.pYd = kb.ps("pYd", [128, 512])
        self.pYo = kb.ps("pYo", [128, 512])
        self.pSt = kb.ps("pSt", [128, 512])
        self.pM = kb.ps("pM", [128, 512])
        self.pab = 0
        self.modres = [kb.sb(f"modres{i}", [128, 2, 6, 8], F32) for i in range(DEPTH)]
        self.rot2 = [(self.pA, "pA"), (self.pB, "pB")]
        self.rot6 = [(self.pA, "pA"), (self.pB, "pB"), (self.pS, "pS"), (self.pYd, "pYd"), (self.pYo, "pYo"), (self.pSt, "pSt")]
        self.rot = self.rot6
        self.wdram = {}

    def load_w(self, w2d, c0, ncols, r0=0, nk=8, key=None):
        kb = self.kb
        slot = self.wrr % self.NWB
        self.wrr += 1
        buf = self.wbuf[slot]
        n = nk * ncols
        assert n <= 4096
        v = buf[:, 0:n].rearrange("p (k n) -> p k n", n=ncols)
        wn = f"wbuf{slot}"
        if key is not None and key in self.wdram:
            kb.dma("sp", buf[:, 0:n], self.wdram[key], reads=[("wd", key)], writes=[wn])
            return v, wn
        src = w2d.rearrange("(kc p) n -> p kc n", p=128)
        for k0 in range(0, nk, 8):
            k1 = min(nk, k0 + 8)
            kb.dma("pool", v[:, k0:k1, :], src[:, r0 + k0:r0 + k1, c0:c0 + ncols], writes=[wn])
        if key is not None:
            scr = kb.dram(f"wd{len(self.wdram)}", [128, n], BF16)
            kb.dma("sp", scr, buf[:, 0:n], reads=[wn], writes=[("wd", key)])
            self.wdram[key] = scr
        return v, wn

    def next_pab(self):
        self.pab = (self.pab + 1) % len(self.rot)
        return self.rot[self.pab]

    def set_wbufs(self, n, stack):
        self.NWB = n
        self.wbuf = [self.kb.sb(f"wbuf{i}", [128, 4096], BF16, stack) for i in range(n)]
        self.wrr = 0

    def colvec(self, name, src1d, n, stack=None):
        kb = self.kb
        t = kb.sb(name, [128, n], F32, stack)
        with kb.nc.allow_non_contiguous_dma(reason="small param vector"):
            kb.dma("sp", t[:], src1d.rearrange("(c p) -> p c", p=128), writes=[name])
        return t

    def rowbc(self, name, src1d, n, stack=None):
        kb = self.kb
        t = kb.sb(name, [128, n], F32, stack)
        kb.dma("sp", t[:], src1d.partition_broadcast(128), writes=[name])
        return t

    def to_fm(self, src_tm, dstT, T):
        kb = self.kb
        with contextlib.ExitStack() as st:
            xin = [kb.sb(f"tfm_in{i}", [128, D], F32, st) for i in range(2)]
            xo = [kb.sb(f"tfm_o{i}", [128, 8, 128], F32, st) for i in range(2)]
            for t in range(T // 128):
                a, o = xin[t % 2], xo[t % 2]
                an, on = f"tfm_in{t % 2}", f"tfm_o{t % 2}"
                kb.dma("sp", a[:], src_tm[t * 128:(t + 1) * 128, :], writes=[an])
                for h in range(2):
                    ps, pn = self.next_pab()
                    for j in range(4):
                        kc = h * 4 + j
                        kb.op("pe", lambda e, ps=ps, j=j, kc=kc, a=a: e.transpose(ps[:, j * 128:(j + 1) * 128], a[:, kc * 128:(kc + 1) * 128], self.ident),
                              reads=[an, "consts"], writes=[pn])
                    kb.op("act", lambda e, ps=ps, o=o, h=h: e.copy(o[:, h * 4:(h + 1) * 4, :], ps[:].rearrange("p (j t) -> p j t", j=4)),
                          reads=[pn], writes=[on])
                kb.dma("sp", dstT.rearrange("(kc p) t -> p kc t", p=128)[:, :, t * 128:(t + 1) * 128], o[:], reads=[on], writes=[("dram", id(dstT))])
            kb.barrier()

    def mod_vectors(self, i, st):
        kb = self.kb
        cc = kb.sb("mod_cc", [128, 8, 2], F32, st)
        with kb.nc.allow_non_contiguous_dma(reason="small"):
            kb.dma("sp", cc[:, :, 0], self.c.rearrange("(c p) -> p c", p=128), writes=["mod_cc"])
            kb.dma("sp", cc[:, :, 1], self.c_ctx.rearrange("(c p) -> p c", p=128), writes=["mod_cc"])
        sc = kb.sb("mod_sc", [128, 8, 2], F32, st)
        kb.op("act", lambda e: e.activation(out=sc[:], in_=cc[:], func=AF.Silu), reads=["mod_cc"], writes=["mod_sc"])
        bm = self.colvec("mod_b", self.b_mod[i], 48, st)
        n1 = self.colvec("mod_n1", self.norm1_g[i], 8, st)
        n2 = self.colvec("mod_n2", self.norm2_g[i], 8, st)
        mv = kb.sb("mod_mv", [128, 48, 2], F32, st)
        wst = kb.sb("mod_w", [128, 8, 512], F32, st)
        wsrc = self.w_mod[i].rearrange("(kc p) n -> p kc n", p=128)
        for blk in range(12):
            kb.dma("sp", wst[:], wsrc[:, :, blk * 512:(blk + 1) * 512], writes=["mod_w"])
            for j in range(4):
                col = blk * 4 + j
                for kc in range(8):
                    kb.op("pe", lambda e, j=j, kc=kc: e.matmul(self.pM[:, 0:2], wst[:, kc, j * 128:(j + 1) * 128], sc[:, kc, :], start=(kc == 0), stop=(kc == 7)),
                          reads=["mod_w", "mod_sc"], writes=["pM"])
                kb.op("dve", lambda e, col=col: e.tensor_tensor(mv[:, col, :], self.pM[:, 0:2], bc(bm[:, col:col + 1], [128, 2]), ALU.add),
                      reads=["pM", "mod_b"], writes=["mod_mv"])
        res = self.modres[i]
        for who in range(2):
            for half, nrm in ((0, n1), (1, n2)):
                b0 = half * 3
                kb.op("dve", lambda e, who=who, b0=b0: e.tensor_copy(res[:, who, b0 + 1, :], mv[:, b0 * 8:(b0 + 1) * 8, who]), reads=["mod_mv"], writes=[f"modres{i}"])
                kb.op("dve", lambda e, who=who, b0=b0, nrm=nrm: e.scalar_tensor_tensor(res[:, who, b0, :], mv[:, (b0 + 1) * 8:(b0 + 2) * 8, who], 1.0, nrm[:], ALU.add, ALU.mult),
                      reads=["mod_mv", "mod_n1", "mod_n2"], writes=[f"modres{i}"])
                kb.op("dve", lambda e, who=who, b0=b0: e.tensor_copy(res[:, who, b0 + 2, :], mv[:, (b0 + 2) * 8:(b0 + 3) * 8, who]), reads=["mod_mv"], writes=[f"modres{i}"])
        return res, f"modres{i}"

    def make_hT(self, srcT, T, t0, ncols, lead, res, resn, who, vec0, hT, hTn, tmp):
        kb = self.kb
        xw, xwn, sq, sqn, rs, rsn = tmp
        lo = t0 - lead
        hi = lo + ncols
        clo, chi = max(lo, 0), min(hi, T)
        j0, j1 = clo - lo, chi - lo
        src = srcT.rearrange("(kc p) t -> p kc t", p=128)
        kb.dma("sp", xw[:, :, j0:j1], src[:, :, clo:chi], reads=[("dram", id(srcT))], writes=[xwn])
        sqk = [f"{sqn}{kc}" for kc in range(8)]
        kb.op("act", lambda e: e.activation(out=sq[:, :, j0:j1], in_=xw[:, :, j0:j1], func=AF.Square), reads=[xwn], writes=sqk)
        for kc in range(8):
            kb.op("pe", lambda e, kc=kc: e.matmul(self.pM[:, j0:j1], self.ones, sq[:, kc, j0:j1], start=(kc == 0), stop=(kc == 7)),
                  reads=[sqk[kc], "consts"], writes=["pM"])
        kb.op("act", lambda e: e.activation(out=rs[:, j0:j1], in_=self.pM[:, j0:j1], func=AF.Sqrt, bias=self.eps_c, scale=1.0 / D),
              reads=["pM", "cst"], writes=[rsn])
        kb.op("dve", lambda e: e.reciprocal(rs[:, j0:j1], rs[:, j0:j1]), reads=[rsn], writes=[rsn])
        for kc in range(8):
            kb.op("dve", lambda e, kc=kc: e.tensor_tensor(sq[:, kc, j0:j1], xw[:, kc, j0:j1], rs[:, j0:j1], ALU.mult), reads=[xwn, rsn], writes=[sqk[kc]])
        for kc in range(8):
            kb.op("act", lambda e, kc=kc: e.activation(out=hT[:, kc, j0:j1], in_=sq[:, kc, j0:j1], func=AF.Identity,
                                                        bias=res[:, who, vec0 + 1, kc:kc + 1], scale=res[:, who, vec0, kc:kc + 1]),
                  reads=[sqk[kc], resn], writes=[hTn])
        if j0 > 0:
            kb.op("dve", lambda e: e.memset(hT[:, :, 0:j0], 0.0), writes=[hTn])
        if j1 < ncols:
            kb.op("dve", lambda e: e.memset(hT[:, :, j1:ncols], 0.0), writes=[hTn])

    def mixer_pass(self, i, d, srcT, dstT, T, who, res, resn, S, Sn, grid, lp, write_out):
        kb = self.kb
        st = contextlib.ExitStack()
        w_in = self.w_in[i]
        ntile = T // TILE
        self.uid = getattr(self, "uid", 0)

        def sbt(n, s, dt=F32, stack=None):
            self.uid += 1
            return kb.sb(f"{n}_{self.uid}", s, dt, stack or st)
        nset = 2 if d == 1 else 1
        hsets = []
        for k in range(nset):
            hsets.append((sbt(f"mx_xw{k}", [128, 8, WIN]), sbt(f"mx_sq{k}", [128, 8, WIN]), sbt(f"mx_rs{k}", [128, WIN]), sbt(f"mx_hT{k}", [128, 8, WIN], BF16)))

        def hset(k):
            xw_, sq_, rs_, hT_ = hsets[k]
            return xw_, f"mx_xw{k}", sq_, rs_, hT_, f"mx_hT{k}", (xw_, f"mx_xw{k}", sq_, f"mx_sq{k}", rs_, f"mx_rs{k}")
        Sbf = sbt("mx_Sbf", [128, INNER], BF16)
        kb.op("act", lambda e: e.copy(Sbf[:], S[:]), reads=[f"{Sn}{g}" for g in range(NG)], writes=[f"mx_Sbf{g}" for g in range(NG)])
        self.rot = self.rot6
        self.set_wbufs(4, st)
        dtb = sbt("mx_dtb", [64, 1]); alg = sbt("mx_alg", [64, 1]); aneg = sbt("mx_aneg", [64, 1])
        with kb.nc.allow_non_contiguous_dma(reason="small"):
            kb.dma("sp", dtb[:], self.ssd_dt_bias[i].rearrange("a (h o) -> (a h) o", o=1), writes=["mx_dtb"])
            kb.dma("sp", alg[:], self.ssd_a_log[i].rearrange("a (h o) -> (a h) o", o=1), writes=["mx_alg"])
        kb.op("act", lambda e: e.activation(out=aneg[:], in_=alg[:], func=AF.Exp), reads=["mx_alg"], writes=["mx_aneg"])
        kb.op("dve", lambda e: e.tensor_scalar(aneg[:], aneg[:], -1.0, None, ALU.mult), reads=["mx_aneg"], writes=["mx_aneg"])
        cw = sbt("mx_cw", [128, 3, 24]); cb = self.colvec("mx_cb", self.ssd_conv_b[i], 24, st)
        with kb.nc.allow_non_contiguous_dma(reason="small"):
            for j in range(3):
                kb.dma("sp", cw[:, j, :], self.ssd_conv_w[i, j].rearrange("(c p) -> p c", p=128), writes=["mx_cw"])
        tri = self.triB if d == 1 else self.triF
        if d == 0:
            Dbc = sbt("mx_Dbc", [128, NH])
            kb.dma("sp", Dbc[:], self.ssd_d[i].partition_broadcast(128), writes=["mx_Dbc"])
            gbc = self.rowbc("mx_gbc", self.ssd_norm_g[i], INNER, st)
            ynT = sbt("mx_ynT", [128, 16, TILE], BF16)
            bgate = self.colvec("mx_bg", self.b_gate[i], 16, st)
            scw = sbt("mx_scw", [128, 3, 8])
            with kb.nc.allow_non_contiguous_dma(reason="small"):
                for j in range(3):
                    kb.dma("sp", scw[:, j, :], self.sc_conv_w[i, j].rearrange("(c p) -> p c", p=128), writes=["mx_scw"])

        tiles = list(range(ntile))
        if d == 1:
            tiles = tiles[::-1]
        for tix, ti in enumerate(tiles):
            t0 = ti * TILE
            if d == 0 or tix == 0:
                sA = contextlib.ExitStack()
                xbcT = sbt("mx_xbcT", [128, 24, TILE], BF16, sA)
                cvt = [sbt(f"mx_cvt{k}", [128, TILE], F32, sA) for k in range(4)]
                dtT = sbt("mx_dtT", [64, TILE], F32, sA)
                dAT = sbt("mx_dAT", [64, TILE], F32, sA)
                dtA_tm = [sbt(f"mx_dtA_tm{k}", [128, 128], F32, sA) for k in range(2)]
                dt_tm = [t[:, 0:64] for t in dtA_tm]
                cstot = [sbt(f"mx_cstot{k}", [128, 2 * NH], F32, sA) for k in range(2)]
                ecd = [sbt(f"mx_ecd{k}", [128, 2 * NH], F32, sA) for k in range(2)]
                cs_sb = [t[:, 0:NH] for t in cstot]
                ecs_sb = [t[:, 0:NH] for t in ecd]
                cd_sb = [t[:, NH:2 * NH] for t in ecd]
                w_sb = [sbt(f"mx_w{k}", [128, NH], F32, sA) for k in range(2)]
                csT_sb = [sbt(f"mx_csT{k}", [NH, 128], F32, sA) for k in range(2)]
                xs_tm = [sbt(f"mx_xs{k}", [128, INNER], BF16, sA) for k in range(2)]
                B_tm = [sbt(f"mx_B{k}", [128, 512], BF16, sA) for k in range(2)]
                xdt = sbt("mx_xdt", [128, INNER], BF16, sA)
                xwt = sbt("mx_xwt", [128, INNER], BF16, sA)
                scm = sbt("mx_scm", [128, 4, 128], F32, sA)
                csb = [sbt(f"mx_csb{k}", [128, 8, 128], F32, sA) for k in range(4)]
                MT = [sbt(f"mx_MT{k}", [128, 8, 128], BF16, sA) for k in range(4)]
                ytmp = [sbt(f"mx_ytmp{k}", [128, 512], F32, sA) for k in range(4)]
                Yc = [sbt(f"mx_Yc{k}", [128, INNER], F32, sA) for k in range(2)]
                if d == 0:
                    zs = [sbt(f"mx_zs{k}", [128, 512], F32, sA) for k in range(2)]
                    ss = sbt("mx_ss", [128, 2], F32, sA)
                    ysq = sbt("mx_ysq", [128, INNER], F32, sA)
                    yn_tm = sbt("mx_yn", [128, INNER], BF16, sA)
            xw, xwn, sq, rs, hT, hTn, tmp = hset(tix % nset)
            if d == 0 or tix == 0:
                self.make_hT(srcT, T, t0, WIN, 1, res, resn, who, 0, hT, hTn, tmp)
            if d == 0:
                kb.dma("sp", xbcT[:].rearrange("p a b -> p (a b)"), self.XB[ti], reads=[("XB", ti)], writes=["mx_xbcT"])
                for c in (0, 1):
                    cg = ti * 2 + c
                    kb.dma("sp", xs_tm[c][:], self.XS[cg], reads=[("XS", cg)], writes=[f"mx_xs{c}"])
                    kb.dma("sp", B_tm[c][:], self.BS[cg], reads=[("BS", cg)], writes=[f"mx_B{c}"])
            wb, wn = self.load_w(w_in, C_DT, 64, key=("in", i, C_DT))
            for kc in range(8):
                kb.op("pe", lambda e, wb=wb, kc=kc: e.matmul(self.pM[0:64, 0:WIN], wb[:, kc, 0:64], hT[:, kc, :], start=(kc == 0), stop=(kc == 7)),
                      reads=[wn, hTn], writes=["pM"])
            kb.op("act", lambda e: e.activation(out=dtT[:], in_=self.pM[0:64, 1:1 + TILE], func=AF.Exp, bias=dtb[:, 0:1]), reads=["pM", "mx_dtb"], writes=["mx_dtT"])
            kb.op("act", lambda e: e.activation(out=dtT[:], in_=dtT[:], func=AF.Ln, bias=self.one_c[0:64, :]), reads=["mx_dtT", "cst"], writes=["mx_dtT"])
            kb.op("dve", lambda e: e.tensor_scalar(dAT[:], dtT[:], aneg[:, 0:1], None, ALU.mult), reads=["mx_dtT", "mx_aneg"], writes=["mx_dAT"])
            chunks = [0, 1] if d == 0 else [1, 0]
            cslot = {}
            for c in chunks:
                cl = slice(c * 128, (c + 1) * 128)
                dsl = slice(d * NH, (d + 1) * NH)
                b1, b1n = self.next_pab()
                kb.op("pe", lambda e: e.transpose(b1[:, 0:64], dtT[:, cl], self.ident[0:64, 0:64]), reads=["mx_dtT", "consts"], writes=[b1n])
                kb.op("pe", lambda e: e.transpose(b1[:, 64:128], dAT[:, cl], self.ident[0:64, 0:64]), reads=["mx_dAT", "consts"], writes=[b1n])
                kb.op("act", lambda e: e.copy(dtA_tm[c][:], b1[:, 0:128]), reads=[b1n], writes=[f"mx_dt_tm{c}"])
                dt_c = dtA_tm[c][:, 0:64]
                dA_c = dtA_tm[c][:, 64:128]
                b2, b2n = self.next_pab()
                kb.op("pe", lambda e: e.matmul(b2[:, 0:NH], tri, dA_c[:, dsl], start=True, stop=True), reads=[f"mx_dt_tm{c}", "consts"], writes=[b2n])
                kb.op("pe", lambda e: e.matmul(b2[:, NH:2 * NH], self.ones, dA_c[:, dsl], start=True, stop=True), reads=[f"mx_dt_tm{c}", "consts"], writes=[b2n])
                kb.op("pe", lambda e: e.matmul(b2[0:NH, 128:256], dA_c[:, dsl], tri, start=True, stop=True), reads=[f"mx_dt_tm{c}", "consts"], writes=[b2n])
                kb.op("act", lambda e: e.copy(cstot[c][:], b2[:, 0:2 * NH]), reads=[b2n], writes=[f"mx_cs{c}"])
                kb.op("act", lambda e: e.copy(csT_sb[c][:], b2[0:NH, 128:256]), reads=[b2n], writes=[f"mx_csT{c}"])
                cslot[c] = self.csrr % self.NCS
                self.csrr += 1
                kb.dma("sp", self.csrow[cslot[c]].rearrange("(h l) -> h l", l=128), csT_sb[c][:], reads=[f"mx_csT{c}"], writes=[f"csrow{cslot[c]}"])
                kb.op("act", lambda e: e.activation(out=ecd[c][:], in_=cstot[c][:], func=AF.Exp), reads=[f"mx_cs{c}"], writes=[f"mx_ecs{c}"])
                kb.op("dve", lambda e: e.tensor_tensor(w_sb[c][:], cstot[c][:, NH:2 * NH], cstot[c][:, 0:NH], ALU.subtract), reads=[f"mx_cs{c}"], writes=[f"mx_w{c}"])
                kb.op("act", lambda e: e.activation(out=w_sb[c][:], in_=w_sb[c][:], func=AF.Exp), reads=[f"mx_w{c}"], writes=[f"mx_w{c}"])
                kb.op("dve", lambda e: e.tensor_tensor(w_sb[c][:], w_sb[c][:], dt_c[:, dsl], ALU.mult), reads=[f"mx_w{c}", f"mx_dt_tm{c}"], writes=[f"mx_w{c}"])
            if d == 1:
                pend = None
                for blk in range(6):
                    wb, wn = self.load_w(w_in, C_XBC + blk * 512, 512, key=("in", i, C_XBC + blk * 512))
                    for j in range(4):
                        cc = blk * 4 + j
                        ps, pn = self.next_pab()
                        for kc in range(8):
                            kb.op("pe", lambda e, ps=ps, wb=wb, j=j, kc=kc: e.matmul(ps[:, 0:WIN], wb[:, kc, j * 128:(j + 1) * 128], hT[:, kc, :], start=(kc == 0), stop=(kc == 7)),
                                  reads=[wn, hTn], writes=[pn])
                        cv = cvt[cc % 4]; cvn = f"mx_cvt{cc % 4}"
                        kb.op("act", lambda e, ps=ps, cv=cv, cc=cc: e.activation(out=cv[:], in_=ps[:, 1:1 + TILE], func=AF.Identity, bias=cb[:, cc:cc + 1], scale=cw[:, 1, cc:cc + 1]),
                              reads=[pn, "mx_cb", "mx_cw"], writes=[cvn])
                        kb.op("dve", lambda e, ps=ps, cv=cv, cc=cc: e.scalar_tensor_tensor(cv[:], ps[:, 0:TILE], cw[:, 0, cc:cc + 1], cv[:], ALU.mult, ALU.add),
                              reads=[pn, "mx_cw", cvn], writes=[cvn])
                        kb.op("dve", lambda e, ps=ps, cv=cv, cc=cc: e.scalar_tensor_tensor(cv[:], ps[:, 2:2 + TILE], cw[:, 2, cc:cc + 1], cv[:], ALU.mult, ALU.add),
                              reads=[pn, "mx_cw", cvn], writes=[cvn])
                        if pend is not None:
                            pend()

                        def pend(cv=cv, cc=cc, cvn=cvn):
                            kb.op("act", lambda e: e.activation(out=xbcT[:, cc, :], in_=cv[:], func=AF.Silu), reads=[cvn], writes=["mx_xbcT"])
                pend()
                kb.dma("sp", self.XB[ti], xbcT[:].rearrange("p a b -> p (a b)"), reads=["mx_xbcT"], writes=[("XB", ti)])
                if tix + 1 < len(tiles):
                    nx = hset((tix + 1) % nset)
                    self.make_hT(srcT, T, tiles[tix + 1] * TILE, WIN, 1, res, resn, who, 0, nx[4], nx[5], nx[6])
                for c in chunks:
                    cl = slice(c * 128, (c + 1) * 128)
                    cg = ti * 2 + c
                    for half in range(2):
                        bt, btn = self.next_pab()
                        btb = bt[:].bitcast(BF16)
                        for j in range(8):
                            cc = half * 8 + j
                            kb.op("pe", lambda e, cc=cc, j=j: e.transpose(btb[:, j * 128:(j + 1) * 128], xbcT[:, cc, cl], self.identb[:]),
                                  reads=["mx_xbcT", "identb"], writes=[btn])
                        kb.op("act", lambda e: e.copy(xs_tm[c][:, half * 1024:(half + 1) * 1024], btb), reads=[btn], writes=[f"mx_xs{c}"])
                    bt, btn = self.next_pab()
                    btb = bt[:].bitcast(BF16)
                    for j in range(4):
                        kb.op("pe", lambda e, j=j: e.transpose(btb[:, j * 128:(j + 1) * 128], xbcT[:, 16 + j, cl], self.identb[:]),
                              reads=["mx_xbcT", "identb"], writes=[btn])
                    kb.op("act", lambda e: e.copy(B_tm[c][:], btb[:, 0:512]), reads=[btn], writes=[f"mx_B{c}"])
                    kb.dma("sp", self.XS[cg], xs_tm[c][:], reads=[f"mx_xs{c}"], writes=[("XS", cg)])
                    kb.dma("sp", self.BS[cg], B_tm[c][:], reads=[f"mx_B{c}"], writes=[("BS", cg)])
            for c in chunks:
                cl = slice(c * 128, (c + 1) * 128)
                tok0 = t0 + c * 128
                dsl = slice(d * NH, (d + 1) * NH)
                slot = cslot[c]
                kb.op("dve", lambda e, c=c, dsl=dsl: e.tensor_tensor(xdt[:].rearrange("p (h q) -> p h q", q=64), xs_tm[c][:].rearrange("p (h q) -> p h q", q=64),
                                                                     bc(dt_tm[c][:, dsl].unsqueeze(2), [128, NH, 64]), ALU.mult),
                      reads=[f"mx_xs{c}", f"mx_dt_tm{c}"], writes=["mx_xdt"])
                kb.op("pool", lambda e, c=c: e.tensor_tensor(xwt[:].rearrange("p (h q) -> p h q", q=64), xs_tm[c][:].rearrange("p (h q) -> p h q", q=64),
                                                             bc(w_sb[c][:].unsqueeze(2), [128, NH, 64]), ALU.mult),
                      reads=[f"mx_xs{c}", f"mx_w{c}"], writes=["mx_xwt"])
                for g in range(NG):
                    kb.op("pe", lambda e, g=g, cl=cl: e.matmul(self.pS[:, g * 128:(g + 1) * 128], xbcT[:, 16 + g, cl], xbcT[:, 20 + g, cl], start=True, stop=True),
                          reads=["mx_xbcT"], writes=["pS"])
                kb.op("dve", lambda e: e.tensor_tensor(scm[:], self.pS[:].rearrange("p (g l) -> p g l", g=4), bc(tri.unsqueeze(1), [128, 4, 128]), ALU.mult),
                      reads=["pS", "consts"], writes=["mx_scm"])
                Y = Yc[c]; Yn = f"mx_Yc{c}"
                Yg = [f"{Yn}g{g}" for g in range(NG)]
                if d == 0:
                    kb.dma("sp", Y[:], self.YB[tok0:tok0 + 128, :], reads=[("YB", tok0 // 128)], writes=Yg)
                    kb.op("pool", lambda e, c=c: e.tensor_tensor(ysq[:].rearrange("p (h q) -> p h q", q=64), xs_tm[c][:].rearrange("p (h q) -> p h q", q=64),
                                                                 bc(Dbc[:].unsqueeze(2), [128, NH, 64]), ALU.mult),
                          reads=[f"mx_xs{c}", "mx_Dbc"], writes=["mx_ysq"])
                    kb.op("pool", lambda e, Y=Y: e.tensor_tensor(Y[:], Y[:], ysq[:], ALU.add), reads=Yg + ["mx_ysq"], writes=Yg)
                for g in range(NG):
                    kb.dma("sp", csb[g][:].rearrange("p h l -> p (h l)"), self.csrow[slot, g * 1024:(g + 1) * 1024].partition_broadcast(128),
                           reads=[f"csrow{slot}"], writes=[f"mx_csb{g}"])
                for g in range(NG):
                    kb.op("dve" if g < 2 else "pool", lambda e, c=c, g=g: e.tensor_tensor(csb[g][:], csb[g][:], bc(cs_sb[c][:, g * 8:(g + 1) * 8].unsqueeze(2), [128, 8, 128]), ALU.subtract),
                          reads=[f"mx_csb{g}", f"mx_cs{c}"], writes=[f"mx_csb{g}"])
                for g in range(NG):
                    kb.op("act", lambda e, g=g: e.activation(out=csb[g][:], in_=csb[g][:], func=AF.Exp), reads=[f"mx_csb{g}"], writes=[f"mx_csb{g}"])
                for g in range(NG):
                    gs_ = slice(g * 512, (g + 1) * 512)
                    kb.op("pool", lambda e, g=g, gs_=gs_: e.tensor_tensor(S[:, gs_].rearrange("p (h q) -> p h q", q=64), S[:, gs_].rearrange("p (h q) -> p h q", q=64),
                                                                     bc(cd_sb[c][:, g * 8:(g + 1) * 8].unsqueeze(2), [128, 8, 64]), ALU.mult),
                          reads=[f"{Sn}{g}", f"mx_ecs{c}"], writes=[f"{Sn}{g}"])
                for g in range(NG):
                    kb.op("dve", lambda e, g=g: e.scalar_tensor_tensor(MT[g][:], csb[g][:], 1.0, bc(scm[:, g, :].unsqueeze(1), [128, 8, 128]), ALU.min, ALU.mult),
                          reads=[f"mx_csb{g}", "mx_scm"], writes=[f"mx_MT{g}"])

                def s2(g):
                    yd, ydn = self.next_pab(); yo, yon = self.next_pab(); stb, stn = self.next_pab()
                    gs = slice(g * 512, (g + 1) * 512)
                    for h in range(8):
                        hh = g * 8 + h
                        kb.op("pe", lambda e, g=g, h=h, hh=hh, yd=yd: e.matmul(yd[:, h * 64:(h + 1) * 64], MT[g][:, h, :], xdt[:, hh * 64:(hh + 1) * 64], start=True, stop=True),
                              reads=[f"mx_MT{g}", "mx_xdt"], writes=[ydn])
                    kb.op("pe", lambda e, g=g, yo=yo, gs=gs: e.matmul(yo[:], xbcT[:, 20 + g, cl], Sbf[:, gs], start=True, stop=True),
                          reads=["mx_xbcT", f"mx_Sbf{g}"], writes=[yon])
                    kb.op("pe", lambda e, g=g, stb=stb, gs=gs: e.matmul(stb[:], B_tm[c][:, g * 128:(g + 1) * 128], xwt[:, gs], start=True, stop=True),
                          reads=[f"mx_B{c}", "mx_xwt"], writes=[stn])
                    return (yd, ydn, yo, yon, stb, stn)

                def s3(g, bk):
                    yd, ydn, yo, yon, stb, stn = bk
                    gs = slice(g * 512, (g + 1) * 512)
                    yt = ytmp[g]; ytn = f"mx_ytmp{g}"
                    kb.op("dve", lambda e: e.tensor_tensor(yt[:].rearrange("p (h q) -> p h q", q=64), yo[:].rearrange("p (h q) -> p h q", q=64),
                                                           bc(ecs_sb[c][:, g * 8:(g + 1) * 8].unsqueeze(2), [128, 8, 64]), ALU.mult),
                          reads=[yon, f"mx_ecs{c}"], writes=[ytn])
                    if d == 1:
                        kb.op("dve", lambda e: e.tensor_tensor(Y[:, gs], yd[:], yt[:], ALU.add), reads=[ydn, ytn], writes=[Yg[g]])
                    else:
                        kb.op("dve", lambda e: e.tensor_tensor(yt[:], yd[:], yt[:], ALU.add), reads=[ydn, ytn], writes=[ytn])
                        kb.op("pool", lambda e: e.tensor_tensor(Y[:, gs], Y[:, gs], yt[:], ALU.add), reads=[Yg[g], ytn], writes=[Yg[g]])
                    kb.op("dve", lambda e: e.tensor_tensor(S[:, gs], S[:, gs], stb[:], ALU.add), reads=[f"{Sn}{g}", stn], writes=[f"{Sn}{g}"])
                    kb.op("act", lambda e: e.copy(Sbf[:, gs], S[:, gs]), reads=[f"{Sn}{g}"], writes=[f"mx_Sbf{g}"])

                bks = {}
                bks[0] = s2(0)
                bks[1] = s2(1)
                s3(0, bks[0])
                bks[2] = s2(2)
                s3(1, bks[1])
                bks[3] = s2(3)
                s3(2, bks[2])
                s3(3, bks[3])
                if d == 1:
                    kb.dma("sp", self.YB[tok0:tok0 + 128, :], Y[:], reads=Yg, writes=[("YB", tok0 // 128)])
            if d == 1:
                if tix == len(tiles) - 1:
                    kb.barrier()
                    sA.close()
                continue
            for blk in range(4):
                wb, wn = self.load_w(w_in, C_Z + blk * 512, 512, key=("in", i, C_Z + blk * 512))
                for c in chunks:
                    ps, pn = self.next_pab()
                    for kc in range(8):
                        kb.op("pe", lambda e, ps=ps, wb=wb, kc=kc, c=c: e.matmul(ps[:], hT[:, kc, 1 + c * 128:1 + (c + 1) * 128], wb[:, kc, :], start=(kc == 0), stop=(kc == 7)),
                              reads=[wn, hTn], writes=[pn])
                    zk = (blk * 2 + c) % 2
                    kb.op("act", lambda e, ps=ps, zk=zk: e.activation(out=zs[zk][:], in_=ps[:], func=AF.Silu), reads=[pn], writes=[f"mx_zs{zk}"])
                    bs = slice(blk * 512, (blk + 1) * 512)
                    kb.op("dve", lambda e, c=c, bs=bs, zk=zk: e.tensor_tensor(Yc[c][:, bs], Yc[c][:, bs], zs[zk][:], ALU.mult), reads=[f"mx_Yc{c}g{blk}", f"mx_zs{zk}"], writes=[f"mx_Yc{c}g{blk}"])
            for c in chunks:
                Y = Yc[c]; Yn = f"mx_Yc{c}"
                Yg = [f"{Yn}g{g}" for g in range(NG)]
                kb.op("act", lambda e, Y=Y: e.activation(out=ysq[:], in_=Y[:], func=AF.Square, accum_out=ss[:, 0:1]), reads=Yg, writes=["mx_ysq", "mx_ss"])
                kb.op("act", lambda e: e.activation(out=ss[:, 1:2], in_=ss[:, 0:1], func=AF.Sqrt, bias=self.eps_c, scale=1.0 / INNER), reads=["mx_ss", "cst"], writes=["mx_ss"])
                kb.op("dve", lambda e: e.reciprocal(ss[:, 1:2], ss[:, 1:2]), reads=["mx_ss"], writes=["mx_ss"])
                kb.op("dve", lambda e, Y=Y: e.scalar_tensor_tensor(yn_tm[:], Y[:], ss[:, 1:2], gbc[:], ALU.mult, ALU.mult), reads=Yg + ["mx_ss", "mx_gbc"], writes=["mx_yn"])
                for half in range(2):
                    for j in range(8):
                        kc = half * 8 + j
                        kb.op("pe", lambda e, j=j, kc=kc: e.transpose(self.pTb[:, j * 128:(j + 1) * 128], yn_tm[:, kc * 128:(kc + 1) * 128], self.identb[:]),
                              reads=["mx_yn", "identb"], writes=["pT"])
                    kb.op("act", lambda e, c=c, half=half: e.copy(ynT[:, half * 8:(half + 1) * 8, c * 128:(c + 1) * 128], self.pTb[:].rearrange("p (j t) -> p j t", j=8)),
                          reads=["pT"], writes=["mx_ynT"])
            kb.barrier()
            sA.close()
            sC = contextlib.ExitStack()
            gbs = sbt("mx_gbs", [128, 8, TILE], F32, sC); gcs = sbt("mx_gcs", [128, 8, TILE], F32, sC); uu = sbt("mx_u", [128, 8, TILE], F32, sC)
            vv = gcs
            svT = sbt("mx_svT", [128, 8, TILE], BF16, sC)
            gT = sbt("mx_gT", [128, 16, TILE], F32, sC)
            t1s = [sbt(f"mx_t1{k}", [128, TILE], F32, sC) for k in range(2)]; t2s = [sbt(f"mx_t2{k}", [128, TILE], F32, sC) for k in range(2)]
            mT = sbt("mx_mT", [128, 8, TILE], BF16, sC)
            xot = sbt("mx_xo", [128, 8, TILE], F32, sC); xon = "mx_xo"
            for blk in range(6):
                wb, wn = self.load_w(w_in, C_SC + blk * 512, 512, key=("in", i, C_SC + blk * 512))
                for j in range(4):
                    cc = blk * 4 + j
                    kind, f = cc // 8, cc % 8
                    ps, pn = self.next_pab()
                    for kc in range(8):
                        kb.op("pe", lambda e, ps=ps, wb=wb, j=j, kc=kc: e.matmul(ps[:, 0:TILE], wb[:, kc, j * 128:(j + 1) * 128], hT[:, kc, 1:1 + TILE], start=(kc == 0), stop=(kc == 7)),
                              reads=[wn, hTn], writes=[pn])
                    if kind == 0:
                        kb.op("act", lambda e, ps=ps, f=f: e.copy(gbs[:, f, :], ps[:, 0:TILE]), reads=[pn], writes=["mx_gbs"])
                    elif kind == 1:
                        kb.op("act", lambda e, ps=ps, f=f: e.copy(gcs[:, f, :], ps[:, 0:TILE]), reads=[pn], writes=["mx_gcs"])
                    else:
                        kb.op("dve", lambda e, ps=ps, f=f: e.tensor_tensor(uu[:, f, :], gcs[:, f, :], ps[:, 0:TILE], ALU.mult), reads=[pn, "mx_gcs"], writes=["mx_u"])
            rows = TILE // grid
            for f in range(8):
                u3 = uu[:, f, :].rearrange("p (r w) -> p r w", w=grid)
                v3 = vv[:, f, :].rearrange("p (r w) -> p r w", w=grid)
                kb.op("act", lambda e, f=f: e.activation(out=vv[:, f, :], in_=uu[:, f, :], func=AF.Identity, scale=scw[:, 1, f:f + 1]), reads=["mx_u", "mx_scw"], writes=["mx_gcs"])
                kb.op("dve", lambda e, f=f, u3=u3, v3=v3: e.scalar_tensor_tensor(v3[:, :, 1:grid], u3[:, :, 0:grid - 1], scw[:, 0, f:f + 1], v3[:, :, 1:grid], ALU.mult, ALU.add),
                      reads=["mx_u", "mx_scw", "mx_gcs"], writes=["mx_gcs"])
                kb.op("dve", lambda e, f=f, u3=u3, v3=v3: e.scalar_tensor_tensor(v3[:, :, 0:grid - 1], u3[:, :, 1:grid], scw[:, 2, f:f + 1], v3[:, :, 0:grid - 1], ALU.mult, ALU.add),
                      reads=["mx_u", "mx_scw", "mx_gcs"], writes=["mx_gcs"])
                kb.op("pool", lambda e, f=f: e.tensor_tensor(svT[:, f, :], gbs[:, f, :], vv[:, f, :], ALU.mult), reads=["mx_gbs", "mx_gcs"], writes=["mx_svT"])
            for blk in range(4):
                wb, wn = self.load_w(w_in, C_GL + blk * 512, 512, key=("in", i, C_GL + blk * 512))
                for j in range(4):
                    cc = blk * 4 + j
                    ps, pn = self.next_pab()
                    for kc in range(8):
                        kb.op("pe", lambda e, ps=ps, wb=wb, j=j, kc=kc: e.matmul(ps[:, 0:TILE], wb[:, kc, j * 128:(j + 1) * 128], hT[:, kc, 1:1 + TILE], start=(kc == 0), stop=(kc == 7)),
                              reads=[wn, hTn], writes=[pn])
                    kb.op("act", lambda e, ps=ps, cc=cc: e.activation(out=gT[:, cc, :], in_=ps[:, 0:TILE], func=AF.Sigmoid, bias=bgate[:, cc:cc + 1]), reads=[pn, "mx_bg"], writes=["mx_gT"])
            for ob in range(4):
                wso, wson = self.load_w(self.w_ssd_out[i], ob * 256, 256, 0, 16, key=("so", i, ob))
                wsc, wscn = self.load_w(self.w_sc_out[i], ob * 256, 256, 0, 8, key=("sc", i, ob))
                for j in range(2):
                    fo = ob * 2 + j
                    for kc in range(16):
                        kb.op("pe", lambda e, wso=wso, j=j, kc=kc: e.matmul(self.pA[:, 0:TILE], wso[:, kc, j * 128:(j + 1) * 128], ynT[:, kc, :], start=(kc == 0), stop=(kc == 15)),
                              reads=[wson, "mx_ynT"], writes=["pA"])
                    for kc in range(8):
                        kb.op("pe", lambda e, wsc=wsc, j=j, kc=kc: e.matmul(self.pB[:, 0:TILE], wsc[:, kc, j * 128:(j + 1) * 128], svT[:, kc, :], start=(kc == 0), stop=(kc == 7)),
                              reads=[wscn, "mx_svT"], writes=["pB"])
                    t1 = t1s[fo % 2]; t2 = t2s[fo % 2]
                    kb.op("dve", lambda e, fo=fo: e.tensor_tensor(t1[:], gT[:, fo, :], self.pA[:, 0:TILE], ALU.mult), reads=["mx_gT", "pA"], writes=[f"mx_t1{fo % 2}"])
                    kb.op("dve", lambda e, fo=fo: e.tensor_tensor(t2[:], gT[:, 8 + fo, :], self.pB[:, 0:TILE], ALU.mult), reads=["mx_gT", "pB"], writes=[f"mx_t2{fo % 2}"])
                    kb.op("pool", lambda e, fo=fo: e.tensor_tensor(mT[:, fo, :], t1[:], t2[:], ALU.add), reads=[f"mx_t1{fo % 2}", f"mx_t2{fo % 2}"], writes=["mx_mT"])
            for ob in range(2):
                wo, won = self.load_w(self.w_o[i], ob * 512, 512, 0, 8, key=("wo", i, ob))
                for j in range(4):
                    fo = ob * 4 + j
                    ps, pn = self.next_pab()
                    for kc in range(8):
                        kb.op("pe", lambda e, ps=ps, wo=wo, j=j, kc=kc: e.matmul(ps[:, 0:TILE], wo[:, kc, j * 128:(j + 1) * 128], mT[:, kc, :], start=(kc == 0), stop=(kc == 7)),
                              reads=[won, "mx_mT"], writes=[pn])
                    kb.op("dve", lambda e, ps=ps, fo=fo, xot=xot: e.scalar_tensor_tensor(xot[:, fo, :], ps[:, 0:TILE], res[:, who, 2, fo:fo + 1], xw[:, fo, 1:1 + TILE], ALU.mult, ALU.add),
                          reads=[pn, resn, xwn], writes=[xon])
            if write_out:
                kb.dma("sp", dstT.rearrange("(kc p) t -> p kc t", p=128)[:, :, t0:t0 + TILE], xot[:], reads=[xon], writes=[("dram", id(dstT))])
            kb.barrier()
            sC.close()
        kb.barrier()
        st.close()

    def ffn(self, i, srcT, dstT, T, who, res, resn, moe):
        kb = self.kb
        st = contextlib.ExitStack()
        sbt = lambda n, s, dt=F32: kb.sb(n, s, dt, st)
        TS = min(T, 1024)
        self.rot = self.rot2
        self.set_wbufs(6, st)
        NE = NEXP if moe else 1
        HID = FFN_EXP if moe else FFN_DENSE
        blocks = [(b0, min(512, HID - b0)) for b0 in range(0, HID, 512)]
        xs = sbt("ff_x", [128, 8, TS])
        h2 = sbt("ff_h2", [128, 8, TS], BF16)
        acc = sbt("ff_acc", [128, 8, TS])
        sq, rs = sbt("ff_sq", [128, 8, 128]), sbt("ff_rs", [128, 128])
        hf = sbt("ff_hf", [128, 8, 128])
        tmp = None
        sa = [sbt(f"ff_sa{k}", [128, TILE]) for k in range(3)]; tt = [sbt(f"ff_tt{k}", [128, TILE]) for k in range(3)]
        hid = [sbt(f"ff_hid{k}", [128, 4, TILE], BF16) for k in range(2)]
        pacc = self.pS
        pbanks = [(self.pS, "pS"), (self.pYd, "pYd"), (self.pYo, "pYo"), (self.pSt, "pSt")]
        if moe:
            rw = sbt("ff_rw", [128, 8, NEXP])
            kb.dma("sp", rw[:], self.router_w[0].rearrange("(kc p) n -> p kc n", p=128), writes=["ff_rw"])
            lg = sbt("ff_lg", [128, NEXP]); l2 = sbt("ff_l2", [128, NEXP]); m1 = sbt("ff_m1", [128, 4])
            mk1 = sbt("ff_mk1", [128, NEXP]); mk2 = sbt("ff_mk2", [128, NEXP]); gt = sbt("ff_gt", [128, NEXP])
            dg = sbt("ff_dg", [128, NEXP, 128])
            gbc = sbt("ff_gbc", [128, NEXP, TS])
        for s0 in range(0, T, TS):
            src = srcT.rearrange("(kc p) t -> p kc t", p=128)
            kb.dma("sp", xs[:], src[:, :, s0:s0 + TS], reads=[("dram", id(srcT))], writes=["ff_x"])
            for q in range(TS // 128):
                ql = slice(q * 128, (q + 1) * 128)
                kb.op("act", lambda e, ql=ql: e.activation(out=sq[:], in_=xs[:, :, ql], func=AF.Square), reads=["ff_x"], writes=[f"ff_sq{k}" for k in range(8)])
                for kc in range(8):
                    kb.op("pe", lambda e, kc=kc: e.matmul(self.pM[:, 0:128], self.ones, sq[:, kc, :], start=(kc == 0), stop=(kc == 7)), reads=[f"ff_sq{kc}", "consts"], writes=["pM"])
                kb.op("act", lambda e: e.activation(out=rs[:], in_=self.pM[:, 0:128], func=AF.Sqrt, bias=self.eps_c, scale=1.0 / D), reads=["pM", "cst"], writes=["ff_rs"])
                kb.op("dve", lambda e: e.reciprocal(rs[:], rs[:]), reads=["ff_rs"], writes=["ff_rs"])
                for kc in range(8):
                    kb.op("dve", lambda e, kc=kc, ql=ql: e.tensor_tensor(sq[:, kc, :], xs[:, kc, ql], rs[:], ALU.mult), reads=["ff_x", "ff_rs"], writes=[f"ff_sq{kc}"])
                    kb.op("act", lambda e, kc=kc: e.activation(out=hf[:, kc, :], in_=sq[:, kc, :], func=AF.Identity, bias=res[:, who, 4, kc:kc + 1], scale=res[:, who, 3, kc:kc + 1]),
                          reads=[f"ff_sq{kc}", resn], writes=[f"ff_hf{kc}"])
                kb.op("pool", lambda e, ql=ql: e.tensor_copy(h2[:, :, ql], hf[:]), reads=[f"ff_hf{k}" for k in range(8)], writes=["ff_h2"])
                if moe:
                    rwf = sbt("ff_rwf", [128, 8, NEXP]) if False else None
                    for kc in range(8):
                        kb.op("pe", lambda e, kc=kc: e.matmul(self.pM[:, 256:256 + NEXP], hf[:, kc, :], rw[:, kc, :], start=(kc == 0), stop=(kc == 7)), reads=[f"ff_hf{kc}", "ff_rw"], writes=["pM"])
                    kb.op("act", lambda e: e.copy(lg[:], self.pM[:, 256:256 + NEXP]), reads=["pM"], writes=["ff_lg"])
                    kb.op("dve", lambda e: e.tensor_reduce(m1[:, 0:1], lg[:], mybir.AxisListType.X, ALU.max), reads=["ff_lg"], writes=["ff_m1"])
                    kb.op("dve", lambda e: e.tensor_tensor(mk1[:], lg[:], bc(m1[:, 0:1], [128, NEXP]), ALU.is_equal), reads=["ff_lg", "ff_m1"], writes=["ff_mk1"])
                    kb.op("dve", lambda e: e.scalar_tensor_tensor(l2[:], mk1[:], -1e30, lg[:], ALU.mult, ALU.add), reads=["ff_mk1", "ff_lg"], writes=["ff_l2"])
                    kb.op("dve", lambda e: e.tensor_reduce(m1[:, 1:2], l2[:], mybir.AxisListType.X, ALU.max), reads=["ff_l2"], writes=["ff_m1"])
                    kb.op("dve", lambda e: e.tensor_tensor(mk2[:], l2[:], bc(m1[:, 1:2], [128, NEXP]), ALU.is_equal), reads=["ff_l2", "ff_m1"], writes=["ff_mk2"])
                    kb.op("dve", lambda e: e.tensor_tensor(m1[:, 2:3], m1[:, 1:2], m1[:, 0:1], ALU.subtract), reads=["ff_m1"], writes=["ff_m1"])
                    kb.op("act", lambda e: e.activation(out=m1[:, 2:3], in_=m1[:, 2:3], func=AF.Exp), reads=["ff_m1"], writes=["ff_m1"])
                    kb.op("dve", lambda e: e.tensor_scalar(m1[:, 2:3], m1[:, 2:3], 1.0, None, ALU.add), reads=["ff_m1"], writes=["ff_m1"])
                    kb.op("dve", lambda e: e.reciprocal(m1[:, 2:3], m1[:, 2:3]), reads=["ff_m1"], writes=["ff_m1"])
                    kb.op("dve", lambda e: e.tensor_scalar(m1[:, 3:4], m1[:, 2:3], -1.0, 1.0, ALU.mult, ALU.add), reads=["ff_m1"], writes=["ff_m1"])
                    kb.op("dve", lambda e: e.tensor_scalar(gt[:], mk1[:], m1[:, 2:3], None, ALU.mult), reads=["ff_mk1", "ff_m1"], writes=["ff_gt"])
                    kb.op("dve", lambda e: e.scalar_tensor_tensor(gt[:], mk2[:], m1[:, 3:4], gt[:], ALU.mult, ALU.add), reads=["ff_mk2", "ff_m1", "ff_gt"], writes=["ff_gt"])
                    kb.op("dve", lambda e: e.tensor_tensor(dg[:], bc(self.ident.unsqueeze(1), [128, NEXP, 128]), bc(gt[:].unsqueeze(2), [128, NEXP, 128]), ALU.mult),
                          reads=["consts", "ff_gt"], writes=["ff_dg"])
                    for hh in range(2):
                        ps, pn = self.next_pab()
                        kb.op("pe", lambda e, ps=ps, hh=hh: e.matmul(ps[:], self.ones, dg[:, hh * 4:(hh + 1) * 4, :].rearrange("p a b -> p (a b)"), start=True, stop=True),
                              reads=["ff_dg", "consts"], writes=[pn])
                        kb.op("act", lambda e, ps=ps, hh=hh, ql=ql: e.copy(gbc[:, hh * 4:(hh + 1) * 4, ql], ps[:].rearrange("p (a b) -> p a b", a=4)), reads=[pn], writes=["ff_gbc"])
            kb.barrier()
            items = []
            for ex in range(NE):
                for bi, (b0, bn) in enumerate(blocks):
                    for tq in range(TS // TILE):
                        items.append((ex, bi, b0, bn, tq))
            wcache = {}
            slots = [(self.pA, "pA"), (self.pB, "pB"), (self.pM, "pM"), (self.pT, "pT")]
            self.ffs = getattr(self, "ffs", 0)

            def slot():
                self.ffs = (self.ffs + 1) % len(slots)
                t, n = slots[self.ffs]
                return t[:, 0:TILE], n

            def Wsrc(ex):
                if moe:
                    return self.moe_w1[0, ex], self.moe_w3[0, ex], self.moe_w2[0, ex]
                return self.ffn_w1[0], self.ffn_w3[0], self.ffn_w2[0]

            def AB(n):
                ex, bi, b0, bn, tq = items[n]
                nh = bn // 128
                W1, W3, W2 = Wsrc(ex)
                if (ex, bi, 1) not in wcache:
                    wcache[(ex, bi, 1)] = self.load_w(W1, b0, bn)
                    wcache[(ex, bi, 3)] = self.load_w(W3, b0, bn)
                ntq = TS // TILE
                if tq == min(1, ntq - 1) and n + ntq - tq < len(items):
                    ex2, bi2, b02, bn2, _ = items[n + ntq - tq]
                    if (ex2, bi2, 1) not in wcache:
                        W1b, W3b, _w = Wsrc(ex2)
                        wcache[(ex2, bi2, 1)] = self.load_w(W1b, b02, bn2)
                        wcache[(ex2, bi2, 3)] = self.load_w(W3b, b02, bn2)
                w1, w1n = wcache[(ex, bi, 1)]
                w3, w3n = wcache[(ex, bi, 3)]
                tl = slice(tq * TILE, (tq + 1) * TILE)
                hd = hid[n % 2]; hdn = f"ff_hid{n % 2}"
                for hc in range(nh):
                    pa, pan = slot()
                    pb_, pbn_ = slot()
                    for kc in range(8):
                        kb.op("pe", lambda e, kc=kc: e.matmul(pa, w1[:, kc, hc * 128:(hc + 1) * 128], h2[:, kc, tl], start=(kc == 0), stop=(kc == 7)),
                              reads=[w1n, "ff_h2"], writes=[pan])
                    for kc in range(8):
                        kb.op("pe", lambda e, kc=kc: e.matmul(pb_, w3[:, kc, hc * 128:(hc + 1) * 128], h2[:, kc, tl], start=(kc == 0), stop=(kc == 7)),
                              reads=[w3n, "ff_h2"], writes=[pbn_])
                    k3 = (n * 4 + hc) % 3
                    kb.op("act", lambda e: e.activation(out=sa[k3][:], in_=pa, func=AF.Silu), reads=[pan], writes=[f"ff_sa{k3}"])
                    if moe:
                        kb.op("dve", lambda e: e.tensor_tensor(tt[k3][:], sa[k3][:], pb_, ALU.mult), reads=[f"ff_sa{k3}", pbn_], writes=[f"ff_tt{k3}"])
                        kb.op("dve", lambda e: e.tensor_tensor(hd[:, hc, :], tt[k3][:], gbc[:, ex, tl], ALU.mult), reads=[f"ff_tt{k3}", "ff_gbc"], writes=[hdn])
                    else:
                        kb.op("dve", lambda e: e.tensor_tensor(hd[:, hc, :], sa[k3][:], pb_, ALU.mult), reads=[f"ff_sa{k3}", pbn_], writes=[hdn])

            def W2s(n):
                ex, bi, b0, bn, tq = items[n]
                nh = bn // 128
                W1, W3, W2 = Wsrc(ex)
                if (ex, bi, 2) not in wcache:
                    wcache[(ex, bi, 2)] = self.load_w_rows(W2, b0 // 128, nh)
                w2, w2n = wcache[(ex, bi, 2)]
                tl = slice(tq * TILE, (tq + 1) * TILE)
                hd = hid[n % 2]; hdn = f"ff_hid{n % 2}"
                for fo in range(8):
                    pb, pbn = pbanks[fo // 2]
                    osl = slice((fo % 2) * TILE, (fo % 2 + 1) * TILE)
                    for hc in range(nh):
                        kb.op("pe", lambda e, hc=hc: e.matmul(pb[:, osl], w2[:, hc, fo * 128:(fo + 1) * 128], hd[:, hc, :], start=(hc == 0), stop=(hc == nh - 1)),
                              reads=[w2n, hdn], writes=[pbn])
                first = (ex == 0 and bi == 0)
                for k4 in range(4):
                    pb, pbn = pbanks[k4]
                    a_v = acc[:, 2 * k4:2 * k4 + 2, tl]
                    p_v = pb[:].rearrange("p (a t) -> p a t", a=2)
                    if first:
                        kb.op("act", lambda e: e.copy(a_v, p_v), reads=[pbn], writes=[f"ff_acc{tq}"])
                    else:
                        kb.op("dve", lambda e: e.tensor_tensor(a_v, a_v, p_v, ALU.add), reads=[pbn, f"ff_acc{tq}"], writes=[f"ff_acc{tq}"])

            AB(0)
            for n in range(1, len(items)):
                AB(n)
                W2s(n - 1)
            W2s(len(items) - 1)
            accn = [f"ff_acc{tq}" for tq in range(TS // TILE)]
            for fo in range(8):
                kb.op("dve", lambda e, fo=fo: e.scalar_tensor_tensor(acc[:, fo, :], acc[:, fo, :], res[:, who, 5, fo:fo + 1], xs[:, fo, :], ALU.mult, ALU.add),
                      reads=accn + [resn, "ff_x"], writes=accn)
            kb.dma("sp", dstT.rearrange("(kc p) t -> p kc t", p=128)[:, :, s0:s0 + TS], acc[:], reads=accn, writes=[("dram", id(dstT))])
            kb.barrier()
        kb.barrier()
        st.close()

    def load_w_rows(self, w2d, r0, nk):
        kb = self.kb
        slot = self.wrr % self.NWB
        self.wrr += 1
        buf = self.wbuf[slot]
        v = buf[:, 0:nk * 1024].rearrange("p (k n) -> p k n", n=1024)
        src = w2d.rearrange("(kc p) n -> p kc n", p=128)
        kb.dma("pool", v, src[:, r0:r0 + nk, :], writes=[f"wbuf{slot}"])
        return v, f"wbuf{slot}"

    def final(self, srcT):
        kb = self.kb
        st = contextlib.ExitStack()
        sbt = lambda n, s, dt=F32: kb.sb(n, s, dt, st)
        fg = self.colvec("fn_g", self.final_g, 8, st)
        xw = [sbt(f"fn_x{k}", [128, 8, 128]) for k in range(2)]
        sq, rs = sbt("fn_sq", [128, 8, 128]), sbt("fn_rs", [128, 128])
        ot = [sbt(f"fn_o{k}", [128, D]) for k in range(2)]
        src = srcT.rearrange("(kc p) t -> p kc t", p=128)
        for t in range(SEQ // 128):
            x_, xn = xw[t % 2], f"fn_x{t % 2}"
            o_, on = ot[t % 2], f"fn_o{t % 2}"
            kb.dma("sp", x_[:], src[:, :, t * 128:(t + 1) * 128], reads=[("dram", id(srcT))], writes=[xn])
            kb.op("act", lambda e, x_=x_: e.activation(out=sq[:], in_=x_[:], func=AF.Square), reads=[xn], writes=[f"fn_sq{k}" for k in range(8)])
            for kc in range(8):
                kb.op("pe", lambda e, kc=kc: e.matmul(self.pM[:, 0:128], self.ones, sq[:, kc, :], start=(kc == 0), stop=(kc == 7)), reads=[f"fn_sq{kc}", "consts"], writes=["pM"])
            kb.op("act", lambda e: e.activation(out=rs[:], in_=self.pM[:, 0:128], func=AF.Sqrt, bias=self.eps_c, scale=1.0 / D), reads=["pM", "cst"], writes=["fn_rs"])
            kb.op("dve", lambda e: e.reciprocal(rs[:], rs[:]), reads=["fn_rs"], writes=["fn_rs"])
            for kc in range(8):
                kb.op("dve", lambda e, kc=kc, x_=x_: e.scalar_tensor_tensor(sq[:, kc, :], x_[:, kc, :], fg[:, kc:kc + 1], rs[:], ALU.mult, ALU.mult), reads=[xn, "fn_g", "fn_rs"], writes=[f"fn_sq{kc}"])
            for h in range(2):
                ps, pn = self.next_pab()
                for j in range(4):
                    kc = h * 4 + j
                    kb.op("pe", lambda e, ps=ps, j=j, kc=kc: e.transpose(ps[:, j * 128:(j + 1) * 128], sq[:, kc, :], self.ident), reads=[f"fn_sq{kc}", "consts"], writes=[pn])
                kb.op("act", lambda e, ps=ps, h=h, o_=o_: e.copy(o_[:, h * 512:(h + 1) * 512], ps[:]), reads=[pn], writes=[on])
            kb.dma("sp", self.out[t * 128:(t + 1) * 128, :], o_[:], reads=[on], writes=["out"])
        kb.barrier()
        st.close()

    def build(self):
        kb = self.kb
        self.to_fm(self.x, self.xT[0], SEQ)
        self.to_fm(self.ctx, self.cT[0], CTX)
        xa, xb = self.xT
        ca, cb_ = self.cT
        Sf = kb.sb("S_f", [128, INNER]); Sb = kb.sb("S_b", [128, INNER])
        for i in range(DEPTH):
            last = i == DEPTH - 1
            st = contextlib.ExitStack()
            res, resn = self.mod_vectors(i, st)
            kb.barrier()
            st.close()
            kb.op("dve", lambda e: e.memset(Sf[:], 0.0), writes=[f"S_f{g}" for g in range(NG)])
            kb.op("dve", lambda e: e.memset(Sb[:], 0.0), writes=[f"S_b{g}" for g in range(NG)])
            self.mixer_pass(i, 1, ca, cb_, CTX, 1, res, resn, Sb, "S_b", CTX, last, False)
            self.mixer_pass(i, 0, ca, cb_, CTX, 1, res, resn, Sf, "S_f", CTX, last, not last)
            self.mixer_pass(i, 1, xa, xb, SEQ, 0, res, resn, Sb, "S_b", 64, last, False)
            self.mixer_pass(i, 0, xa, xb, SEQ, 0, res, resn, Sf, "S_f", 64, last, True)
            self.ffn(i, xb, xa, SEQ, 0, res, resn, moe=(i % 2 == 1))
            if not last:
                self.ffn(i, cb_, ca, CTX, 1, res, resn, moe=(i % 2 == 1))
        self.final(xa)
        return kb.finish()


def _consts():
    c = np.zeros((128, 512), np.float32)
    c[:, 0:128] = np.eye(128, dtype=np.float32)
    l = np.arange(128)
    c[:, 128:256] = (l[:, None] <= l[None, :]).astype(np.float32)
    c[:, 256:384] = (l[:, None] >= l[None, :]).astype(np.float32)
    c[:, 384:512] = 1.0
    return c


_NAMES = ["w_mod", "b_mod", "norm1_g", "norm2_g", "w_in", "b_gate", "ssd_conv_w", "ssd_conv_b", "ssd_dt_bias", "ssd_a_log",
          "ssd_d", "ssd_norm_g", "w_ssd_out", "sc_conv_w", "w_sc_out", "w_o", "ffn_w1", "ffn_w3", "ffn_w2", "router_w",
          "moe_w1", "moe_w3", "moe_w2", "final_g", "c_ctx"]


def kernel(**inputs):
    prog = Prog()
    nc = prog.build()
    shared = {n: np.ascontiguousarray(np.asarray(inputs[n], dtype=np.float32)) for n in _NAMES}
    shared["consts"] = _consts()
    x = np.asarray(inputs["x"], dtype=np.float32)
    c = np.asarray(inputs["c"], dtype=np.float32)
    ctx = np.asarray(inputs["ctx"], dtype=np.float32)
    in_maps = []
    for b in range(8):
        m = dict(shared)
        m["x"] = np.ascontiguousarray(x[b])
        m["c"] = np.ascontiguousarray(c[b])
        m["ctx"] = np.ascontiguousarray(ctx[b])
        in_maps.append(m)
    res = run_bass_kernel_spmd(nc, in_maps, core_ids=list(range(8)))
    return np.stack([np.asarray(r["out"]) for r in res.results], axis=0).astype(np.float32)
```

```python
import contextlib
import numpy as np
import concourse.bass as bass
import concourse.mybir as mybir
from concourse.bass_utils import run_bass_kernel_spmd

F32 = mybir.dt.float32
BF16 = mybir.dt.bfloat16
AF = mybir.ActivationFunctionType
ALU = mybir.AluOpType

D = 1024
SEQ = 4096
CTX = 256
DEPTH = 2
INNER = 2048
NH = 32
NG = 4
XBC = 3072
INW = 10304
C_Z, C_XBC, C_DT, C_SC, C_GL = 0, 2048, 5120, 5184, 8256
FFN_DENSE = 2816
FFN_EXP = 3584
NEXP = 8
EPS = 1e-6
TILE = 256
WIN = TILE + 2

NDS = 12
SAME_ENGINE_SYNC = True


class KB:
    def __init__(self):
        self.nc = bass.Bass("TRN2", target_bir_lowering=False)
        nc = self.nc
        self.es = contextlib.ExitStack()
        self.eng = {"pe": nc.tensor, "act": nc.scalar, "dve": nc.vector, "pool": nc.gpsimd, "sp": nc.sync}
        self.sem, self.cnt = {}, {}
        for e in self.eng:
            self.sem[e] = self.es.enter_context(nc.semaphore("s_" + e))
            self.cnt[e] = 0
        self.dsems, self.dcount, self.drr = {}, {}, {}
        self.semobj = {}
        for e, s in self.sem.items():
            self.semobj[("c", e)] = s
        for q in ("sp", "pool", "act"):
            self.dsems[q] = []
            for i in range(NDS):
                s = self.es.enter_context(nc.semaphore(f"d_{q}{i}"))
                self.dsems[q].append(("d", q, i))
                self.semobj[("d", q, i)] = s
                self.dcount[("d", q, i)] = 0
            self.drr[q] = 0
        self.waited, self.res_w, self.res_r = {}, {}, {}
        self.ninst = 0

    def sb(self, name, shape, dt=F32, stack=None):
        self.nuid = getattr(self, "nuid", 0) + 1
        return (stack or self.es).enter_context(self.nc.sbuf_tensor(f"{name}_u{self.nuid}", list(shape), dt))

    def ps(self, name, shape, dt=F32, stack=None):
        return (stack or self.es).enter_context(self.nc.psum_tensor(name, list(shape), dt))

    def dram(self, name, shape, dt=F32, kind="Internal"):
        return self.nc.dram_tensor(name, list(shape), dt, kind=kind).ap()

    def _wait(self, e, key, val):
        if key == ("c", e) and (e == "pe" or not SAME_ENGINE_SYNC):
            return
        k = (e, key)
        if self.waited.get(k, 0) >= val:
            return
        self.eng[e].wait_ge(self.semobj[key], val)
        self.waited[k] = val

    def _deps(self, reads, writes):
        deps = {}
        for r in reads:
            t = self.res_w.get(r)
            if t is not None:
                deps[t[0]] = max(deps.get(t[0], 0), t[1])
        for w in writes:
            t = self.res_w.get(w)
            if t is not None:
                deps[t[0]] = max(deps.get(t[0], 0), t[1])
            for k, v in self.res_r.get(w, {}).items():
                deps[k] = max(deps.get(k, 0), v)
        return deps

    def _record(self, token, reads, writes):
        for r in reads:
            d = self.res_r.setdefault(r, {})
            d[token[0]] = max(d.get(token[0], 0), token[1])
        for w in writes:
            self.res_w[w] = token
            self.res_r[w] = {}

    def op(self, e, fn, reads=(), writes=()):
        for key, val in self._deps(reads, writes).items():
            self._wait(e, key, val)
        inst = fn(self.eng[e])
        self.cnt[e] += 1
        inst.then_inc(self.sem[e], 1)
        self._record((("c", e), self.cnt[e]), reads, writes)
        self.ninst += 1
        return inst

    def dma(self, q, out, in_, reads=(), writes=(), **kw):
        i = self.drr[q] % NDS
        self.drr[q] += 1
        key = self.dsems[q][i]
        if self.dcount[key] > 0:
            self._wait(q, key, 16 * self.dcount[key])
        for k, val in self._deps(reads, writes).items():
            self._wait(q, k, val)
        inst = self.eng[q].dma_start(out=out, in_=in_, **kw)
        inst.then_inc(self.semobj[key], 16)
        self.dcount[key] += 1
        self._record((key, 16 * self.dcount[key]), reads, writes)
        self.ninst += 1
        return inst

    def barrier(self):
        for e in self.eng:
            for f in self.eng:
                if f != e and self.cnt[f] > 0:
                    self._wait(e, ("c", f), self.cnt[f])
            for key, c in self.dcount.items():
                if c > 0:
                    self._wait(e, key, 16 * c)

    def finish(self):
        self.barrier()
        self.es.close()
        return self.nc


def bc(ap, shape):
    return ap.to_broadcast(list(shape))


class Prog:
    def __init__(self, debug=False, stop_after=None):
        self.debug = debug
        self.stop_after = stop_after
        self.kb = KB()
        kb = self.kb
        nc = kb.nc
        I = lambda n, s: nc.dram_tensor(n, list(s), F32, kind="ExternalInput").ap()
        self.x = I("x", [SEQ, D])
        self.c = I("c", [D])
        self.ctx = I("ctx", [CTX, D])
        self.c_ctx = I("c_ctx", [D])
        self.w_mod = I("w_mod", [DEPTH, D, 6 * D])
        self.b_mod = I("b_mod", [DEPTH, 6 * D])
        self.norm1_g = I("norm1_g", [DEPTH, D])
        self.norm2_g = I("norm2_g", [DEPTH, D])
        self.w_in = I("w_in", [DEPTH, D, INW])
        self.b_gate = I("b_gate", [DEPTH, 2 * D])
        self.ssd_conv_w = I("ssd_conv_w", [DEPTH, 3, XBC])
        self.ssd_conv_b = I("ssd_conv_b", [DEPTH, XBC])
        self.ssd_dt_bias = I("ssd_dt_bias", [DEPTH, 2, NH])
        self.ssd_a_log = I("ssd_a_log", [DEPTH, 2, NH])
        self.ssd_d = I("ssd_d", [DEPTH, NH])
        self.ssd_norm_g = I("ssd_norm_g", [DEPTH, INNER])
        self.w_ssd_out = I("w_ssd_out", [DEPTH, INNER, D])
        self.sc_conv_w = I("sc_conv_w", [DEPTH, 3, D])
        self.w_sc_out = I("w_sc_out", [DEPTH, D, D])
        self.w_o = I("w_o", [DEPTH, D, D])
        self.ffn_w1 = I("ffn_w1", [1, D, FFN_DENSE])
        self.ffn_w3 = I("ffn_w3", [1, D, FFN_DENSE])
        self.ffn_w2 = I("ffn_w2", [1, FFN_DENSE, D])
        self.router_w = I("router_w", [1, D, NEXP])
        self.moe_w1 = I("moe_w1", [1, NEXP, D, FFN_EXP])
        self.moe_w3 = I("moe_w3", [1, NEXP, D, FFN_EXP])
        self.moe_w2 = I("moe_w2", [1, NEXP, FFN_EXP, D])
        self.final_g = I("final_g", [D])
        self.consts_d = I("consts", [128, 512])
        self.out = nc.dram_tensor("out", [SEQ, D], F32, kind="ExternalOutput").ap()
        self.xT = [kb.dram(f"xT{i}", [D, SEQ]) for i in range(2)]
        self.cT = [kb.dram(f"cT{i}", [D, CTX]) for i in range(2)]
        self.YB = kb.dram("YB", [SEQ, INNER])
        self.XB = kb.dram("XBst", [SEQ // TILE, 128, 24 * TILE], BF16)
        self.XS = kb.dram("XSst", [SEQ // 128, 128, INNER], BF16)
        self.BS = kb.dram("BSst", [SEQ // 128, 128, 512], BF16)
        self.NCS = 4
        self.csrow = kb.dram("csrow", [self.NCS, NH * 128])
        self.csrr = 0
        self.dbg = {}
        self.consts = kb.sb("consts", [128, 512])
        kb.dma("sp", self.consts[:], self.consts_d, writes=["consts"])
        self.ident = self.consts[:, 0:128]
        self.triF = self.consts[:, 128:256]
        self.triB = self.consts[:, 256:384]
        self.ones = self.consts[:, 384:512]
        self.identb = kb.sb("identb", [128, 128], BF16)
        kb.op("dve", lambda e: e.tensor_copy(self.identb[:], self.ident), reads=["consts"], writes=["identb"])
        self.cst = kb.sb("cst", [128, 4])
        kb.op("dve", lambda e: e.memset(self.cst[:, 0:1], 1.0), writes=["cst"])
        kb.op("dve", lambda e: e.memset(self.cst[:, 1:2], EPS), writes=["cst"])
        kb.op("dve", lambda e: e.memset(self.cst[:, 2:3], 0.0), writes=["cst"])
        self.one_c = self.cst[:, 0:1]
        self.eps_c = self.cst[:, 1:2]
        self.pA = kb.ps("pA", [128, 512])
        self.pB = kb.ps("pB", [128, 512])
        self.pT = kb.ps("pT", [128, 512])
        self.pTb = self.pT[:].bitcast(BF16)
        self.pS = kb.ps("pS", [128, 512])
        self.pYd = kb.ps("pYd", [128, 512])
        self.pYo = kb.ps("pYo", [128, 512])
        self.pSt = kb.ps("pSt", [128, 512])
        self.pM = kb.ps("pM", [128, 512])
        self.pab = 0
        self.modres = [kb.sb(f"modres{i}", [128, 2, 6, 8], F32) for i in range(DEPTH)]
        self.rot2 = [(self.pA, "pA"), (self.pB, "pB")]
        self.rot6 = [(self.pA, "pA"), (self.pB, "pB"), (self.pS, "pS"), (self.pYd, "pYd"), (self.pYo, "pYo"), (self.pSt, "pSt")]
        self.rot = self.rot6
        self.wdram = {}

    def load_w(self, w2d, c0, ncols, r0=0, nk=8, key=None):
        kb = self.kb
        slot = self.wrr % self.NWB
        self.wrr += 1
        buf = self.wbuf[slot]
        n = nk * ncols
        assert n <= 4096
        v = buf[:, 0:n].rearrange("p (k n) -> p k n", n=ncols)
        wn = f"wbuf{slot}"
        if key is not None and key in self.wdram:
            kb.dma("sp", buf[:, 0:n], self.wdram[key], reads=[("wd", key)], writes=[wn])
            return v, wn
        src = w2d.rearrange("(kc p) n -> p kc n", p=128)
        for k0 in range(0, nk, 8):
            k1 = min(nk, k0 + 8)
            kb.dma("pool", v[:, k0:k1, :], src[:, r0 + k0:r0 + k1, c0:c0 + ncols], writes=[wn])
        if key is not None:
            scr = kb.dram(f"wd{len(self.wdram)}", [128, n], BF16)
            kb.dma("sp", scr, buf[:, 0:n], reads=[wn], writes=[("wd", key)])
            self.wdram[key] = scr
        return v, wn

    def next_pab(self):
        self.pab = (self.pab + 1) % len(self.rot)
        return self.rot[self.pab]

    def set_wbufs(self, n, stack):
        self.NWB = n
        self.wbuf = [self.kb.sb(f"wbuf{i}", [128, 4096], BF16, stack) for i in range(n)]
        self.wrr = 0

    def colvec(self, name, src1d, n, stack=None):
        kb = self.kb
        t = kb.sb(name, [128, n], F32, stack)
        with kb.nc.allow_non_contiguous_dma(reason="small param vector"):
            kb.dma("sp", t[:], src1d.rearrange("(c p) -> p c", p=128), writes=[name])
        return t

    def rowbc(self, name, src1d, n, stack=None):
        kb = self.kb
        t = kb.sb(name, [128, n], F32, stack)
        kb.dma("sp", t[:], src1d.partition_broadcast(128), writes=[name])
        return t

    def to_fm(self, src_tm, dstT, T):
        kb = self.kb
        with contextlib.ExitStack() as st:
            xin = [kb.sb(f"tfm_in{i}", [128, D], F32, st) for i in range(2)]
            xo = [kb.sb(f"tfm_o{i}", [128, 8, 128], F32, st) for i in range(2)]
            for t in range(T // 128):
                a, o = xin[t % 2], xo[t % 2]
                an, on = f"tfm_in{t % 2}", f"tfm_o{t % 2}"
                kb.dma("sp", a[:], src_tm[t * 128:(t + 1) * 128, :], writes=[an])
                for h in range(2):
                    ps, pn = self.next_pab()
                    for j in range(4):
                        kc = h * 4 + j
                        kb.op("pe", lambda e, ps=ps, j=j, kc=kc, a=a: e.transpose(ps[:, j * 128:(j + 1) * 128], a[:, kc * 128:(kc + 1) * 128], self.ident),
                              reads=[an, "consts"], writes=[pn])
                    kb.op("act", lambda e, ps=ps, o=o, h=h: e.copy(o[:, h * 4:(h + 1) * 4, :], ps[:].rearrange("p (j t) -> p j t", j=4)),
                          reads=[pn], writes=[on])
                kb.dma("sp", dstT.rearrange("(kc p) t -> p kc t", p=128)[:, :, t * 128:(t + 1) * 128], o[:], reads=[on], writes=[("dram", id(dstT))])
            kb.barrier()

    def mod_vectors(self, i, st):
        kb = self.kb
        cc = kb.sb("mod_cc", [128, 8, 2], F32, st)
        with kb.nc.allow_non_contiguous_dma(reason="small"):
            kb.dma("sp", cc[:, :, 0], self.c.rearrange("(c p) -> p c", p=128), writes=["mod_cc"])
            kb.dma("sp", cc[:, :, 1], self.c_ctx.rearrange("(c p) -> p c", p=128), writes=["mod_cc"])
        sc = kb.sb("mod_sc", [128, 8, 2], F32, st)
        kb.op("act", lambda e: e.activation(out=sc[:], in_=cc[:], func=AF.Silu), reads=["mod_cc"], writes=["mod_sc"])
        bm = self.colvec("mod_b", self.b_mod[i], 48, st)
        n1 = self.colvec("mod_n1", self.norm1_g[i], 8, st)
        n2 = self.colvec("mod_n2", self.norm2_g[i], 8, st)
        mv = kb.sb("mod_mv", [128, 48, 2], F32, st)
        wst = kb.sb("mod_w", [128, 8, 512], F32, st)
        wsrc = self.w_mod[i].rearrange("(kc p) n -> p kc n", p=128)
        for blk in range(12):
            kb.dma("sp", wst[:], wsrc[:, :, blk * 512:(blk + 1) * 512], writes=["mod_w"])
            for j in range(4):
                col = blk * 4 + j
                for kc in range(8):
                    kb.op("pe", lambda e, j=j, kc=kc: e.matmul(self.pM[:, 0:2], wst[:, kc, j * 128:(j + 1) * 128], sc[:, kc, :], start=(kc == 0), stop=(kc == 7)),
                          reads=["mod_w", "mod_sc"], writes=["pM"])
                kb.op("dve", lambda e, col=col: e.tensor_tensor(mv[:, col, :], self.pM[:, 0:2], bc(bm[:, col:col + 1], [128, 2]), ALU.add),
                      reads=["pM", "mod_b"], writes=["mod_mv"])
        res = self.modres[i]
        for who in range(2):
            for half, nrm in ((0, n1), (1, n2)):
                b0 = half * 3
                kb.op("dve", lambda e, who=who, b0=b0: e.tensor_copy(res[:, who, b0 + 1, :], mv[:, b0 * 8:(b0 + 1) * 8, who]), reads=["mod_mv"], writes=[f"modres{i}"])
                kb.op("dve", lambda e, who=who, b0=b0, nrm=nrm: e.scalar_tensor_tensor(res[:, who, b0, :], mv[:, (b0 + 1) * 8:(b0 + 2) * 8, who], 1.0, nrm[:], ALU.add, ALU.mult),
                      reads=["mod_mv", "mod_n1", "mod_n2"], writes=[f"modres{i}"])
                kb.op("dve", lambda e, who=who, b0=b0: e.tensor_copy(res[:, who, b0 + 2, :], mv[:, (b0 + 2) * 8:(b0 + 3) * 8, who]), reads=["mod_mv"], writes=[f"modres{i}"])
        return res, f"modres{i}"

    def make_hT(self, srcT, T, t0, ncols, lead, res, resn, who, vec0, hT, hTn, tmp):
        kb = self.kb
        xw, xwn, sq, sqn, rs, rsn = tmp
        lo = t0 - lead
        hi = lo + ncols
        clo, chi = max(lo, 0), min(hi, T)
        j0, j1 = clo - lo, chi - lo
        src = srcT.rearrange("(kc p) t -> p kc t", p=128)
        kb.dma("sp", xw[:, :, j0:j1], src[:, :, clo:chi], reads=[("dram", id(srcT))], writes=[xwn])
        sqk = [f"{sqn}{kc}" for kc in range(8)]
        kb.op("act", lambda e: e.activation(out=sq[:, :, j0:j1], in_=xw[:, :, j0:j1], func=AF.Square), reads=[xwn], writes=sqk)
        for kc in range(8):
            kb.op("pe", lambda e, kc=kc: e.matmul(self.pM[:, j0:j1], self.ones, sq[:, kc, j0:j1], start=(kc == 0), stop=(kc == 7)),
                  reads=[sqk[kc], "consts"], writes=["pM"])
        kb.op("act", lambda e: e.activation(out=rs[:, j0:j1], in_=self.pM[:, j0:j1], func=AF.Sqrt, bias=self.eps_c, scale=1.0 / D),
              reads=["pM", "cst"], writes=[rsn])
        kb.op("dve", lambda e: e.reciprocal(rs[:, j0:j1], rs[:, j0:j1]), reads=[rsn], writes=[rsn])
        for kc in range(8):
            kb.op("dve", lambda e, kc=kc: e.tensor_tensor(sq[:, kc, j0:j1], xw[:, kc, j0:j1], rs[:, j0:j1], ALU.mult), reads=[xwn, rsn], writes=[sqk[kc]])
        for kc in range(8):
            kb.op("act", lambda e, kc=kc: e.activation(out=hT[:, kc, j0:j1], in_=sq[:, kc, j0:j1], func=AF.Identity,
                                                        bias=res[:, who, vec0 + 1, kc:kc + 1], scale=res[:, who, vec0, kc:kc + 1]),
                  reads=[sqk[kc], resn], writes=[hTn])
        if j0 > 0:
            kb.op("dve", lambda e: e.memset(hT[:, :, 0:j0], 0.0), writes=[hTn])
        if j1 < ncols:
            kb.op("dve", lambda e: e.memset(hT[:, :, j1:ncols], 0.0), writes=[hTn])

    def mixer_pass(self, i, d, srcT, dstT, T, who, res, resn, S, Sn, grid, lp, write_out):
        kb = self.kb
        st = contextlib.ExitStack()
        w_in = self.w_in[i]
        ntile = T // TILE
        self.uid = getattr(self, "uid", 0)

        def sbt(n, s, dt=F32, stack=None):
            self.uid += 1
            return kb.sb(f"{n}_{self.uid}", s, dt, stack or st)
        nset = 2
        hsets = []
        for k in range(nset):
            hsets.append((sbt(f"mx_xw{k}", [128, 8, WIN]), sbt(f"mx_sq{k}", [128, 8, WIN]), sbt(f"mx_rs{k}", [128, WIN]), sbt(f"mx_hT{k}", [128, 8, WIN], BF16)))

        def hset(k):
            xw_, sq_, rs_, hT_ = hsets[k]
            return xw_, f"mx_xw{k}", sq_, rs_, hT_, f"mx_hT{k}", (xw_, f"mx_xw{k}", sq_, f"mx_sq{k}", rs_, f"mx_rs{k}")
        Sbf = sbt("mx_Sbf", [128, INNER], BF16)
        kb.op("act", lambda e: e.copy(Sbf[:], S[:]), reads=[f"{Sn}{g}" for g in range(NG)], writes=[f"mx_Sbf{g}" for g in range(NG)])
        self.rot = self.rot6
        self.set_wbufs(4 if d == 1 else 3, st)
        dtb = sbt("mx_dtb", [64, 1]); alg = sbt("mx_alg", [64, 1]); aneg = sbt("mx_aneg", [64, 1])
        with kb.nc.allow_non_contiguous_dma(reason="small"):
            kb.dma("sp", dtb[:], self.ssd_dt_bias[i].rearrange("a (h o) -> (a h) o", o=1), writes=["mx_dtb"])
            kb.dma("sp", alg[:], self.ssd_a_log[i].rearrange("a (h o) -> (a h) o", o=1), writes=["mx_alg"])
        kb.op("act", lambda e: e.activation(out=aneg[:], in_=alg[:], func=AF.Exp), reads=["mx_alg"], writes=["mx_aneg"])
        kb.op("dve", lambda e: e.tensor_scalar(aneg[:], aneg[:], -1.0, None, ALU.mult), reads=["mx_aneg"], writes=["mx_aneg"])
        cw = sbt("mx_cw", [128, 3, 24]); cb = self.colvec("mx_cb", self.ssd_conv_b[i], 24, st)
        with kb.nc.allow_non_contiguous_dma(reason="small"):
            for j in range(3):
                kb.dma("sp", cw[:, j, :], self.ssd_conv_w[i, j].rearrange("(c p) -> p c", p=128), writes=["mx_cw"])
        tri = self.triB if d == 1 else self.triF
        if d == 0:
            Dbc = sbt("mx_Dbc", [128, NH])
            kb.dma("sp", Dbc[:], self.ssd_d[i].partition_broadcast(128), writes=["mx_Dbc"])
            gbc = self.rowbc("mx_gbc", self.ssd_norm_g[i], INNER, st)
            Did = sbt("mx_Did", [128, NH, 128], BF16)
            kb.op("dve", lambda e: e.tensor_tensor(Did[:], bc(self.ident.unsqueeze(1), [128, NH, 128]), bc(Dbc[:].unsqueeze(2), [128, NH, 128]), ALU.mult),
                  reads=["consts", "mx_Dbc"], writes=["mx_Did"])
            ynT = sbt("mx_ynT", [128, 16, TILE], BF16)
            bgate = self.colvec("mx_bg", self.b_gate[i], 16, st)
            scw = sbt("mx_scw", [128, 3, 8])
            with kb.nc.allow_non_contiguous_dma(reason="small"):
                for j in range(3):
                    kb.dma("sp", scw[:, j, :], self.sc_conv_w[i, j].rearrange("(c p) -> p c", p=128), writes=["mx_scw"])

        tiles = list(range(ntile))
        if d == 1:
            tiles = tiles[::-1]
        for tix, ti in enumerate(tiles):
            t0 = ti * TILE
            if d == 0 or tix == 0:
                sA = contextlib.ExitStack()
                xbcT = sbt("mx_xbcT", [128, 24, TILE], BF16, sA)
                cvt = [sbt(f"mx_cvt{k}", [128, TILE], F32, sA) for k in range(4)] if d == 1 else None
                dtT = sbt("mx_dtT", [64, TILE], F32, sA)
                dAT = sbt("mx_dAT", [64, TILE], F32, sA)
                dtA_tm = [sbt(f"mx_dtA_tm{k}", [128, 128], F32, sA) for k in range(2)]
                dt_tm = [t[:, 0:64] for t in dtA_tm]
                cstot = [sbt(f"mx_cstot{k}", [128, 2 * NH], F32, sA) for k in range(2)]
                ecd = [sbt(f"mx_ecd{k}", [128, 2 * NH], F32, sA) for k in range(2)]
                cs_sb = [t[:, 0:NH] for t in cstot]
                ecs_sb = [t[:, 0:NH] for t in ecd]
                cd_sb = [t[:, NH:2 * NH] for t in ecd]
                w_sb = [sbt(f"mx_w{k}", [128, NH], F32, sA) for k in range(2)]
                csT_sb = [sbt(f"mx_csT{k}", [NH, 128], F32, sA) for k in range(2)]
                xs_tm = [sbt(f"mx_xs{k}", [128, INNER], BF16, sA) for k in range(2)]
                B_tm = [sbt(f"mx_B{k}", [128, 512], BF16, sA) for k in range(2)]
                xdt = sbt("mx_xdt", [128, INNER], BF16, sA)
                xwt = sbt("mx_xwt", [128, INNER], BF16, sA)
                scm = sbt("mx_scm", [128, 4, 128], F32, sA)
                csb = [sbt(f"mx_csb{k}", [128, 8, 128], F32, sA) for k in range(4)]
                MT = [sbt(f"mx_MT{k}", [128, 8, 128], BF16, sA) for k in range(4)]
                ytmp = [sbt(f"mx_ytmp{k}", [128, 512], F32, sA) for k in range(4)]
                Yc = [sbt(f"mx_Yc{k}", [128, INNER], F32, sA) for k in range(2)]
                if d == 0:
                    zs = [sbt(f"mx_zs{k}", [128, 512], F32, sA) for k in range(2)]
                    ss = sbt("mx_ss", [128, 2], F32, sA)
                    yn_tm = sbt("mx_yn", [128, INNER], BF16, sA)
            xw, xwn, sq, rs, hT, hTn, tmp = hset(tix % nset)
            if tix == 0:
                self.make_hT(srcT, T, t0, WIN, 1, res, resn, who, 0, hT, hTn, tmp)
            if d == 0:
                kb.dma("sp", xbcT[:].rearrange("p a b -> p (a b)"), self.XB[ti], reads=[("XB", ti)], writes=["mx_xbcT"])
                for c in (0, 1):
                    cg = ti * 2 + c
                    kb.dma("sp", xs_tm[c][:], self.XS[cg], reads=[("XS", cg)], writes=[f"mx_xs{c}"])
                    kb.dma("sp", B_tm[c][:], self.BS[cg], reads=[("BS", cg)], writes=[f"mx_B{c}"])
            wb, wn = self.load_w(w_in, C_DT, 64, key=("in", i, C_DT))
            for kc in range(8):
                kb.op("pe", lambda e, wb=wb, kc=kc: e.matmul(self.pM[0:64, 0:WIN], wb[:, kc, 0:64], hT[:, kc, :], start=(kc == 0), stop=(kc == 7)),
                      reads=[wn, hTn], writes=["pM"])
            kb.op("act", lambda e: e.activation(out=dtT[:], in_=self.pM[0:64, 1:1 + TILE], func=AF.Exp, bias=dtb[:, 0:1]), reads=["pM", "mx_dtb"], writes=["mx_dtT"])
            kb.op("act", lambda e: e.activation(out=dtT[:], in_=dtT[:], func=AF.Ln, bias=self.one_c[0:64, :]), reads=["mx_dtT", "cst"], writes=["mx_dtT"])
            kb.op("dve", lambda e: e.tensor_scalar(dAT[:], dtT[:], aneg[:, 0:1], None, ALU.mult), reads=["mx_dtT", "mx_aneg"], writes=["mx_dAT"])
            chunks = [0, 1] if d == 0 else [1, 0]
            cslot = {}
            for c in chunks:
                cl = slice(c * 128, (c + 1) * 128)
                dsl = slice(d * NH, (d + 1) * NH)
                b1, b1n = self.next_pab()
                kb.op("pe", lambda e: e.transpose(b1[:, 0:64], dtT[:, cl], self.ident[0:64, 0:64]), reads=["mx_dtT", "consts"], writes=[b1n])
                kb.op("pe", lambda e: e.transpose(b1[:, 64:128], dAT[:, cl], self.ident[0:64, 0:64]), reads=["mx_dAT", "consts"], writes=[b1n])
                kb.op("act", lambda e: e.copy(dtA_tm[c][:], b1[:, 0:128]), reads=[b1n], writes=[f"mx_dt_tm{c}"])
                dt_c = dtA_tm[c][:, 0:64]
                dA_c = dtA_tm[c][:, 64:128]
                b2, b2n = self.next_pab()
                kb.op("pe", lambda e: e.matmul(b2[:, 0:NH], tri, dA_c[:, dsl], start=True, stop=True), reads=[f"mx_dt_tm{c}", "consts"], writes=[b2n])
                kb.op("pe", lambda e: e.matmul(b2[:, NH:2 * NH], self.ones, dA_c[:, dsl], start=True, stop=True), reads=[f"mx_dt_tm{c}", "consts"], writes=[b2n])
                kb.op("pe", lambda e: e.matmul(b2[0:NH, 128:256], dA_c[:, dsl], tri, start=True, stop=True), reads=[f"mx_dt_tm{c}", "consts"], writes=[b2n])
                kb.op("act", lambda e: e.copy(cstot[c][:], b2[:, 0:2 * NH]), reads=[b2n], writes=[f"mx_cs{c}"])
                kb.op("act", lambda e: e.copy(csT_sb[c][:], b2[0:NH, 128:256]), reads=[b2n], writes=[f"mx_csT{c}"])
                cslot[c] = self.csrr % self.NCS
                self.csrr += 1
                kb.dma("sp", self.csrow[cslot[c]].rearrange("(h l) -> h l", l=128), csT_sb[c][:], reads=[f"mx_csT{c}"], writes=[f"csrow{cslot[c]}"])
                kb.op("act", lambda e: e.activation(out=ecd[c][:], in_=cstot[c][:], func=AF.Exp), reads=[f"mx_cs{c}"], writes=[f"mx_ecs{c}"])
                kb.op("dve", lambda e: e.tensor_tensor(w_sb[c][:], cstot[c][:, NH:2 * NH], cstot[c][:, 0:NH], ALU.subtract), reads=[f"mx_cs{c}"], writes=[f"mx_w{c}"])
                kb.op("act", lambda e: e.activation(out=w_sb[c][:], in_=w_sb[c][:], func=AF.Exp), reads=[f"mx_w{c}"], writes=[f"mx_w{c}"])
                kb.op("dve", lambda e: e.tensor_tensor(w_sb[c][:], w_sb[c][:], dt_c[:, dsl], ALU.mult), reads=[f"mx_w{c}", f"mx_dt_tm{c}"], writes=[f"mx_w{c}"])
            if d == 1:
                pend = None
                for blk in range(6):
                    wb, wn = self.load_w(w_in, C_XBC + blk * 512, 512, key=("in", i, C_XBC + blk * 512))
                    for j in range(4):
                        cc = blk * 4 + j
                        ps, pn = self.next_pab()
                        for kc in range(8):
                            kb.op("pe", lambda e, ps=ps, wb=wb, j=j, kc=kc: e.matmul(ps[:, 0:WIN], wb[:, kc, j * 128:(j + 1) * 128], hT[:, kc, :], start=(kc == 0), stop=(kc == 7)),
                                  reads=[wn, hTn], writes=[pn])
                        cv = cvt[cc % 4]; cvn = f"mx_cvt{cc % 4}"
                        kb.op("act", lambda e, ps=ps, cv=cv, cc=cc: e.activation(out=cv[:], in_=ps[:, 1:1 + TILE], func=AF.Identity, bias=cb[:, cc:cc + 1], scale=cw[:, 1, cc:cc + 1]),
                              reads=[pn, "mx_cb", "mx_cw"], writes=[cvn])
                        kb.op("dve", lambda e, ps=ps, cv=cv, cc=cc: e.scalar_tensor_tensor(cv[:], ps[:, 0:TILE], cw[:, 0, cc:cc + 1], cv[:], ALU.mult, ALU.add),
                              reads=[pn, "mx_cw", cvn], writes=[cvn])
                        kb.op("dve", lambda e, ps=ps, cv=cv, cc=cc: e.scalar_tensor_tensor(cv[:], ps[:, 2:2 + TILE], cw[:, 2, cc:cc + 1], cv[:], ALU.mult, ALU.add),
                              reads=[pn, "mx_cw", cvn], writes=[cvn])
                        if pend is not None:
                            pend()

                        def pend(cv=cv, cc=cc, cvn=cvn):
                            kb.op("act", lambda e: e.activation(out=xbcT[:, cc, :], in_=cv[:], func=AF.Silu), reads=[cvn], writes=["mx_xbcT"])
                pend()
                kb.dma("sp", self.XB[ti], xbcT[:].rearrange("p a b -> p (a b)"), reads=["mx_xbcT"], writes=[("XB", ti)])
                if tix + 1 < len(tiles):
                    nx = hset((tix + 1) % nset)
                    self.make_hT(srcT, T, tiles[tix + 1] * TILE, WIN, 1, res, resn, who, 0, nx[4], nx[5], nx[6])
                for c in chunks:
                    cl = slice(c * 128, (c + 1) * 128)
                    cg = ti * 2 + c
                    for half in range(2):
                        bt, btn = self.next_pab()
                        btb = bt[:].bitcast(BF16)
                        for j in range(8):
                            cc = half * 8 + j
                            kb.op("pe", lambda e, cc=cc, j=j: e.transpose(btb[:, j * 128:(j + 1) * 128], xbcT[:, cc, cl], self.identb[:]),
                                  reads=["mx_xbcT", "identb"], writes=[btn])
                        kb.op("act", lambda e: e.copy(xs_tm[c][:, half * 1024:(half + 1) * 1024], btb), reads=[btn], writes=[f"mx_xs{c}"])
                    bt, btn = self.next_pab()
                    btb = bt[:].bitcast(BF16)
                    for j in range(4):
                        kb.op("pe", lambda e, j=j: e.transpose(btb[:, j * 128:(j + 1) * 128], xbcT[:, 16 + j, cl], self.identb[:]),
                              reads=["mx_xbcT", "identb"], writes=[btn])
                    kb.op("act", lambda e: e.copy(B_tm[c][:], btb[:, 0:512]), reads=[btn], writes=[f"mx_B{c}"])
                    kb.dma("sp", self.XS[cg], xs_tm[c][:], reads=[f"mx_xs{c}"], writes=[("XS", cg)])
                    kb.dma("sp", self.BS[cg], B_tm[c][:], reads=[f"mx_B{c}"], writes=[("BS", cg)])
            if d == 0 and tix + 1 < len(tiles):
                nx = hset((tix + 1) % nset)
                self.make_hT(srcT, T, tiles[tix + 1] * TILE, WIN, 1, res, resn, who, 0, nx[4], nx[5], nx[6])
            for c in chunks:
                cl = slice(c * 128, (c + 1) * 128)
                tok0 = t0 + c * 128
                dsl = slice(d * NH, (d + 1) * NH)
                slot = cslot[c]
                kb.op("dve", lambda e, c=c, dsl=dsl: e.tensor_tensor(xdt[:].rearrange("p (h q) -> p h q", q=64), xs_tm[c][:].rearrange("p (h q) -> p h q", q=64),
                                                                     bc(dt_tm[c][:, dsl].unsqueeze(2), [128, NH, 64]), ALU.mult),
                      reads=[f"mx_xs{c}", f"mx_dt_tm{c}"], writes=["mx_xdt"])
                kb.op("pool", lambda e, c=c: e.tensor_tensor(xwt[:].rearrange("p (h q) -> p h q", q=64), xs_tm[c][:].rearrange("p (h q) -> p h q", q=64),
                                                             bc(w_sb[c][:].unsqueeze(2), [128, NH, 64]), ALU.mult),
                      reads=[f"mx_xs{c}", f"mx_w{c}"], writes=["mx_xwt"])
                for g in range(NG):
                    kb.op("pe", lambda e, g=g, cl=cl: e.matmul(self.pS[:, g * 128:(g + 1) * 128], xbcT[:, 16 + g, cl], xbcT[:, 20 + g, cl], start=True, stop=True),
                          reads=["mx_xbcT"], writes=["pS"])
                kb.op("dve", lambda e: e.tensor_tensor(scm[:], self.pS[:].rearrange("p (g l) -> p g l", g=4), bc(tri.unsqueeze(1), [128, 4, 128]), ALU.mult),
                      reads=["pS", "consts"], writes=["mx_scm"])
                Y = Yc[c]; Yn = f"mx_Yc{c}"
                Yg = [f"{Yn}g{g}" for g in range(NG)]
                if d == 0:
                    kb.dma("sp", Y[:], self.YB[tok0:tok0 + 128, :], reads=[("YB", tok0 // 128)], writes=Yg)
                for g in range(NG):
                    kb.dma("sp", csb[g][:].rearrange("p h l -> p (h l)"), self.csrow[slot, g * 1024:(g + 1) * 1024].partition_broadcast(128),
                           reads=[f"csrow{slot}"], writes=[f"mx_csb{g}"])
                for g in range(NG):
                    kb.op("dve" if g < 2 else "pool", lambda e, c=c, g=g: e.tensor_tensor(csb[g][:], csb[g][:], bc(cs_sb[c][:, g * 8:(g + 1) * 8].unsqueeze(2), [128, 8, 128]), ALU.subtract),
                          reads=[f"mx_csb{g}", f"mx_cs{c}"], writes=[f"mx_csb{g}"])
                for g in range(NG):
                    kb.op("act", lambda e, g=g: e.activation(out=csb[g][:], in_=csb[g][:], func=AF.Exp), reads=[f"mx_csb{g}"], writes=[f"mx_csb{g}"])
                for g in range(NG):
                    gs_ = slice(g * 512, (g + 1) * 512)
                    kb.op("pool", lambda e, g=g, gs_=gs_: e.tensor_tensor(S[:, gs_].rearrange("p (h q) -> p h q", q=64), S[:, gs_].rearrange("p (h q) -> p h q", q=64),
                                                                     bc(cd_sb[c][:, g * 8:(g + 1) * 8].unsqueeze(2), [128, 8, 64]), ALU.mult),
                          reads=[f"{Sn}{g}", f"mx_ecs{c}"], writes=[f"{Sn}{g}"])
                for g in range(NG):
                    kb.op("dve", lambda e, g=g: e.scalar_tensor_tensor(MT[g][:], csb[g][:], 1.0, bc(scm[:, g, :].unsqueeze(1), [128, 8, 128]), ALU.min, ALU.mult),
                          reads=[f"mx_csb{g}", "mx_scm"], writes=[f"mx_MT{g}"])

                def s2(g):
                    yd, ydn = self.next_pab(); yo, yon = self.next_pab(); stb, stn = self.next_pab()
                    gs = slice(g * 512, (g + 1) * 512)
                    for h in range(8):
                        hh = g * 8 + h
                        kb.op("pe", lambda e, g=g, h=h, hh=hh, yd=yd: e.matmul(yd[:, h * 64:(h + 1) * 64], MT[g][:, h, :], xdt[:, hh * 64:(hh + 1) * 64], start=True, stop=(d == 1)),
                              reads=[f"mx_MT{g}", "mx_xdt"], writes=[ydn])
                        if d == 0:
                            kb.op("pe", lambda e, h=h, hh=hh, yd=yd: e.matmul(yd[:, h * 64:(h + 1) * 64], Did[:, hh, :], xs_tm[c][:, hh * 64:(hh + 1) * 64], start=False, stop=True),
                                  reads=["mx_Did", f"mx_xs{c}"], writes=[ydn])
                    kb.op("pe", lambda e, g=g, yo=yo, gs=gs: e.matmul(yo[:], xbcT[:, 20 + g, cl], Sbf[:, gs], start=True, stop=True),
                          reads=["mx_xbcT", f"mx_Sbf{g}"], writes=[yon])
                    kb.op("pe", lambda e, g=g, stb=stb, gs=gs: e.matmul(stb[:], B_tm[c][:, g * 128:(g + 1) * 128], xwt[:, gs], start=True, stop=True),
                          reads=[f"mx_B{c}", "mx_xwt"], writes=[stn])
                    return (yd, ydn, yo, yon, stb, stn)

                def s3(g, bk):
                    yd, ydn, yo, yon, stb, stn = bk
                    gs = slice(g * 512, (g + 1) * 512)
                    yt = ytmp[g]; ytn = f"mx_ytmp{g}"
                    kb.op("dve", lambda e: e.tensor_tensor(yt[:].rearrange("p (h q) -> p h q", q=64), yo[:].rearrange("p (h q) -> p h q", q=64),
                                                           bc(ecs_sb[c][:, g * 8:(g + 1) * 8].unsqueeze(2), [128, 8, 64]), ALU.mult),
                          reads=[yon, f"mx_ecs{c}"], writes=[ytn])
                    if d == 1:
                        kb.op("dve", lambda e: e.tensor_tensor(Y[:, gs], yd[:], yt[:], ALU.add), reads=[ydn, ytn], writes=[Yg[g]])
                    else:
                        kb.op("dve", lambda e: e.tensor_tensor(yt[:], yd[:], yt[:], ALU.add), reads=[ydn, ytn], writes=[ytn])
                        kb.op("pool", lambda e: e.tensor_tensor(Y[:, gs], Y[:, gs], yt[:], ALU.add), reads=[Yg[g], ytn], writes=[Yg[g]])
                    kb.op("dve", lambda e: e.tensor_tensor(S[:, gs], S[:, gs], stb[:], ALU.add), reads=[f"{Sn}{g}", stn], writes=[f"{Sn}{g}"])
                    kb.op("act", lambda e: e.copy(Sbf[:, gs], S[:, gs]), reads=[f"{Sn}{g}"], writes=[f"mx_Sbf{g}"])

                bks = {}
                bks[0] = s2(0)
                bks[1] = s2(1)
                s3(0, bks[0])
                bks[2] = s2(2)
                s3(1, bks[1])
                bks[3] = s2(3)
                s3(2, bks[2])
                s3(3, bks[3])
                if d == 1:
                    kb.dma("sp", self.YB[tok0:tok0 + 128, :], Y[:], reads=Yg, writes=[("YB", tok0 // 128)])
            if d == 1:
                if tix == len(tiles) - 1:
                    kb.barrier()
                    sA.close()
                continue
            for blk in range(4):
                wb, wn = self.load_w(w_in, C_Z + blk * 512, 512, key=("in", i, C_Z + blk * 512))
                for c in chunks:
                    ps, pn = self.next_pab()
                    for kc in range(8):
                        kb.op("pe", lambda e, ps=ps, wb=wb, kc=kc, c=c: e.matmul(ps[:], hT[:, kc, 1 + c * 128:1 + (c + 1) * 128], wb[:, kc, :], start=(kc == 0), stop=(kc == 7)),
                              reads=[wn, hTn], writes=[pn])
                    zk = (blk * 2 + c) % 2
                    kb.op("act", lambda e, ps=ps, zk=zk: e.activation(out=zs[zk][:], in_=ps[:], func=AF.Silu), reads=[pn], writes=[f"mx_zs{zk}"])
                    bs = slice(blk * 512, (blk + 1) * 512)
                    kb.op("dve", lambda e, c=c, bs=bs, zk=zk: e.tensor_tensor(Yc[c][:, bs], Yc[c][:, bs], zs[zk][:], ALU.mult), reads=[f"mx_Yc{c}g{blk}", f"mx_zs{zk}"], writes=[f"mx_Yc{c}g{blk}"])
            for c in chunks:
                Y = Yc[c]; Yn = f"mx_Yc{c}"
                Yg = [f"{Yn}g{g}" for g in range(NG)]
                kb.op("act", lambda e, Y=Y: e.activation(out=yn_tm[:], in_=Y[:], func=AF.Square, accum_out=ss[:, 0:1]), reads=Yg, writes=["mx_yn", "mx_ss"])
                kb.op("act", lambda e: e.activation(out=ss[:, 1:2], in_=ss[:, 0:1], func=AF.Sqrt, bias=self.eps_c, scale=1.0 / INNER), reads=["mx_ss", "cst"], writes=["mx_ss"])
                kb.op("dve", lambda e: e.reciprocal(ss[:, 1:2], ss[:, 1:2]), reads=["mx_ss"], writes=["mx_ss"])
                kb.op("dve", lambda e, Y=Y: e.scalar_tensor_tensor(yn_tm[:], Y[:], ss[:, 1:2], gbc[:], ALU.mult, ALU.mult), reads=Yg + ["mx_ss", "mx_gbc"], writes=["mx_yn"])
                for half in range(2):
                    for j in range(8):
                        kc = half * 8 + j
                        kb.op("pe", lambda e, j=j, kc=kc: e.transpose(self.pTb[:, j * 128:(j + 1) * 128], yn_tm[:, kc * 128:(kc + 1) * 128], self.identb[:]),
                              reads=["mx_yn", "identb"], writes=["pT"])
                    kb.op("act", lambda e, c=c, half=half: e.copy(ynT[:, half * 8:(half + 1) * 8, c * 128:(c + 1) * 128], self.pTb[:].rearrange("p (j t) -> p j t", j=8)),
                          reads=["pT"], writes=["mx_ynT"])
            kb.barrier()
            sA.close()
            sC = contextlib.ExitStack()
            gbs = sbt("mx_gbs", [128, 8, TILE], F32, sC); gcs = sbt("mx_gcs", [128, 8, TILE], F32, sC); uu = sbt("mx_u", [128, 8, TILE], F32, sC)
            vv = gcs
            svT = sbt("mx_svT", [128, 8, TILE], BF16, sC)
            gT = sbt("mx_gT", [128, 16, TILE], F32, sC)
            t1s = [sbt(f"mx_t1{k}", [128, TILE], F32, sC) for k in range(2)]; t2s = [sbt(f"mx_t2{k}", [128, TILE], F32, sC) for k in range(2)]
            mT = sbt("mx_mT", [128, 8, TILE], BF16, sC)
            xot = sbt("mx_xo", [128, 8, TILE], F32, sC); xon = "mx_xo"
            for blk in range(6):
                wb, wn = self.load_w(w_in, C_SC + blk * 512, 512, key=("in", i, C_SC + blk * 512))
                for j in range(4):
                    cc = blk * 4 + j
                    kind, f = cc // 8, cc % 8
                    ps, pn = self.next_pab()
                    for kc in range(8):
                        kb.op("pe", lambda e, ps=ps, wb=wb, j=j, kc=kc: e.matmul(ps[:, 0:TILE], wb[:, kc, j * 128:(j + 1) * 128], hT[:, kc, 1:1 + TILE], start=(kc == 0), stop=(kc == 7)),
                              reads=[wn, hTn], writes=[pn])
                    if kind == 0:
                        kb.op("act", lambda e, ps=ps, f=f: e.copy(gbs[:, f, :], ps[:, 0:TILE]), reads=[pn], writes=["mx_gbs"])
                    elif kind == 1:
                        kb.op("act", lambda e, ps=ps, f=f: e.copy(gcs[:, f, :], ps[:, 0:TILE]), reads=[pn], writes=["mx_gcs"])
                    else:
                        kb.op("dve", lambda e, ps=ps, f=f: e.tensor_tensor(uu[:, f, :], gcs[:, f, :], ps[:, 0:TILE], ALU.mult), reads=[pn, "mx_gcs"], writes=["mx_u"])
            rows = TILE // grid
            for f in range(8):
                u3 = uu[:, f, :].rearrange("p (r w) -> p r w", w=grid)
                v3 = vv[:, f, :].rearrange("p (r w) -> p r w", w=grid)
                kb.op("act", lambda e, f=f: e.activation(out=vv[:, f, :], in_=uu[:, f, :], func=AF.Identity, scale=scw[:, 1, f:f + 1]), reads=["mx_u", "mx_scw"], writes=["mx_gcs"])
                kb.op("dve", lambda e, f=f, u3=u3, v3=v3: e.scalar_tensor_tensor(v3[:, :, 1:grid], u3[:, :, 0:grid - 1], scw[:, 0, f:f + 1], v3[:, :, 1:grid], ALU.mult, ALU.add),
                      reads=["mx_u", "mx_scw", "mx_gcs"], writes=["mx_gcs"])
                kb.op("dve", lambda e, f=f, u3=u3, v3=v3: e.scalar_tensor_tensor(v3[:, :, 0:grid - 1], u3[:, :, 1:grid], scw[:, 2, f:f + 1], v3[:, :, 0:grid - 1], ALU.mult, ALU.add),
                      reads=["mx_u", "mx_scw", "mx_gcs"], writes=["mx_gcs"])
                kb.op("pool", lambda e, f=f: e.tensor_tensor(svT[:, f, :], gbs[:, f, :], vv[:, f, :], ALU.mult), reads=["mx_gbs", "mx_gcs"], writes=["mx_svT"])
            for blk in range(4):
                wb, wn = self.load_w(w_in, C_GL + blk * 512, 512, key=("in", i, C_GL + blk * 512))
                for j in range(4):
                    cc = blk * 4 + j
                    ps, pn = self.next_pab()
                    for kc in range(8):
                        kb.op("pe", lambda e, ps=ps, wb=wb, j=j, kc=kc: e.matmul(ps[:, 0:TILE], wb[:, kc, j * 128:(j + 1) * 128], hT[:, kc, 1:1 + TILE], start=(kc == 0), stop=(kc == 7)),
                              reads=[wn, hTn], writes=[pn])
                    kb.op("act", lambda e, ps=ps, cc=cc: e.activation(out=gT[:, cc, :], in_=ps[:, 0:TILE], func=AF.Sigmoid, bias=bgate[:, cc:cc + 1]), reads=[pn, "mx_bg"], writes=["mx_gT"])
            for ob in range(4):
                wso, wson = self.load_w(self.w_ssd_out[i], ob * 256, 256, 0, 16, key=("so", i, ob))
                wsc, wscn = self.load_w(self.w_sc_out[i], ob * 256, 256, 0, 8, key=("sc", i, ob))
                for j in range(2):
                    fo = ob * 2 + j
                    for kc in range(16):
                        kb.op("pe", lambda e, wso=wso, j=j, kc=kc: e.matmul(self.pA[:, 0:TILE], wso[:, kc, j * 128:(j + 1) * 128], ynT[:, kc, :], start=(kc == 0), stop=(kc == 15)),
                              reads=[wson, "mx_ynT"], writes=["pA"])
                    for kc in range(8):
                        kb.op("pe", lambda e, wsc=wsc, j=j, kc=kc: e.matmul(self.pB[:, 0:TILE], wsc[:, kc, j * 128:(j + 1) * 128], svT[:, kc, :], start=(kc == 0), stop=(kc == 7)),
                              reads=[wscn, "mx_svT"], writes=["pB"])
                    t1 = t1s[fo % 2]; t2 = t2s[fo % 2]
                    kb.op("dve", lambda e, fo=fo: e.tensor_tensor(t1[:], gT[:, fo, :], self.pA[:, 0:TILE], ALU.mult), reads=["mx_gT", "pA"], writes=[f"mx_t1{fo % 2}"])
                    kb.op("dve", lambda e, fo=fo: e.tensor_tensor(t2[:], gT[:, 8 + fo, :], self.pB[:, 0:TILE], ALU.mult), reads=["mx_gT", "pB"], writes=[f"mx_t2{fo % 2}"])
                    kb.op("pool", lambda e, fo=fo: e.tensor_tensor(mT[:, fo, :], t1[:], t2[:], ALU.add), reads=[f"mx_t1{fo % 2}", f"mx_t2{fo % 2}"], writes=["mx_mT"])
            for ob in range(2):
                wo, won = self.load_w(self.w_o[i], ob * 512, 512, 0, 8, key=("wo", i, ob))
                for j in range(4):
                    fo = ob * 4 + j
                    ps, pn = self.next_pab()
                    for kc in range(8):
                        kb.op("pe", lambda e, ps=ps, wo=wo, j=j, kc=kc: e.matmul(ps[:, 0:TILE], wo[:, kc, j * 128:(j + 1) * 128], mT[:, kc, :], start=(kc == 0), stop=(kc == 7)),
                              reads=[won, "mx_mT"], writes=[pn])
                    kb.op("dve", lambda e, ps=ps, fo=fo, xot=xot: e.scalar_tensor_tensor(xot[:, fo, :], ps[:, 0:TILE], res[:, who, 2, fo:fo + 1], xw[:, fo, 1:1 + TILE], ALU.mult, ALU.add),
                          reads=[pn, resn, xwn], writes=[xon])
            if write_out:
                kb.dma("sp", dstT.rearrange("(kc p) t -> p kc t", p=128)[:, :, t0:t0 + TILE], xot[:], reads=[xon], writes=[("dram", id(dstT))])
            kb.barrier()
            sC.close()
        kb.barrier()
        st.close()

    def ffn(self, i, srcT, dstT, T, who, res, resn, moe):
        kb = self.kb
        st = contextlib.ExitStack()
        sbt = lambda n, s, dt=F32: kb.sb(n, s, dt, st)
        TS = min(T, 1024)
        self.rot = self.rot2
        self.set_wbufs(6, st)
        NE = NEXP if moe else 1
        HID = FFN_EXP if moe else FFN_DENSE
        blocks = [(b0, min(512, HID - b0)) for b0 in range(0, HID, 512)]
        xs = sbt("ff_x", [128, 8, TS])
        h2 = sbt("ff_h2", [128, 8, TS], BF16)
        acc = sbt("ff_acc", [128, 8, TS])
        sq, rs = sbt("ff_sq", [128, 8, 128]), sbt("ff_rs", [128, 128])
        hf = sbt("ff_hf", [128, 8, 128])
        tmp = None
        sa = [sbt(f"ff_sa{k}", [128, TILE]) for k in range(3)]; tt = [sbt(f"ff_tt{k}", [128, TILE]) for k in range(3)]
        hid = [sbt(f"ff_hid{k}", [128, 4, TILE], BF16) for k in range(2)]
        pacc = self.pS
        pbanks = [(self.pS, "pS"), (self.pYd, "pYd"), (self.pYo, "pYo"), (self.pSt, "pSt")]
        if moe:
            rw = sbt("ff_rw", [128, 8, NEXP])
            kb.dma("sp", rw[:], self.router_w[0].rearrange("(kc p) n -> p kc n", p=128), writes=["ff_rw"])
            lg = sbt("ff_lg", [128, NEXP]); l2 = sbt("ff_l2", [128, NEXP]); m1 = sbt("ff_m1", [128, 4])
            mk1 = sbt("ff_mk1", [128, NEXP]); mk2 = sbt("ff_mk2", [128, NEXP]); gt = sbt("ff_gt", [128, NEXP])
            dg = sbt("ff_dg", [128, NEXP, 128])
            gbc = sbt("ff_gbc", [128, NEXP, TS])
        for s0 in range(0, T, TS):
            src = srcT.rearrange("(kc p) t -> p kc t", p=128)
            kb.dma("sp", xs[:], src[:, :, s0:s0 + TS], reads=[("dram", id(srcT))], writes=["ff_x"])
            for q in range(TS // 128):
                ql = slice(q * 128, (q + 1) * 128)
                kb.op("act", lambda e, ql=ql: e.activation(out=sq[:], in_=xs[:, :, ql], func=AF.Square), reads=["ff_x"], writes=[f"ff_sq{k}" for k in range(8)])
                for kc in range(8):
                    kb.op("pe", lambda e, kc=kc: e.matmul(self.pM[:, 0:128], self.ones, sq[:, kc, :], start=(kc == 0), stop=(kc == 7)), reads=[f"ff_sq{kc}", "consts"], writes=["pM"])
                kb.op("act", lambda e: e.activation(out=rs[:], in_=self.pM[:, 0:128], func=AF.Sqrt, bias=self.eps_c, scale=1.0 / D), reads=["pM", "cst"], writes=["ff_rs"])
                kb.op("dve", lambda e: e.reciprocal(rs[:], rs[:]), reads=["ff_rs"], writes=["ff_rs"])
                for kc in range(8):
                    kb.op("dve", lambda e, kc=kc, ql=ql: e.tensor_tensor(sq[:, kc, :], xs[:, kc, ql], rs[:], ALU.mult), reads=["ff_x", "ff_rs"], writes=[f"ff_sq{kc}"])
                    kb.op("act", lambda e, kc=kc: e.activation(out=hf[:, kc, :], in_=sq[:, kc, :], func=AF.Identity, bias=res[:, who, 4, kc:kc + 1], scale=res[:, who, 3, kc:kc + 1]),
                          reads=[f"ff_sq{kc}", resn], writes=[f"ff_hf{kc}"])
                kb.op("pool", lambda e, ql=ql: e.tensor_copy(h2[:, :, ql], hf[:]), reads=[f"ff_hf{k}" for k in range(8)], writes=["ff_h2"])
                if moe:
                    rwf = sbt("ff_rwf", [128, 8, NEXP]) if False else None
                    for kc in range(8):
                        kb.op("pe", lambda e, kc=kc: e.matmul(self.pM[:, 256:256 + NEXP], hf[:, kc, :], rw[:, kc, :], start=(kc == 0), stop=(kc == 7)), reads=[f"ff_hf{kc}", "ff_rw"], writes=["pM"])
                    kb.op("act", lambda e: e.copy(lg[:], self.pM[:, 256:256 + NEXP]), reads=["pM"], writes=["ff_lg"])
                    kb.op("dve", lambda e: e.tensor_reduce(m1[:, 0:1], lg[:], mybir.AxisListType.X, ALU.max), reads=["ff_lg"], writes=["ff_m1"])
                    kb.op("dve", lambda e: e.tensor_tensor(mk1[:], lg[:], bc(m1[:, 0:1], [128, NEXP]), ALU.is_equal), reads=["ff_lg", "ff_m1"], writes=["ff_mk1"])
                    kb.op("dve", lambda e: e.scalar_tensor_tensor(l2[:], mk1[:], -1e30, lg[:], ALU.mult, ALU.add), reads=["ff_mk1", "ff_lg"], writes=["ff_l2"])
                    kb.op("dve", lambda e: e.tensor_reduce(m1[:, 1:2], l2[:], mybir.AxisListType.X, ALU.max), reads=["ff_l2"], writes=["ff_m1"])
                    kb.op("dve", lambda e: e.tensor_tensor(mk2[:], l2[:], bc(m1[:, 1:2], [128, NEXP]), ALU.is_equal), reads=["ff_l2", "ff_m1"], writes=["ff_mk2"])
                    kb.op("dve", lambda e: e.tensor_tensor(m1[:, 2:3], m1[:, 1:2], m1[:, 0:1], ALU.subtract), reads=["ff_m1"], writes=["ff_m1"])
                    kb.op("act", lambda e: e.activation(out=m1[:, 2:3], in_=m1[:, 2:3], func=AF.Exp), reads=["ff_m1"], writes=["ff_m1"])
                    kb.op("dve", lambda e: e.tensor_scalar(m1[:, 2:3], m1[:, 2:3], 1.0, None, ALU.add), reads=["ff_m1"], writes=["ff_m1"])
                    kb.op("dve", lambda e: e.reciprocal(m1[:, 2:3], m1[:, 2:3]), reads=["ff_m1"], writes=["ff_m1"])
                    kb.op("dve", lambda e: e.tensor_scalar(m1[:, 3:4], m1[:, 2:3], -1.0, 1.0, ALU.mult, ALU.add), reads=["ff_m1"], writes=["ff_m1"])
                    kb.op("dve", lambda e: e.tensor_scalar(gt[:], mk1[:], m1[:, 2:3], None, ALU.mult), reads=["ff_mk1", "ff_m1"], writes=["ff_gt"])
                    kb.op("dve", lambda e: e.scalar_tensor_tensor(gt[:], mk2[:], m1[:, 3:4], gt[:], ALU.mult, ALU.add), reads=["ff_mk2", "ff_m1", "ff_gt"], writes=["ff_gt"])
                    kb.op("dve", lambda e: e.tensor_tensor(dg[:], bc(self.ident.unsqueeze(1), [128, NEXP, 128]), bc(gt[:].unsqueeze(2), [128, NEXP, 128]), ALU.mult),
                          reads=["consts", "ff_gt"], writes=["ff_dg"])
                    for hh in range(2):
                        ps, pn = self.next_pab()
                        kb.op("pe", lambda e, ps=ps, hh=hh: e.matmul(ps[:], self.ones, dg[:, hh * 4:(hh + 1) * 4, :].rearrange("p a b -> p (a b)"), start=True, stop=True),
                              reads=["ff_dg", "consts"], writes=[pn])
                        kb.op("act", lambda e, ps=ps, hh=hh, ql=ql: e.copy(gbc[:, hh * 4:(hh + 1) * 4, ql], ps[:].rearrange("p (a b) -> p a b", a=4)), reads=[pn], writes=["ff_gbc"])
            kb.barrier()
            items = []
            for ex in range(NE):
                for bi, (b0, bn) in enumerate(blocks):
                    for tq in range(TS // TILE):
                        items.append((ex, bi, b0, bn, tq))
            wcache = {}
            slots = [(self.pA, "pA"), (self.pB, "pB"), (self.pM, "pM"), (self.pT, "pT")]
            self.ffs = getattr(self, "ffs", 0)

            def slot():
                self.ffs = (self.ffs + 1) % len(slots)
                t, n = slots[self.ffs]
                return t[:, 0:TILE], n

            def Wsrc(ex):
                if moe:
                    return self.moe_w1[0, ex], self.moe_w3[0, ex], self.moe_w2[0, ex]
                return self.ffn_w1[0], self.ffn_w3[0], self.ffn_w2[0]

            def AB(n):
                ex, bi, b0, bn, tq = items[n]
                nh = bn // 128
                W1, W3, W2 = Wsrc(ex)
                if (ex, bi, 1) not in wcache:
                    wcache[(ex, bi, 1)] = self.load_w(W1, b0, bn)
                    wcache[(ex, bi, 3)] = self.load_w(W3, b0, bn)
                ntq = TS // TILE
                if tq == min(1, ntq - 1) and n + ntq - tq < len(items):
                    ex2, bi2, b02, bn2, _ = items[n + ntq - tq]
                    if (ex2, bi2, 1) not in wcache:
                        W1b, W3b, _w = Wsrc(ex2)
                        wcache[(ex2, bi2, 1)] = self.load_w(W1b, b02, bn2)
                        wcache[(ex2, bi2, 3)] = self.load_w(W3b, b02, bn2)
                w1, w1n = wcache[(ex, bi, 1)]
                w3, w3n = wcache[(ex, bi, 3)]
                tl = slice(tq * TILE, (tq + 1) * TILE)
                hd = hid[n % 2]; hdn = f"ff_hid{n % 2}"
                for hc in range(nh):
                    pa, pan = slot()
                    pb_, pbn_ = slot()
                    for kc in range(8):
                        kb.op("pe", lambda e, kc=kc: e.matmul(pa, w1[:, kc, hc * 128:(hc + 1) * 128], h2[:, kc, tl], start=(kc == 0), stop=(kc == 7)),
                              reads=[w1n, "ff_h2"], writes=[pan])
                    for kc in range(8):
                        kb.op("pe", lambda e, kc=kc: e.matmul(pb_, w3[:, kc, hc * 128:(hc + 1) * 128], h2[:, kc, tl], start=(kc == 0), stop=(kc == 7)),
                              reads=[w3n, "ff_h2"], writes=[pbn_])
                    k3 = (n * 4 + hc) % 3
                    kb.op("act", lambda e: e.activation(out=sa[k3][:], in_=pa, func=AF.Silu), reads=[pan], writes=[f"ff_sa{k3}"])
                    if moe:
                        kb.op("dve", lambda e: e.tensor_tensor(tt[k3][:], sa[k3][:], pb_, ALU.mult), reads=[f"ff_sa{k3}", pbn_], writes=[f"ff_tt{k3}"])
                        kb.op("dve", lambda e: e.tensor_tensor(hd[:, hc, :], tt[k3][:], gbc[:, ex, tl], ALU.mult), reads=[f"ff_tt{k3}", "ff_gbc"], writes=[hdn])
                    else:
                        kb.op("dve", lambda e: e.tensor_tensor(hd[:, hc, :], sa[k3][:], pb_, ALU.mult), reads=[f"ff_sa{k3}", pbn_], writes=[hdn])

            def W2s(n):
                ex, bi, b0, bn, tq = items[n]
                nh = bn // 128
                W1, W3, W2 = Wsrc(ex)
                if (ex, bi, 2) not in wcache:
                    wcache[(ex, bi, 2)] = self.load_w_rows(W2, b0 // 128, nh)
                w2, w2n = wcache[(ex, bi, 2)]
                tl = slice(tq * TILE, (tq + 1) * TILE)
                hd = hid[n % 2]; hdn = f"ff_hid{n % 2}"
                for fo in range(8):
                    pb, pbn = pbanks[fo // 2]
                    osl = slice((fo % 2) * TILE, (fo % 2 + 1) * TILE)
                    for hc in range(nh):
                        kb.op("pe", lambda e, hc=hc: e.matmul(pb[:, osl], w2[:, hc, fo * 128:(fo + 1) * 128], hd[:, hc, :], start=(hc == 0), stop=(hc == nh - 1)),
                              reads=[w2n, hdn], writes=[pbn])
                first = (ex == 0 and bi == 0)
                for k4 in range(4):
                    pb, pbn = pbanks[k4]
                    a_v = acc[:, 2 * k4:2 * k4 + 2, tl]
                    p_v = pb[:].rearrange("p (a t) -> p a t", a=2)
                    if first:
                        kb.op("act", lambda e: e.copy(a_v, p_v), reads=[pbn], writes=[f"ff_acc{tq}"])
                    else:
                        kb.op("dve", lambda e: e.tensor_tensor(a_v, a_v, p_v, ALU.add), reads=[pbn, f"ff_acc{tq}"], writes=[f"ff_acc{tq}"])

            AB(0)
            for n in range(1, len(items)):
                AB(n)
                W2s(n - 1)
            W2s(len(items) - 1)
            accn = [f"ff_acc{tq}" for tq in range(TS // TILE)]
            for fo in range(8):
                kb.op("dve", lambda e, fo=fo: e.scalar_tensor_tensor(acc[:, fo, :], acc[:, fo, :], res[:, who, 5, fo:fo + 1], xs[:, fo, :], ALU.mult, ALU.add),
                      reads=accn + [resn, "ff_x"], writes=accn)
            kb.dma("sp", dstT.rearrange("(kc p) t -> p kc t", p=128)[:, :, s0:s0 + TS], acc[:], reads=accn, writes=[("dram", id(dstT))])
            kb.barrier()
        kb.barrier()
        st.close()

    def load_w_rows(self, w2d, r0, nk):
        kb = self.kb
        slot = self.wrr % self.NWB
        self.wrr += 1
        buf = self.wbuf[slot]
        v = buf[:, 0:nk * 1024].rearrange("p (k n) -> p k n", n=1024)
        src = w2d.rearrange("(kc p) n -> p kc n", p=128)
        kb.dma("pool", v, src[:, r0:r0 + nk, :], writes=[f"wbuf{slot}"])
        return v, f"wbuf{slot}"

    def final(self, srcT):
        kb = self.kb
        st = contextlib.ExitStack()
        sbt = lambda n, s, dt=F32: kb.sb(n, s, dt, st)
        fg = self.colvec("fn_g", self.final_g, 8, st)
        xw = [sbt(f"fn_x{k}", [128, 8, 128]) for k in range(2)]
        sq, rs = sbt("fn_sq", [128, 8, 128]), sbt("fn_rs", [128, 128])
        ot = [sbt(f"fn_o{k}", [128, D]) for k in range(2)]
        src = srcT.rearrange("(kc p) t -> p kc t", p=128)
        for t in range(SEQ // 128):
            x_, xn = xw[t % 2], f"fn_x{t % 2}"
            o_, on = ot[t % 2], f"fn_o{t % 2}"
            kb.dma("sp", x_[:], src[:, :, t * 128:(t + 1) * 128], reads=[("dram", id(srcT))], writes=[xn])
            kb.op("act", lambda e, x_=x_: e.activation(out=sq[:], in_=x_[:], func=AF.Square), reads=[xn], writes=[f"fn_sq{k}" for k in range(8)])
            for kc in range(8):
                kb.op("pe", lambda e, kc=kc: e.matmul(self.pM[:, 0:128], self.ones, sq[:, kc, :], start=(kc == 0), stop=(kc == 7)), reads=[f"fn_sq{kc}", "consts"], writes=["pM"])
            kb.op("act", lambda e: e.activation(out=rs[:], in_=self.pM[:, 0:128], func=AF.Sqrt, bias=self.eps_c, scale=1.0 / D), reads=["pM", "cst"], writes=["fn_rs"])
            kb.op("dve", lambda e: e.reciprocal(rs[:], rs[:]), reads=["fn_rs"], writes=["fn_rs"])
            for kc in range(8):
                kb.op("dve", lambda e, kc=kc, x_=x_: e.scalar_tensor_tensor(sq[:, kc, :], x_[:, kc, :], fg[:, kc:kc + 1], rs[:], ALU.mult, ALU.mult), reads=[xn, "fn_g", "fn_rs"], writes=[f"fn_sq{kc}"])
            for h in range(2):
                ps, pn = self.next_pab()
                for j in range(4):
                    kc = h * 4 + j
                    kb.op("pe", lambda e, ps=ps, j=j, kc=kc: e.transpose(ps[:, j * 128:(j + 1) * 128], sq[:, kc, :], self.ident), reads=[f"fn_sq{kc}", "consts"], writes=[pn])
                kb.op("act", lambda e, ps=ps, h=h, o_=o_: e.copy(o_[:, h * 512:(h + 1) * 512], ps[:]), reads=[pn], writes=[on])
            kb.dma("sp", self.out[t * 128:(t + 1) * 128, :], o_[:], reads=[on], writes=["out"])
        kb.barrier()
        st.close()

    def build(self):
        kb = self.kb
        self.to_fm(self.x, self.xT[0], SEQ)
        self.to_fm(self.ctx, self.cT[0], CTX)
        xa, xb = self.xT
        ca, cb_ = self.cT
        Sf = kb.sb("S_f", [128, INNER]); Sb = kb.sb("S_b", [128, INNER])
        for i in range(DEPTH):
            last = i == DEPTH - 1
            st = contextlib.ExitStack()
            res, resn = self.mod_vectors(i, st)
            kb.barrier()
            st.close()
            kb.op("dve", lambda e: e.memset(Sf[:], 0.0), writes=[f"S_f{g}" for g in range(NG)])
            kb.op("dve", lambda e: e.memset(Sb[:], 0.0), writes=[f"S_b{g}" for g in range(NG)])
            self.mixer_pass(i, 1, ca, cb_, CTX, 1, res, resn, Sb, "S_b", CTX, last, False)
            self.mixer_pass(i, 0, ca, cb_, CTX, 1, res, resn, Sf, "S_f", CTX, last, not last)
            self.mixer_pass(i, 1, xa, xb, SEQ, 0, res, resn, Sb, "S_b", 64, last, False)
            self.mixer_pass(i, 0, xa, xb, SEQ, 0, res, resn, Sf, "S_f", 64, last, True)
            self.ffn(i, xb, xa, SEQ, 0, res, resn, moe=(i % 2 == 1))
            if not last:
                self.ffn(i, cb_, ca, CTX, 1, res, resn, moe=(i % 2 == 1))
        self.final(xa)
        return kb.finish()


def _consts():
    c = np.zeros((128, 512), np.float32)
    c[:, 0:128] = np.eye(128, dtype=np.float32)
    l = np.arange(128)
    c[:, 128:256] = (l[:, None] <= l[None, :]).astype(np.float32)
    c[:, 256:384] = (l[:, None] >= l[None, :]).astype(np.float32)
    c[:, 384:512] = 1.0
    return c


_NAMES = ["w_mod", "b_mod", "norm1_g", "norm2_g", "w_in", "b_gate", "ssd_conv_w", "ssd_conv_b", "ssd_dt_bias", "ssd_a_log",
          "ssd_d", "ssd_norm_g", "w_ssd_out", "sc_conv_w", "w_sc_out", "w_o", "ffn_w1", "ffn_w3", "ffn_w2", "router_w",
          "moe_w1", "moe_w3", "moe_w2", "final_g", "c_ctx"]


def kernel(**inputs):
    prog = Prog()
    nc = prog.build()
    shared = {n: np.ascontiguousarray(np.asarray(inputs[n], dtype=np.float32)) for n in _NAMES}
    shared["consts"] = _consts()
    x = np.asarray(inputs["x"], dtype=np.float32)
    c = np.asarray(inputs["c"], dtype=np.float32)
    ctx = np.asarray(inputs["ctx"], dtype=np.float32)
    in_maps = []
    for b in range(8):
        m = dict(shared)
        m["x"] = np.ascontiguousarray(x[b])
        m["c"] = np.ascontiguousarray(c[b])
        m["ctx"] = np.ascontiguousarray(ctx[b])
        in_maps.append(m)
    res = run_bass_kernel_spmd(nc, in_maps, core_ids=list(range(8)))
    return np.stack([np.asarray(r["out"]) for r in res.results], axis=0).astype(np.float32)
```
